# Optimizing a Trainium2 kernel written in Bass

```python
import jax, jax.numpy as jnp
from jax import lax
import numpy as np

D_MODEL = 2048
BATCH = 2
SEQ = 8192
DEPTH = 2

N_MIXERS = 2
N_ATTN_LAYERS = (DEPTH + 1) // 2
N_DN_LAYERS = DEPTH // 2
RMS_EPS = 1e-6

N_HEADS = 16
N_KV_HEADS = 4
HEAD_DIM = 128
ROT_FRACTION = 4
ROPE_THETA = 500000.0
IDX_HEADS = 16
IDX_DIM = 64
INDEX_TOPK = 256
Q_BLOCK = 128
ATTN_SPLITS = (N_HEADS * HEAD_DIM, N_KV_HEADS * HEAD_DIM, N_KV_HEADS * HEAD_DIM, IDX_HEADS * IDX_DIM, IDX_DIM, IDX_HEADS)
ATTN_PROJ = sum(ATTN_SPLITS)

DN_QK_HEADS = 16
DN_V_HEADS = 32
DN_HEAD_DIM = 128
DN_KEY_DIM = DN_QK_HEADS * DN_HEAD_DIM
DN_VAL_DIM = DN_V_HEADS * DN_HEAD_DIM
DN_CONV_WIDTH = 4
DN_CHUNK = 64
DN_CONV_CH = 2 * DN_KEY_DIM + DN_VAL_DIM
DN_SPLITS = (DN_KEY_DIM, DN_KEY_DIM, DN_VAL_DIM, DN_VAL_DIM, DN_V_HEADS, DN_V_HEADS)
DN_PROJ = sum(DN_SPLITS)

PEER_HEADS = 8
PEER_NKEYS = 128
PEER_EXPERTS = PEER_NKEYS * PEER_NKEYS
PEER_QDIM = 256
PEER_TOPK = 16
PEER_BLOCK = 128

PLE_DIM = 256

kernel_name = 'hybrid_dsa_gdn_peer_ple'

F32 = jnp.float32


def split_cols(y, sizes):
    offs = [int(o) for o in np.cumsum(sizes)[:-1]]
    return jnp.split(y, offs, axis=-1)


def rmsnorm(x, gain):
    xf = x.astype(F32)
    y = xf * lax.rsqrt(jnp.mean(xf * xf, axis=-1, keepdims=True) + RMS_EPS)
    return (y * gain.astype(F32)).astype(x.dtype)


def l2norm(x):
    xf = x.astype(F32)
    return xf * lax.rsqrt(jnp.sum(xf * xf, axis=-1, keepdims=True) + RMS_EPS)


def rope_partial(x, pos):
    d = x.shape[-1]
    rot = d // ROT_FRACTION
    half = rot // 2
    inv_freq = ROPE_THETA ** (-jnp.arange(half, dtype=F32) * (2.0 / rot))
    ang = pos.astype(F32)[:, None] * inv_freq[None, :]
    cos = jnp.cos(ang)[None, :, None, :]
    sin = jnp.sin(ang)[None, :, None, :]
    xf = x.astype(F32)
    x1, x2, rest = xf[..., :half], xf[..., half:rot], xf[..., rot:]
    return jnp.concatenate([x1 * cos - x2 * sin, x2 * cos + x1 * sin, rest], axis=-1).astype(x.dtype)


def dsa_attention(xn, w_in, q_gain, k_gain, w_out):
    B, T, _ = xn.shape
    pos = jnp.arange(T, dtype=jnp.int32)
    q, k, v, iq, ik, iw = split_cols(xn @ w_in, ATTN_SPLITS)
    q = rope_partial(rmsnorm(q.reshape(B, T, N_HEADS, HEAD_DIM), q_gain), pos)
    k = rope_partial(rmsnorm(k.reshape(B, T, N_KV_HEADS, HEAD_DIM), k_gain), pos)
    v = v.reshape(B, T, N_KV_HEADS, HEAD_DIM)
    iq = rope_partial(iq.reshape(B, T, IDX_HEADS, IDX_DIM), pos).astype(F32)
    ik = rope_partial(ik.reshape(B, T, 1, IDX_DIM), pos)[:, :, 0].astype(F32)
    iw = iw.astype(F32) * (IDX_HEADS ** -0.5)
    topk = min(INDEX_TOPK, T // 4)
    nb = T // Q_BLOCK
    group = N_HEADS // N_KV_HEADS

    def to_blocks(a):
        return jnp.moveaxis(a.reshape(B, nb, Q_BLOCK, *a.shape[2:]), 1, 0)

    def block(args):
        qb, iqb, iwb, pb = args
        s = jax.nn.relu(jnp.einsum('bqhd,bsd->bqhs', iqb, ik) * (IDX_DIM ** -0.5))
        score = jnp.einsum('bqhs,bqh->bqs', s, iwb)
        causal = pos[None, None, :] <= pb[None, :, None]
        score = jnp.where(causal, score, -jnp.inf)
        _, idx = lax.top_k(score, topk)
        valid = idx <= pb[None, :, None]
        kg = jax.vmap(lambda kb, ib: kb[ib])(k, idx)
        vg = jax.vmap(lambda vb, ib: vb[ib])(v, idx)
        qg = qb.reshape(B, Q_BLOCK, N_KV_HEADS, group, HEAD_DIM).astype(F32)
        logits = jnp.einsum('bqngd,bqknd->bqngk', qg, kg.astype(F32)) * (HEAD_DIM ** -0.5)
        logits = jnp.where(valid[:, :, None, None, :], logits, -jnp.inf)
        probs = jax.nn.softmax(logits, axis=-1)
        ob = jnp.einsum('bqngk,bqknd->bqngd', probs, vg.astype(F32))
        return ob.reshape(B, Q_BLOCK, N_HEADS * HEAD_DIM).astype(xn.dtype)

    o = lax.map(block, (to_blocks(q), to_blocks(iq), to_blocks(iw), pos.reshape(nb, Q_BLOCK)))
    o = jnp.moveaxis(o, 0, 1).reshape(B, T, N_HEADS * HEAD_DIM)
    return o @ w_out


def causal_depthwise_conv(x, w):
    return lax.conv_general_dilated(
        x, w[:, None, :].astype(x.dtype), window_strides=(1,),
        padding=[(DN_CONV_WIDTH - 1, 0)], dimension_numbers=('NWC', 'WIO', 'NWC'),
        feature_group_count=x.shape[-1])


def chunk_gated_delta_rule(q, k, v, g, beta):
    B, T, H, dk = k.shape
    dv = v.shape[-1]
    C = DN_CHUNK
    N = T // C

    def chunks(a):
        return jnp.moveaxis(a.reshape(B, N, C, H, *a.shape[3:]), 3, 1)

    q = chunks(q) * (dk ** -0.5)
    k = chunks(k)
    v = chunks(v)
    g = jnp.cumsum(chunks(g), axis=-1)
    beta = chunks(beta)
    ar = jnp.arange(C)
    tril = ar[:, None] >= ar[None, :]
    strict = ar[:, None] > ar[None, :]
    decay = jnp.exp(jnp.where(tril, g[..., :, None] - g[..., None, :], -jnp.inf))
    k_beta = k * beta[..., None]
    a = jnp.where(strict, jnp.einsum('bhncd,bhnsd->bhncs', k_beta, k) * decay, 0.0)
    m = a + jnp.eye(C, dtype=a.dtype)
    rhs = jnp.concatenate([v * beta[..., None], k_beta * jnp.exp(g)[..., None]], axis=-1)
    sol = lax.linalg.triangular_solve(m, rhs, left_side=True, lower=True, unit_diagonal=True)
    u, w = sol[..., :dv], sol[..., dv:]
    qk = jnp.einsum('bhncd,bhnsd->bhncs', q, k) * decay

    def step(state, xs):
        q_i, k_i, u_i, w_i, qk_i, g_i = xs
        v_new = u_i - jnp.einsum('bhcd,bhde->bhce', w_i, state)
        o_i = (jnp.einsum('bhcd,bhde->bhce', q_i * jnp.exp(g_i)[..., None], state)
               + jnp.einsum('bhcs,bhse->bhce', qk_i, v_new))
        g_last = g_i[..., -1:]
        state = (state * jnp.exp(g_last)[..., None]
                 + jnp.einsum('bhcd,bhce->bhde', k_i * jnp.exp(g_last - g_i)[..., None], v_new))
        return state, o_i

    xs = tuple(jnp.moveaxis(t, 2, 0) for t in (q, k, u, w, qk, g))
    state0 = jnp.zeros((B, H, dk, dv), F32)
    _, o = lax.scan(step, state0, xs)
    return jnp.transpose(o, (1, 0, 3, 2, 4)).reshape(B, T, H, dv)


def gated_deltanet(xn, w_in, conv_w, a_log, dt_bias, norm_gain, w_out):
    B, T, _ = xn.shape
    q, k, v, z, b, a = split_cols(xn @ w_in, DN_SPLITS)
    qkv = jax.nn.silu(causal_depthwise_conv(jnp.concatenate([q, k, v], axis=-1), conv_w))
    q, k, v = split_cols(qkv, (DN_KEY_DIM, DN_KEY_DIM, DN_VAL_DIM))
    rep = DN_V_HEADS // DN_QK_HEADS
    q = jnp.repeat(l2norm(q.reshape(B, T, DN_QK_HEADS, DN_HEAD_DIM)), rep, axis=2)
    k = jnp.repeat(l2norm(k.reshape(B, T, DN_QK_HEADS, DN_HEAD_DIM)), rep, axis=2)
    v = v.reshape(B, T, DN_V_HEADS, DN_HEAD_DIM).astype(F32)
    beta = jax.nn.sigmoid(b.astype(F32))
    g = -jnp.exp(a_log.astype(F32)) * jax.nn.softplus(a.astype(F32) + dt_bias.astype(F32))
    o = chunk_gated_delta_rule(q, k, v, g, beta)
    o = rmsnorm(o, norm_gain) * jax.nn.silu(z.reshape(B, T, DN_V_HEADS, DN_HEAD_DIM).astype(F32))
    return o.reshape(B, T, DN_VAL_DIM).astype(xn.dtype) @ w_out


def peer_ffn(xn, w_q, sub_keys, u_tab, v_tab):
    B, T, D = xn.shape
    q = (xn @ w_q).reshape(B, T, PEER_HEADS, 2, PEER_QDIM // 2).astype(F32)
    s = jnp.einsum('bthpd,hpkd->bthpk', q, sub_keys.astype(F32))
    s_top, i_top = lax.top_k(s, PEER_TOPK)
    cand_s = (s_top[..., 0, :, None] + s_top[..., 1, None, :]).reshape(B, T, PEER_HEADS, PEER_TOPK * PEER_TOPK)
    cand_i = (i_top[..., 0, :, None] * PEER_NKEYS + i_top[..., 1, None, :]).reshape(B, T, PEER_HEADS, PEER_TOPK * PEER_TOPK)
    f_s, f_pos = lax.top_k(cand_s, PEER_TOPK)
    eid = jnp.take_along_axis(cand_i, f_pos, axis=-1)
    gate = jax.nn.softmax(f_s, axis=-1)
    nsel = PEER_HEADS * PEER_TOPK
    nb = (B * T) // PEER_BLOCK
    xb = xn.reshape(nb, PEER_BLOCK, D)
    eb = eid.reshape(nb, PEER_BLOCK, nsel)
    gb = gate.reshape(nb, PEER_BLOCK, nsel)

    def block(args):
        x_i, e_i, g_i = args
        act = jnp.einsum('nkd,nd->nk', u_tab[e_i], x_i)
        coef = (g_i * jax.nn.gelu(act.astype(F32), approximate=False)).astype(x_i.dtype)
        return jnp.einsum('nk,nkd->nd', coef, v_tab[e_i])

    y = lax.map(block, (xb, eb, gb))
    return y.reshape(B, T, D)


def setup_inputs(seed: int = 0) -> dict:
    key = jax.random.key(seed)
    ks = jax.random.split(key, 24)

    def nrm(k, shape, scale):
        return jax.random.normal(k, shape, F32) * scale

    def gain(k, shape):
        return 1.0 + 0.05 * jax.random.normal(k, shape, F32)

    NA, NB = N_ATTN_LAYERS, N_DN_LAYERS
    return {
        'x': nrm(ks[0], (BATCH, SEQ, D_MODEL), 1.0),
        'p': nrm(ks[1], (DEPTH, BATCH, SEQ, PLE_DIM), 1.0),
        'norm_mix': gain(ks[2], (DEPTH, D_MODEL)),
        'norm_ffn': gain(ks[3], (DEPTH, D_MODEL)),
        'norm_ple': gain(ks[4], (DEPTH, D_MODEL)),
        'attn_w_in': nrm(ks[5], (NA, D_MODEL, ATTN_PROJ), D_MODEL ** -0.5),
        'attn_q_norm': gain(ks[6], (NA, HEAD_DIM)),
        'attn_k_norm': gain(ks[7], (NA, HEAD_DIM)),
        'attn_w_out': nrm(ks[8], (NA, N_HEADS * HEAD_DIM, D_MODEL), (N_HEADS * HEAD_DIM) ** -0.5),
        'dn_w_in': nrm(ks[9], (NB, D_MODEL, DN_PROJ), D_MODEL ** -0.5),
        'dn_conv': nrm(ks[10], (NB, DN_CONV_WIDTH, DN_CONV_CH), DN_CONV_WIDTH ** -0.5),
        'dn_a_log': jnp.log(jax.random.uniform(ks[11], (NB, DN_V_HEADS), F32, 1.0, 16.0)),
        'dn_dt_bias': nrm(ks[12], (NB, DN_V_HEADS), 0.1),
        'dn_norm': gain(ks[13], (NB, DN_HEAD_DIM)),
        'dn_w_out': nrm(ks[14], (NB, DN_VAL_DIM, D_MODEL), DN_VAL_DIM ** -0.5),
        'peer_w_q': nrm(ks[15], (DEPTH, D_MODEL, PEER_HEADS * PEER_QDIM), D_MODEL ** -0.5),
        'peer_keys': nrm(ks[16], (DEPTH, PEER_HEADS, 2, PEER_NKEYS, PEER_QDIM // 2), (PEER_QDIM // 2) ** -0.5),
        'peer_u': nrm(ks[17], (DEPTH, PEER_EXPERTS, D_MODEL), D_MODEL ** -0.5),
        'peer_v': nrm(ks[18], (DEPTH, PEER_EXPERTS, D_MODEL), (PEER_HEADS * PEER_TOPK) ** -0.5),
        'ple_w_in': nrm(ks[19], (DEPTH, PLE_DIM, D_MODEL), PLE_DIM ** -0.5),
        'ple_w_gate': nrm(ks[20], (DEPTH, D_MODEL, D_MODEL), D_MODEL ** -0.5),
    }


def reference(x, p, norm_mix, norm_ffn, norm_ple, attn_w_in, attn_q_norm, attn_k_norm, attn_w_out,
              dn_w_in, dn_conv, dn_a_log, dn_dt_bias, dn_norm, dn_w_out,
              peer_w_q, peer_keys, peer_u, peer_v, ple_w_in, ple_w_gate):
    h = x
    for i in range(DEPTH):
        hn = rmsnorm(h, norm_mix[i])
        j = i // N_MIXERS
        if i % N_MIXERS == 0:
            mix = dsa_attention(hn, attn_w_in[j], attn_q_norm[j], attn_k_norm[j], attn_w_out[j])
        else:
            mix = gated_deltanet(hn, dn_w_in[j], dn_conv[j], dn_a_log[j], dn_dt_bias[j], dn_norm[j], dn_w_out[j])
        h = h + mix
        h = h + peer_ffn(rmsnorm(h, norm_ffn[i]), peer_w_q[i], peer_keys[i], peer_u[i], peer_v[i])
        gate = jax.nn.sigmoid((rmsnorm(h, norm_ple[i]) @ ple_w_gate[i]).astype(F32))
        h = h + (gate * (p[i] @ ple_w_in[i]).astype(F32)).astype(h.dtype)
    return h
```

```python
import numpy as np
from contextlib import ExitStack
import concourse.bass as bass
import concourse.mybir as mybir
from concourse.bass_utils import run_bass_kernel_spmd

F32 = mybir.dt.float32
BF16 = mybir.dt.bfloat16
I32 = mybir.dt.int32
U32 = mybir.dt.uint32
AF = mybir.ActivationFunctionType
ALU = mybir.AluOpType
AX = mybir.AxisListType

ENGS = ("pe", "act", "dve", "pool", "sp")


class T:
    __slots__ = ("t", "name", "lw", "rd")

    def __init__(self, t, name):
        self.t = t
        self.name = name
        self.lw = None
        self.rd = []

    def __getitem__(self, idx):
        return self.t[idx]


class TV:
    __slots__ = ("t", "name", "par")

    def __init__(self, par, ap, name):
        self.par = par
        self.t = ap
        self.name = name

    def __getitem__(self, idx):
        return self.t[idx]

    @property
    def lw(self):
        return self.par.lw

    @lw.setter
    def lw(self, v):
        self.par.lw = v

    @property
    def rd(self):
        return self.par.rd

    @rd.setter
    def rd(self, v):
        self.par.rd = v


class Prog:
    def __init__(self, name="k", ndma=6, same_eng_sync=True):
        self.nc = bass.Bass("TRN2", target_bir_lowering=False)
        self.es = ExitStack()
        self.ops = {e: [] for e in ENGS}
        self.cnt = {e: 0 for e in ENGS}
        self.known = {e: {} for e in ENGS}
        self.sem = {}
        for e in ENGS:
            self.sem[e] = self.es.enter_context(self.nc.semaphore("c_" + e))
        self.ndma = ndma
        self.dsem = {}
        self.dcnt = {}
        for q in ("sp", "act", "pool"):
            self.dsem[q] = [self.es.enter_context(self.nc.semaphore(f"d_{q}{i}")) for i in range(ndma)]
            self.dcnt[q] = 0
        self.same = same_eng_sync
        self.n_t = 0
        self.phase_es = None
        self.dram_dep = {}
        self.ccsem = self.es.enter_context(self.nc.semaphore("ccsem"))
        self.ccn = 0
        self.ext_in = []
        self.ext_out = []

    def dram(self, name, shape, dt, kind):
        return self.nc.dram_tensor(name, list(shape), dt, kind=kind).ap()

    def sb(self, shape, dt, name=None):
        self.n_t += 1
        name = name or f"sb{self.n_t}"
        t = (self.phase_es or self.es).enter_context(self.nc.sbuf_tensor(name, list(shape), dt))
        return T(t, name)

    def ps(self, shape, dt=F32, name=None):
        self.n_t += 1
        name = name or f"ps{self.n_t}"
        t = (self.phase_es or self.es).enter_context(self.nc.psum_tensor(name, list(shape), dt))
        return T(t, name)

    def scratch(self, name, shape, dt):
        t = self.nc.dram_tensor(name, list(shape), dt)
        self.dram_dep[name] = T(None, name)
        return t.ap()

    def begin_phase(self):
        self.phase_es = ExitStack()

    def end_phase(self):
        self.barrier()
        self.phase_es.close()
        self.phase_es = None

    def barrier(self):
        for e in ENGS:
            toks = []
            for f in ENGS:
                if f != e and self.cnt[f] > 0:
                    toks.append((f, self.sem[f], self.cnt[f], f))
            for q in ("sp", "act", "pool"):
                n = self.dcnt[q]
                for slot in range(min(n, self.ndma)):
                    last_i = ((n - 1 - slot) // self.ndma) * self.ndma + slot
                    toks.append((f"d_{q}{slot}", self.dsem[q][slot], 16 * (last_i // self.ndma + 1), "dma"))
            if self.ccn:
                toks.append(("cc", self.ccsem, self.ccn, "dma"))
            waits = self._waits(e, toks)
            self.ops[e].append((waits, None, None))

    def coll(self, kind, op, groups, src, dst):
        reads = [self.dram_dep[src.name]]
        writes = [self.dram_dep[dst.name]]
        waits = self._waits("pool", self._deps(reads, writes))
        self.ccn += 1
        tok = ("cc", self.ccsem, self.ccn, "dma")

        def fn(e, kind=kind, op=op, groups=groups, src=src, dst=dst):
            return e.collective_compute(kind, op, replica_groups=groups, ins=[src.opt()], outs=[dst.opt()])
        self.ops["pool"].append((waits, fn, (self.ccsem, 1)))
        self._commit(tok, reads, writes)

    def ext(self, name):
        return T(None, name)

    def _deps(self, reads, writes):
        toks = []
        for r in reads:
            if r.lw is not None:
                toks.append(r.lw)
        for w in writes:
            if w.lw is not None:
                toks.append(w.lw)
            toks.extend(w.rd)
        return toks

    def _waits(self, eng, toks):
        kn = self.known[eng]
        need = {}
        for (key, semh, val, src_eng) in toks:
            if src_eng == eng and (not self.same or eng == "pe"):
                continue
            if kn.get(key, 0) >= val:
                continue
            if key not in need or need[key][1] < val:
                need[key] = (semh, val)
        out = []
        for key, (semh, val) in need.items():
            kn[key] = val
            out.append((semh, val))
        return out

    def _commit(self, tok, reads, writes):
        for w in writes:
            w.lw = tok
            w.rd = []
        for r in reads:
            if r in writes:
                continue
            r.rd.append(tok)
            if len(r.rd) > 24:
                best = {}
                for t in r.rd:
                    if t[0] not in best or best[t[0]][2] < t[2]:
                        best[t[0]] = t
                r.rd = list(best.values())

    def op(self, eng, fn, reads=(), writes=()):
        reads = [r for r in reads if r is not None]
        writes = [w for w in writes if w is not None]
        waits = self._waits(eng, self._deps(reads, writes))
        self.cnt[eng] += 1
        seq = self.cnt[eng]
        tok = (eng, self.sem[eng], seq, eng)
        self.ops[eng].append((waits, fn, (self.sem[eng], 1)))
        self._commit(tok, reads, writes)
        return tok

    def dma(self, q, out, in_, reads=(), writes=(), **kw):
        reads = [r for r in reads if r is not None]
        writes = [w for w in writes if w is not None]
        nm = getattr(in_, "name", None)
        if nm in self.dram_dep:
            reads.append(self.dram_dep[nm])
        nm = getattr(out, "name", None)
        if nm in self.dram_dep:
            writes.append(self.dram_dep[nm])
        i = self.dcnt[q]
        self.dcnt[q] += 1
        slot = i % self.ndma
        val = 16 * (i // self.ndma + 1)
        semh = self.dsem[q][slot]
        key = f"d_{q}{slot}"
        toks = self._deps(reads, writes)
        if i >= self.ndma:
            toks.append((key, semh, val - 16, "dma"))
        waits = self._waits(q, toks)
        tok = (key, semh, val, "dma")

        def fn(e, out=out, in_=in_, kw=kw):
            return e.dma_start(out=out, in_=in_, **kw)
        self.ops[q].append((waits, fn, (semh, 16)))
        self._commit(tok, reads, writes)
        return tok

    def finish(self, final_toks=()):
        nc = self.nc
        fin = []
        for q in ("sp", "act", "pool"):
            n = self.dcnt[q]
            for slot in range(min(n, self.ndma)):
                last_i = ((n - 1 - slot) // self.ndma) * self.ndma + slot
                fin.append((f"d_{q}{slot}", self.dsem[q][slot], 16 * (last_i // self.ndma + 1), "dma"))
        for e in ENGS:
            if e != "sp" and self.cnt[e] > 0:
                fin.append((e, self.sem[e], self.cnt[e], e))
        if self.ccn:
            fin.append(("cc", self.ccsem, self.ccn, "dma"))
        fwaits = self._waits("sp", fin)
        ops = self.ops

        def run(e, lst, extra=()):
            for waits, fn, inc in lst:
                for (s, v) in waits:
                    e.wait_ge(s, v)
                if fn is not None:
                    fn(e).then_inc(inc[0], inc[1])
            for (s, v) in extra:
                e.wait_ge(s, v)

        with nc.Block() as block:
            @block.sync
            def _(e):
                run(e, ops["sp"], fwaits)

            @block.tensor
            def _(e):
                run(e, ops["pe"])

            @block.scalar
            def _(e):
                run(e, ops["act"])

            @block.vector
            def _(e):
                run(e, ops["dve"])

            @block.gpsimd
            def _(e):
                run(e, ops["pool"])
        self.es.close()
        return nc

    def stats(self):
        return {e: len(self.ops[e]) for e in ENGS}


class IO:
    def __init__(self, P, prefix, bind=None):
        self.P = P
        self.prefix = prefix
        self.bind = bind or {}

    def __call__(self, name, shape, dt, kind):
        if name in self.bind:
            ap = self.bind[name]
            assert list(ap.shape) == list(shape), (name, ap.shape, shape)
            return ap
        full = self.prefix + name
        if kind == "ExternalInput":
            self.P.ext_in.append(full)
        else:
            self.P.ext_out.append(full)
        return self.P.dram(full, shape, dt, kind)


import ml_dtypes
BF = ml_dtypes.bfloat16


D = 2048; NT = 2048; TT = 512; KC = 16
EPS = 1e-6

def emit_A(P, io):
    xT = io("xT", [D, NT], F32, "ExternalInput")
    gmix = io("gmix", [128, KC], F32, "ExternalInput")
    w_in = io("w_in", [D, 4176], F32, "ExternalInput")
    qkg = io("qkg", [128, 2], F32, "ExternalInput")
    csq = io("csq", [32, 2, NT], F32, "ExternalInput")
    csi = io("csi", [128, 2, NT], F32, "ExternalInput")
    perm = io("perm", [128, 2, 128], F32, "ExternalInput")
    qT = io("qT", [2048, NT], BF16, "ExternalOutput")
    kT = io("kT", [512, NT], BF16, "ExternalOutput")
    vv = io("v", [NT, 512], BF16, "ExternalOutput")
    iqT = io("iqT", [1024, NT], F32, "ExternalOutput")
    ikT = io("ikT", [64, NT], F32, "ExternalOutput")
    iw = io("iw", [NT, 16], F32, "ExternalOutput")

    gm = P.sb([128, KC], F32); qk = P.sb([128, 2], F32); pm = P.sb([128, 2, 128], F32)
    ones_b = P.sb([128, 128], BF16); ones_f = P.sb([128, 128], F32); epsb = P.sb([128, 1], F32)
    P.dma("sp", gm[:], gmix[:, :], writes=[gm]); P.dma("sp", qk[:], qkg[:, :], writes=[qk]); P.dma("sp", pm[:], perm[:, :, :], writes=[pm])
    P.op("dve", lambda e: e.memset(ones_b[:], 1.0), writes=[ones_b])
    P.op("dve", lambda e: e.memset(ones_f[:], 1.0 / 128), writes=[ones_f])
    P.op("dve", lambda e: e.memset(epsb[:], EPS), writes=[epsb])

    xs = P.sb([128, KC, TT], F32)
    sq = P.sb([128, KC, TT], BF16)
    hb = P.sb([128, KC, TT], BF16)
    rstd = P.sb([128, TT], F32)
    cq = P.sb([32, 2, TT], F32); ci = P.sb([128, 2, TT], F32)
    w32 = [P.sb([128, KC, 512], F32) for _ in range(2)]
    wb = [P.sb([128, KC, 512], BF16) for _ in range(2)]
    ps_ms = P.ps([128, TT]); ps_pj = [P.ps([128, TT]) for _ in range(2)]; ps_ms2 = P.ps([128, TT]); ps_rot = P.ps([128, TT])
    sq2 = [P.sb([128, TT], F32) for _ in range(2)]
    r2 = [P.sb([128, TT], F32) for _ in range(2)]
    qn = [P.sb([128, TT], F32) for _ in range(2)]
    t1 = [P.sb([128, TT], F32) for _ in range(2)]
    t2 = [P.sb([128, TT], F32) for _ in range(2)]
    ob = [P.sb([128, TT], BF16) for _ in range(2)]
    of = [P.sb([128, TT], F32) for _ in range(2)]
    vb = [P.sb([128, 512], BF16) for _ in range(2)]
    iwb = [P.sb([128, 16], F32) for _ in range(2)]
    xTv = xT.rearrange("(c p) t -> p c t", p=128)
    wv = w_in.rearrange("(c p) n -> p c n", p=128)
    cnt = [0]
    for tt in range(NT // TT):
        ts = slice(tt * TT, (tt + 1) * TT)
        P.dma("sp", xs[:], xTv[:, :, ts], writes=[xs])
        P.dma("act", cq[:], csq[:, :, ts], writes=[cq])
        P.dma("act", ci[:], csi[:, :, ts], writes=[ci])
        P.op("act", lambda e: e.activation(out=sq[:], in_=xs[:], func=AF.Square), reads=[xs], writes=[sq])
        for c in range(KC):
            P.op("pe", lambda e, c=c: e.matmul(ps_ms[:], lhsT=ones_b[:], rhs=sq[:, c, :], start=(c == 0), stop=(c == KC - 1)), reads=[ones_b, sq], writes=[ps_ms])
        P.op("act", lambda e: e.activation(out=rstd[:], in_=ps_ms[:], func=AF.Sqrt, bias=epsb[:, 0:1], scale=1.0 / D), reads=[ps_ms, epsb], writes=[rstd])
        P.op("dve", lambda e: e.reciprocal(out=rstd[:], in_=rstd[:]), reads=[rstd], writes=[rstd])
        for c in range(KC):
            P.op("dve", lambda e, c=c: e.scalar_tensor_tensor(out=xs[:, c, :], in0=xs[:, c, :], scalar=gm[:, c:c + 1], in1=rstd[:], op0=ALU.mult, op1=ALU.mult), reads=[xs, gm, rstd], writes=[xs])
        P.op("pool", lambda e: e.tensor_copy(out=hb[:], in_=xs[:]), reads=[xs], writes=[hb])
        for g in range(9):
            n0 = g * 512; ncol = 512 if g < 8 else 80
            bi = cnt[0] % 2; cnt[0] += 1
            W32 = w32[bi]; WB = wb[bi]
            P.dma("sp", W32[:, :, 0:ncol], wv[:, :, n0:n0 + ncol], writes=[W32])
            if g <= 5:
                P.op("pool", lambda e, W32=W32, WB=WB: e.tensor_copy(out=WB[:], in_=W32[:]), reads=[W32], writes=[WB])
            if g <= 4:
                for j in range(4):
                    ch = g * 4 + j
                    pj = ps_pj[ch % 2]; S2 = sq2[ch % 2]; R2 = r2[ch % 2]; QN = qn[ch % 2]; T1 = t1[ch % 2]; T2 = t2[ch % 2]; OB = ob[ch % 2]
                    for c in range(KC):
                        P.op("pe", lambda e, c=c, j=j, pj=pj, WB=WB: e.matmul(pj[:], lhsT=WB[:, c, j * 128:(j + 1) * 128], rhs=hb[:, c, :], start=(c == 0), stop=(c == KC - 1)), reads=[WB, hb], writes=[pj])
                    P.op("act", lambda e, pj=pj, S2=S2: e.activation(out=S2[:], in_=pj[:], func=AF.Square), reads=[pj], writes=[S2])
                    P.op("pe", lambda e, S2=S2: e.matmul(ps_ms2[:], lhsT=ones_f[:], rhs=S2[:], start=True, stop=True), reads=[ones_f, S2], writes=[ps_ms2])
                    P.op("act", lambda e, R2=R2: e.activation(out=R2[:], in_=ps_ms2[:], func=AF.Sqrt, bias=epsb[:, 0:1], scale=1.0), reads=[ps_ms2, epsb], writes=[R2])
                    P.op("dve", lambda e, R2=R2: e.reciprocal(out=R2[:], in_=R2[:]), reads=[R2], writes=[R2])
                    gi = 0 if g < 4 else 1
                    P.op("dve", lambda e, pj=pj, QN=QN, R2=R2, gi=gi: e.scalar_tensor_tensor(out=QN[:], in0=pj[:], scalar=qk[:, gi:gi + 1], in1=R2[:], op0=ALU.mult, op1=ALU.mult), reads=[pj, qk, R2], writes=[QN])
                    P.op("pe", lambda e, QN=QN: e.matmul(ps_rot[0:32, :], lhsT=pm[0:32, 0, 0:32], rhs=QN[0:32, :], start=True, stop=True), reads=[pm, QN], writes=[ps_rot])
                    P.op("pool", lambda e, QN=QN, T1=T1: e.tensor_tensor(out=T1[0:32, :], in0=QN[0:32, :], in1=cq[:, 0, :], op=ALU.mult), reads=[QN, cq], writes=[T1])
                    P.op("dve", lambda e, T2=T2: e.tensor_tensor(out=T2[0:32, :], in0=ps_rot[0:32, :], in1=cq[:, 1, :], op=ALU.mult), reads=[ps_rot, cq], writes=[T2])
                    P.op("pool", lambda e, QN=QN, T1=T1, T2=T2: e.tensor_tensor(out=QN[0:32, :], in0=T1[0:32, :], in1=T2[0:32, :], op=ALU.add), reads=[T1, T2], writes=[QN])
                    P.op("act", lambda e, QN=QN, OB=OB: e.activation(out=OB[:], in_=QN[:], func=AF.Copy), reads=[QN], writes=[OB])
                    dst = qT[ch * 128:(ch + 1) * 128, ts] if g < 4 else kT[j * 128:(j + 1) * 128, ts]
                    P.dma("pool", dst, OB[:], reads=[OB])
            elif g == 5:
                for s in range(4):
                    pj = ps_pj[s % 2]; VB = vb[s % 2]
                    for c in range(KC):
                        P.op("pe", lambda e, c=c, s=s, pj=pj, WB=WB: e.matmul(pj[:], lhsT=hb[:, c, s * 128:(s + 1) * 128], rhs=WB[:, c, :], start=(c == 0), stop=(c == KC - 1)), reads=[WB, hb], writes=[pj])
                    P.op("act", lambda e, pj=pj, VB=VB: e.activation(out=VB[:], in_=pj[:], func=AF.Copy), reads=[pj], writes=[VB])
                    P.dma("pool", vv[tt * TT + s * 128: tt * TT + (s + 1) * 128, :], VB[:], reads=[VB])
            else:
                nch = 4 if g < 8 else 1
                for j in range(nch):
                    M = 128 if g < 8 else 64
                    ch = (g - 6) * 4 + j
                    pj = ps_pj[j % 2]; QN = qn[j % 2]; T1 = t1[j % 2]; T2 = t2[j % 2]; OF = of[j % 2]
                    for c in range(KC):
                        P.op("pe", lambda e, c=c, j=j, pj=pj, W32=W32, M=M: e.matmul(pj[0:M, :], lhsT=W32[:, c, j * 128:j * 128 + M], rhs=xs[:, c, :], start=(c == 0), stop=(c == KC - 1)), reads=[W32, xs], writes=[pj])
                    P.op("act", lambda e, pj=pj, QN=QN, M=M: e.activation(out=QN[0:M, :], in_=pj[0:M, :], func=AF.Copy), reads=[pj], writes=[QN])
                    P.op("pe", lambda e, QN=QN, M=M: e.matmul(ps_rot[0:M, :], lhsT=pm[0:M, 1, 0:M], rhs=QN[0:M, :], start=True, stop=True), reads=[pm, QN], writes=[ps_rot])
                    P.op("pool", lambda e, QN=QN, T1=T1, M=M: e.tensor_tensor(out=T1[0:M, :], in0=QN[0:M, :], in1=ci[0:M, 0, :], op=ALU.mult), reads=[QN, ci], writes=[T1])
                    P.op("dve", lambda e, T2=T2, M=M: e.tensor_tensor(out=T2[0:M, :], in0=ps_rot[0:M, :], in1=ci[0:M, 1, :], op=ALU.mult), reads=[ps_rot, ci], writes=[T2])
                    P.op("pool", lambda e, OF=OF, T1=T1, T2=T2, M=M: e.tensor_tensor(out=OF[0:M, :], in0=T1[0:M, :], in1=T2[0:M, :], op=ALU.add), reads=[T1, T2], writes=[OF])
                    dst = iqT[ch * 128:(ch + 1) * 128, ts] if g < 8 else ikT[:, ts]
                    P.dma("pool", dst, OF[0:M, :], reads=[OF])
                if g == 8:
                    for s in range(4):
                        pj = ps_pj[s % 2]; IW = iwb[s % 2]
                        for c in range(KC):
                            P.op("pe", lambda e, c=c, s=s, pj=pj, W32=W32: e.matmul(pj[:, 0:16], lhsT=xs[:, c, s * 128:(s + 1) * 128], rhs=W32[:, c, 64:80], start=(c == 0), stop=(c == KC - 1)), reads=[W32, xs], writes=[pj])
                        P.op("act", lambda e, pj=pj, IW=IW: e.activation(out=IW[:], in_=pj[:, 0:16], func=AF.Copy), reads=[pj], writes=[IW])
                        P.dma("pool", iw[tt * TT + s * 128: tt * TT + (s + 1) * 128, :], IW[:], reads=[IW])
    return


def rope_tables(pos):
    pos = pos.astype(np.float32)
    theta = np.float32(500000.0)
    f16 = (theta ** (-np.arange(16, dtype=np.float32) * np.float32(2.0 / 32))).astype(np.float32)
    ang = pos[None, :] * f16[:, None]
    csq = np.zeros((32, 2, len(pos)), np.float32)
    csq[0:16, 0] = np.cos(ang); csq[16:32, 0] = np.cos(ang)
    csq[0:16, 1] = np.sin(ang); csq[16:32, 1] = np.sin(ang)
    f8 = (theta ** (-np.arange(8, dtype=np.float32) * np.float32(2.0 / 16))).astype(np.float32)
    ang8 = pos[None, :] * f8[:, None]
    csi = np.zeros((128, 2, len(pos)), np.float32)
    csi[:, 0] = 1.0
    for h in range(2):
        for half in range(2):
            csi[h * 64 + half * 8: h * 64 + half * 8 + 8, 0] = np.cos(ang8)
            csi[h * 64 + half * 8: h * 64 + half * 8 + 8, 1] = np.sin(ang8)
    perm = np.zeros((128, 2, 128), np.float32)
    for m in range(16):
        perm[m + 16, 0, m] = -1.0
        perm[m, 0, m + 16] = 1.0
    for h in range(2):
        for m in range(8):
            perm[h * 64 + m + 8, 1, h * 64 + m] = -1.0
            perm[h * 64 + m, 1, h * 64 + m + 8] = 1.0
    return csq, csi, perm


def host_A(x, norm_mix0, attn_w_in, qn, kn):
    maps = []
    for core in range(8):
        b = core // 4; t0 = (core % 4) * NT
        pos = np.arange(t0, t0 + NT)
        csq, csi, perm = rope_tables(pos)
        maps.append({
            "xT": np.ascontiguousarray(x[b, t0:t0 + NT, :].T),
            "gmix": np.ascontiguousarray(norm_mix0.reshape(KC, 128).T),
            "w_in": attn_w_in,
            "qkg": np.ascontiguousarray(np.stack([qn, kn], axis=1)),
            "csq": csq, "csi": csi, "perm": perm,
        })
    return maps


TKEYS = 8192; NQT = 16
NBIS = 24; LO0 = -2048.0; TOPK = 256.0

def emit_B(P, io, nqt=NQT):
    n_sc = 0; n_v = 0; n_pl = 0
    qTc = io("qTc", [2048, 2048], BF16, "ExternalInput")
    kT = io("kT", [2048, 2048], BF16, "ExternalInput")
    vv = io("v", [TKEYS, 512], BF16, "ExternalInput")
    ikT = io("ikT", [256, 2048], F32, "ExternalInput")
    iqTc = io("iqTc", [1024, 2048], F32, "ExternalInput")
    iwc = io("iwc", [2048, 16], F32, "ExternalInput")
    cbias = io("cbias", [128, 512], F32, "ExternalInput")
    identd = io("ident", [128, 128], BF16, "ExternalInput")
    oT = io("oT", [2048, 2048], BF16, "ExternalOutput")

    kt = P.sb([128, 4, TKEYS], BF16); ik = P.sb([64, TKEYS], F32)
    iw = P.sb([128, NQT, 16], F32); cb = P.sb([128, 512], F32); ident = P.sb([128, 128], BF16)
    ones_b = P.sb([128, 128], BF16)
    for g in range(4):
        for r in range(4):
            P.dma("sp", kt[:, g, :].rearrange("d (j r t) -> d j r t", r=4, t=128)[:, :, r, :], kT[(g // 2) * 1024 + r * 256 + (g % 2) * 128:(g // 2) * 1024 + r * 256 + (g % 2) * 128 + 128, :].rearrange("d (j t) -> d j t", t=128), writes=[kt])
    for r in range(4):
        P.dma("act", ik[:].rearrange("d (j r t) -> d j r t", r=4, t=128)[:, :, r, :], ikT[r * 64:(r + 1) * 64, :].rearrange("d (j t) -> d j t", t=128), writes=[ik])
    P.dma("act", iw[:], iwc.rearrange("(j p) h -> p j h", p=128), writes=[iw])
    P.dma("act", cb[:], cbias[:, :], writes=[cb]); P.dma("act", ident[:], identd[:, :], writes=[ident])
    P.op("dve", lambda e: e.memset(ones_b[:], 1.0), writes=[ones_b])

    score = P.sb([128, TKEYS], F32); junk = P.sb([128, TKEYS], BF16)
    maskT = P.sb([128, 64, 128], BF16)
    qt = [P.sb([128, 16, 128], BF16) for _ in range(2)]
    iq = [P.sb([64, 16, 128], F32) for _ in range(1)]
    rl = [P.sb([128, 512], F32) for _ in range(4)]
    mk_ = [P.sb([128, 512], BF16) for _ in range(2)]
    vt = [P.sb([128, 4, 128], BF16) for _ in range(3)]
    pe_ = [P.sb([128, 512], BF16) for _ in range(3)]
    pmk = [P.sb([128, 512], BF16) for _ in range(3)]
    lo = P.sb([128, 1], F32); mid = P.sb([128, 1], F32); cntall = P.sb([128, NBIS], F32); tmp = P.sb([128, 1], F32)
    rs = P.sb([128, 512], F32); ob = [P.sb([128, 512], BF16) for _ in range(2)]
    ps_sc = [P.ps([128, 512]) for _ in range(2)]
    ps_tr = P.ps([128, 4, 128], BF16)
    ps_pl = [P.ps([128, 512]) for _ in range(2)]
    ps_o = P.ps([128, 512]); ps_s = P.ps([128, 512])
    sc_rot = [ps_sc[0], ps_sc[1], ps_pl[0], ps_pl[1]]
    qv = qTc.rearrange("(h d) q -> d h q", d=128)
    iqv = iqTc.rearrange("(h d) q -> d h q", d=64)
    SCALE = 128 ** -0.5
    for j in range(nqt):
        NCH = j + 1
        QT = qt[j % 2]; IQ = iq[0]
        P.dma("sp", QT[:], qv[:, :, j * 128:(j + 1) * 128], writes=[QT])
        P.dma("sp", IQ[:], iqv[:, :, j * 128:(j + 1) * 128], writes=[IQ])
        for ch in range(NCH):
            cs = slice(ch * 512, (ch + 1) * 512)
            for h in range(16):
                ps = sc_rot[n_sc % 4]; RL = rl[n_sc % 4]; n_sc += 1
                P.op("pe", lambda e, ps=ps, h=h, cs=cs, IQ=IQ: e.matmul(ps[:], lhsT=IQ[:, h, :], rhs=ik[:, cs], start=True, stop=True), reads=[IQ, ik], writes=[ps])
                P.op("act", lambda e, ps=ps, RL=RL: e.activation(out=RL[:], in_=ps[:], func=AF.Relu), reads=[ps], writes=[RL])
                if h == 0:
                    P.op("dve", lambda e, RL=RL, cs=cs, j=j, h=h: e.tensor_scalar(out=score[:, cs], in0=RL[:], scalar1=iw[:, j, h:h + 1], scalar2=None, op0=ALU.mult), reads=[RL, iw], writes=[score])
                else:
                    P.op("dve", lambda e, RL=RL, cs=cs, j=j, h=h: e.scalar_tensor_tensor(out=score[:, cs], in0=RL[:], scalar=iw[:, j, h:h + 1], in1=score[:, cs], op0=ALU.mult, op1=ALU.add), reads=[RL, iw, score], writes=[score])
            if ch == NCH - 1:
                P.op("dve", lambda e, cs=cs: e.tensor_tensor(out=score[:, cs], in0=score[:, cs], in1=cb[:], op=ALU.add), reads=[score, cb], writes=[score])
        S = NCH * 512
        P.op("dve", lambda e: e.memset(cntall[:], 0.0), writes=[cntall])
        step = -LO0
        P.op("dve", lambda e: e.memset(mid[:], 0.0), writes=[mid])
        for it in range(NBIS):
            st = step
            P.op("dve", lambda e, S=S, it=it: e.tensor_scalar(out=junk[:, 0:S], in0=score[:, 0:S], scalar1=mid[:, 0:1], scalar2=0.0, op0=ALU.is_ge, op1=ALU.add, accum_out=cntall[:, it:it + 1]), reads=[score, mid, cntall], writes=[junk, cntall])
            P.op("dve", lambda e, it=it: e.tensor_scalar(out=tmp[:], in0=cntall[:, it:it + 1], scalar1=TOPK, scalar2=0.5, op0=ALU.is_ge, op1=ALU.subtract), reads=[cntall], writes=[tmp])
            P.op("dve", lambda e, st=st: e.scalar_tensor_tensor(out=mid[:], in0=tmp[:], scalar=st, in1=mid[:], op0=ALU.mult, op1=ALU.add), reads=[tmp, mid], writes=[mid])
            step *= 0.5
        fs = step
        P.op("dve", lambda e, fs=fs: e.tensor_scalar(out=lo[:], in0=mid[:], scalar1=-fs, scalar2=None, op0=ALU.add), reads=[mid], writes=[lo])
        for ch in range(NCH):
            cs = slice(ch * 512, (ch + 1) * 512)
            MK = mk_[ch % 2]
            P.op("dve", lambda e, MK=MK, cs=cs: e.tensor_scalar(out=MK[:], in0=score[:, cs], scalar1=lo[:, 0:1], scalar2=None, op0=ALU.is_ge), reads=[score, lo], writes=[MK])
            for t in range(4):
                P.op("pe", lambda e, MK=MK, t=t: e.transpose(out=ps_tr[:, t, :], in_=MK[:, t * 128:(t + 1) * 128], identity=ident[:]), reads=[MK, ident], writes=[ps_tr])
            P.op("act", lambda e, ch=ch: e.activation(out=maskT[:, ch * 4:(ch + 1) * 4, :], in_=ps_tr[:], func=AF.Copy), reads=[ps_tr], writes=[maskT])
        for g in range(4):
            nst = NCH * 4
            tiles = [(ch, t) for ch in range(NCH) for t in range(4)]
            vts = {}

            def issue_qk(idx, g=g, QT=QT):
                nonlocal n_v
                ch, t = tiles[idx]; st_ = ch * 4 + t
                if t == 0:
                    VT = vt[n_v % 3]; n_v += 1
                    P.dma("sp", VT[:], vv[(ch // 8) * 4096:(ch // 8 + 1) * 4096, :].rearrange("(r n) c -> n r c", r=4)[(ch % 8) * 128:(ch % 8 + 1) * 128, :, g * 128:(g + 1) * 128], writes=[VT])
                    vts[ch] = VT
                pl = ps_pl[idx % 2]
                P.op("pe", lambda e, pl=pl, st_=st_: e.matmul(pl[:], lhsT=kt[:, g, st_ * 128:(st_ + 1) * 128], rhs=QT[:, g * 4:(g + 1) * 4, :], start=True, stop=True), reads=[kt, QT], writes=[pl])
                return pl
            nxt = issue_qk(0)
            for idx in range(len(tiles)):
                pl = nxt
                if idx + 1 < len(tiles):
                    nxt = issue_qk(idx + 1)
                ch, t = tiles[idx]; st_ = ch * 4 + t; VT = vts[ch]
                PE_ = pe_[n_pl % 3]; PM = pmk[n_pl % 3]; n_pl += 1
                P.op("act", lambda e, pl=pl, PE_=PE_: e.activation(out=PE_[:], in_=pl[:], func=AF.Exp, scale=SCALE), reads=[pl], writes=[PE_])
                P.op("pool", lambda e, PE_=PE_, PM=PM, st_=st_: e.tensor_tensor(out=PM[:].rearrange("p (h q) -> p h q", h=4), in0=PE_[:].rearrange("p (h q) -> p h q", h=4), in1=maskT[:, st_:st_ + 1, :].to_broadcast([128, 4, 128]), op=ALU.mult), reads=[PE_, maskT], writes=[PM])
                P.op("pe", lambda e, VT=VT, t=t, PM=PM, st_=st_, nst=nst: e.matmul(ps_o[:], lhsT=VT[:, t, :], rhs=PM[:], start=(st_ == 0), stop=(st_ == nst - 1)), reads=[VT, PM], writes=[ps_o])
                P.op("pe", lambda e, PM=PM, st_=st_, nst=nst: e.matmul(ps_s[:], lhsT=ones_b[:], rhs=PM[:], start=(st_ == 0), stop=(st_ == nst - 1)), reads=[ones_b, PM], writes=[ps_s])
            OB = ob[g % 2]
            P.op("dve", lambda e: e.reciprocal(out=rs[:], in_=ps_s[:]), reads=[ps_s], writes=[rs])
            P.op("dve", lambda e, OB=OB: e.tensor_tensor(out=OB[:], in0=ps_o[:], in1=rs[:], op=ALU.mult), reads=[ps_o, rs], writes=[OB])
            P.dma("act", oT[g * 512:(g + 1) * 512, j * 128:(j + 1) * 128].rearrange("(h d) q -> d h q", d=128), OB[:].rearrange("p (h q) -> p h q", h=4), reads=[OB])
    return


def host_B(Aout, nqt=NQT):
    maps = []
    ident = np.eye(128, dtype=np.float32).astype(BF)
    for core in range(8):
        b = core // 4; c = core % 4
        cores_b = [b * 4 + i for i in range(4)]
        qT_full = np.concatenate([Aout[i]["qT"] for i in cores_b], axis=1)
        kT_full = np.concatenate([Aout[i]["kT"] for i in cores_b], axis=1)
        v_full = np.concatenate([Aout[i]["v"] for i in cores_b], axis=0)
        ik_full = np.concatenate([Aout[i]["ikT"] for i in cores_b], axis=1)
        iq_full = np.concatenate([Aout[i]["iqT"] for i in cores_b], axis=1)
        iw_full = np.concatenate([Aout[i]["iw"] for i in cores_b], axis=0)
        sel = np.concatenate([np.arange((4 * j + c) * 128, (4 * j + c + 1) * 128) for j in range(16)])
        r = np.arange(128)[:, None]; t = np.arange(512)[None, :]
        cbias = np.where(t <= c * 128 + r, 0.0, -1e5).astype(np.float32)
        maps.append({"qTc": np.ascontiguousarray(qT_full[:, sel]), "kT": kT_full, "v": v_full, "ikT": ik_full,
                     "iqTc": np.ascontiguousarray(iq_full[:, sel]), "iwc": np.ascontiguousarray(iw_full[sel]),
                     "cbias": cbias, "ident": ident})
    return maps


def emit_O(P, io, K):
    KC = K // 128
    aT = io("aT", [K, 2048], BF16, "ExternalInput")
    resT = io("resT", [2048, 2048], F32, "ExternalInput")
    w = io("w", [K, 2048], F32, "ExternalInput")
    hT = io("hT", [2048, 2048], F32, "ExternalOutput")
    a = P.sb([128, KC, 2048], BF16)
    av = aT.rearrange("(c p) t -> p c t", p=128)
    for c0 in range(0, KC, 8):
        P.dma("sp", a[:, c0:c0 + 8, :], av[:, c0:c0 + 8, :], writes=[a])
    wb = [P.sb([128, KC, 256], BF16) for _ in range(2)]
    rs = [P.sb([128, 512], F32) for _ in range(3)]
    ob = [P.sb([128, 512], F32) for _ in range(3)]
    ps = [P.ps([128, 512]) for _ in range(3)]
    wv = w.rearrange("(c p) n -> p c n", p=128)
    n = 0
    for nc2 in range(8):
        WB = wb[nc2 % 2]
        P.dma("pool", WB[:], wv[:, :, nc2 * 256:(nc2 + 1) * 256], writes=[WB])
        for sub in range(2):
            r0 = nc2 * 256 + sub * 128
            for tt in range(4):
                ts = slice(tt * 512, (tt + 1) * 512)
                p_ = ps[n % 3]; R = rs[n % 3]; O = ob[n % 3]; n += 1
                P.dma("act", R[:], resT[r0:r0 + 128, ts], writes=[R])
                for c in range(KC):
                    P.op("pe", lambda e, c=c, p_=p_, WB=WB, sub=sub, ts=ts: e.matmul(p_[:], lhsT=WB[:, c, sub * 128:(sub + 1) * 128], rhs=a[:, c, ts], start=(c == 0), stop=(c == KC - 1)), reads=[WB, a], writes=[p_])
                P.op("dve", lambda e, p_=p_, R=R, O=O: e.tensor_tensor(out=O[:], in0=p_[:], in1=R[:], op=ALU.add), reads=[p_, R], writes=[O])
                P.dma("sp", hT[r0:r0 + 128, ts], O[:], reads=[O])
    return


def assemble_o(Bout):
    o_full = np.zeros((2, 2048, 8192), dtype=BF)
    for core in range(8):
        b = core // 4; c = core % 4
        for j in range(16):
            o_full[b][:, (4 * j + c) * 128:(4 * j + c + 1) * 128] = Bout[core][:, j * 128:(j + 1) * 128]
    return o_full


EPS = 1e-6; D = 2048; KC = 16; TG = 256; NE = 16384; EC = 256

def emit_P(P, io, want_hn, add=False, ngroups=8, nec=NE // EC, dbg=False):
    NE = nec * EC if dbg else 16384
    DBG = {}
    def dump(name, t, ap, shape):
        if not dbg or name in DBG: return
        DBG[name] = io('dbg_' + name, shape, F32, 'ExternalOutput')
        P.dma('sp', DBG[name], ap, reads=[t])
    hT = io("hT", [D, 2048], F32, "ExternalInput")
    gn = io("gn", [128, 3, KC], F32, "ExternalInput")
    w_q = io("w_q", [D, D], F32, "ExternalInput")
    keysT = io("keysT", [128, 16, 128], F32, "ExternalInput")
    uT = io("uT", [D, NE], F32, "ExternalInput")
    vt = io("vt", [NE, D], F32, "ExternalInput")
    wg = io("wg", [D, D], F32, "ExternalInput")
    wpi = io("wpi", [256, D], F32, "ExternalInput")
    pT = io("pT", [256, 2048], F32, "ExternalInput")
    identd = io("ident", [128, 128], BF16, "ExternalInput")
    h3T = io("h3T", [D, 2048], F32, "ExternalOutput")
    mixT = io("mixT", [D, 2048], F32, "ExternalInput") if add else None
    hnT = io("hnT", [D, 2048], BF16, "ExternalOutput") if want_hn else None

    G = P.sb([128, 3, KC], F32); KT = P.sb([128, 16, 128], F32); ident = P.sb([128, 128], BF16)
    WPI = P.sb([128, 2, D], BF16); ones_b = P.sb([128, 128], BF16); epsb = P.sb([128, 1], F32)
    P.dma("sp", G[:], gn[:, :, :], writes=[G]); P.dma("sp", KT[:], keysT[:, :, :], writes=[KT]); P.dma("sp", ident[:], identd[:, :], writes=[ident])
    P.dma("pool", WPI[:], wpi.rearrange("(c p) n -> p c n", p=128), writes=[WPI])
    P.op("dve", lambda e: e.memset(ones_b[:], 1.0), writes=[ones_b])
    P.op("dve", lambda e: e.memset(epsb[:], EPS), writes=[epsb])

    H = P.sb([128, KC, TG], F32); XY = P.sb([128, KC, TG], F32); QP = P.sb([128, 16, TG], F32); NB = P.sb([128, KC, TG], BF16)
    rstd = P.sb([128, TG], F32)
    S = [P.sb([128, 16, 128], F32) for _ in range(2)]
    Cc = [P.sb([128, 8, 128], F32) for _ in range(2)]
    W1 = [P.sb([128, 8, 128], F32) for _ in range(2)]
    E2 = [P.sb([128, 8, 128], F32) for _ in range(2)]
    t16 = P.sb([128, 16, 16], F32); wk = P.sb([128, 256], F32); cand = P.sb([128, 8, 256], F32); f16 = P.sb([128, 8, 16], F32)
    thr = P.sb([128, 8], F32); fmax = P.sb([128, 8], F32); ex = P.sb([128, 8, 16], F32); Z = P.sb([128, 8], F32); rZ = P.sb([128, 8], F32)
    ub = [P.sb([128, KC, EC], BF16) for _ in range(2)]
    vb = [P.sb([128, EC // 128, D], BF16) for _ in range(2)]
    gel = [P.sb([128, EC], F32) for _ in range(2)]
    NI = EC // 128
    AA = [P.sb([128, 8, NI, 128], F32) for _ in range(2)]
    Gb = [[P.sb([128, NI, 128], F32) for _ in range(2)] for _ in range(2)]
    identf = P.sb([128, 128], F32)
    P.op("dve", lambda e: e.tensor_copy(out=identf[:], in_=ident[:]), reads=[ident], writes=[identf])
    coef = [P.sb([128, EC], BF16) for _ in range(2)]
    coefT = [P.sb([128, NI, TG], BF16) for _ in range(2)]
    wq = P.sb([128, KC, 128], F32)
    wgb = [P.sb([128, KC, 128], BF16) for _ in range(2)]
    PT = P.sb([128, 2, TG], BF16)
    gate = [P.sb([128, TG], F32) for _ in range(2)]; gtmp = [P.sb([128, TG], F32) for _ in range(2)]
    HN = NB if want_hn else None
    ps_ms = P.ps([128, 512]); ps_a = P.ps([128, 512]); ps_b = P.ps([128, 512]); ps_tr = P.ps([128, 2, NI, 128], BF16)
    ps4 = [P.ps([128, 512]) for _ in range(4)]
    hv = hT.rearrange("(c p) t -> p c t", p=128)
    h3v = h3T.rearrange("(c p) t -> p c t", p=128)
    hnv = hnT.rearrange("(c p) t -> p c t", p=128) if want_hn else None
    wqv = w_q.rearrange("(c p) n -> p c n", p=128)
    wgv = wg.rearrange("(c p) n -> p c n", p=128)
    uv = uT.rearrange("(c p) e -> p c e", p=128)
    ptv = pT.rearrange("(c p) t -> p c t", p=128)

    def rmsnorm(src, which, out32, outb):
        P.op("act", lambda e: e.activation(out=outb[:], in_=src[:], func=AF.Square), reads=[src], writes=[outb])
        for c in range(KC):
            P.op("pe", lambda e, c=c: e.matmul(ps_ms[:, 0:TG], lhsT=ones_b[:], rhs=outb[:, c, :], start=(c == 0), stop=(c == KC - 1)), reads=[ones_b, outb], writes=[ps_ms])
        P.op("act", lambda e: e.activation(out=rstd[:], in_=ps_ms[:, 0:TG], func=AF.Sqrt, bias=epsb[:, 0:1], scale=1.0 / D), reads=[ps_ms, epsb], writes=[rstd])
        P.op("dve", lambda e: e.reciprocal(out=rstd[:], in_=rstd[:]), reads=[rstd], writes=[rstd])
        for c in range(KC):
            if out32 is not None:
                P.op("dve", lambda e, c=c: e.scalar_tensor_tensor(out=out32[:, c, :], in0=src[:, c, :], scalar=G[:, which, c:c + 1], in1=rstd[:], op0=ALU.mult, op1=ALU.mult), reads=[src, G, rstd], writes=[out32])
            else:
                P.op("dve", lambda e, c=c: e.scalar_tensor_tensor(out=outb[:, c, :], in0=src[:, c, :], scalar=G[:, which, c:c + 1], in1=rstd[:], op0=ALU.mult, op1=ALU.mult), reads=[src, G, rstd], writes=[outb])
        if out32 is not None:
            P.op("act", lambda e: e.activation(out=outb[:], in_=out32[:], func=AF.Copy), reads=[out32], writes=[outb])

    n_w = 0
    for gi in range(ngroups):
        gs = slice(gi * TG, (gi + 1) * TG)
        P.dma("sp", H[:], hv[:, :, gs], writes=[H])
        P.dma("pool", PT[:], ptv[:, :, gs], writes=[PT])
        if add:
            P.dma("act", QP[:], mixT.rearrange("(c p) t -> p c t", p=128)[:, :, gs], writes=[QP])
            P.op("dve", lambda e: e.tensor_tensor(out=H[:], in0=H[:], in1=QP[:], op=ALU.add), reads=[H, QP], writes=[H])
        rmsnorm(H, 0, XY, NB)
        for n in range(16):
            P.dma("sp", wq[:], wqv[:, :, n * 128:(n + 1) * 128], writes=[wq])
            pq = ps_a if n % 2 == 0 else ps_b
            for c in range(KC):
                P.op("pe", lambda e, c=c, pq=pq: e.matmul(pq[:, 0:TG], lhsT=wq[:, c, :], rhs=XY[:, c, :], start=(c == 0), stop=(c == KC - 1)), reads=[wq, XY], writes=[pq])
            P.op("act", lambda e, n=n, pq=pq: e.activation(out=QP[:, n, :], in_=pq[:, 0:TG], func=AF.Copy), reads=[pq], writes=[QP])
        for tl in range(2):
            tsl = slice(tl * 128, (tl + 1) * 128)
            St = S[tl]; Ct = Cc[tl]; W1t = W1[tl]; E2t = E2[tl]
            for hp in range(16):
                pb = ps4[hp // 4]
                P.op("pe", lambda e, hp=hp, pb=pb, tsl=tsl: e.matmul(pb[:, (hp % 4) * 128:(hp % 4 + 1) * 128], lhsT=QP[:, hp, tsl], rhs=KT[:, hp, :], start=True, stop=True), reads=[QP, KT], writes=[pb])
            for q4 in range(4):
                P.op("act", lambda e, q4=q4, St=St: e.activation(out=St[:, q4 * 4:(q4 + 1) * 4, :], in_=ps4[q4][:].rearrange("p (a b) -> p a b", a=4), func=AF.Copy), reads=[ps4[q4]], writes=[St])
            for hp in range(16):
                P.op("dve", lambda e, hp=hp, St=St: e.max(out=t16[:, hp, 0:8], in_=St[:, hp, :]), reads=[St], writes=[t16])
                P.op("dve", lambda e, hp=hp, St=St: e.match_replace(out=wk[:, 0:128], in_to_replace=t16[:, hp, 0:8], in_values=St[:, hp, :], imm_value=-1e30), reads=[St, t16], writes=[wk])
                P.op("dve", lambda e, hp=hp: e.max(out=t16[:, hp, 8:16], in_=wk[:, 0:128]), reads=[wk], writes=[t16])
            for h in range(8):
                P.op("dve", lambda e, h=h: e.tensor_tensor(out=cand[:, h, :].rearrange("p (a b) -> p a b", a=16), in0=t16[:, 2 * h, :].unsqueeze(2).to_broadcast([128, 16, 16]), in1=t16[:, 2 * h + 1, :].unsqueeze(1).to_broadcast([128, 16, 16]), op=ALU.add), reads=[t16], writes=[cand])
            for h in range(8):
                P.op("dve", lambda e, h=h: e.max(out=f16[:, h, 0:8], in_=cand[:, h, :]), reads=[cand], writes=[f16])
                P.op("dve", lambda e, h=h: e.match_replace(out=wk[:], in_to_replace=f16[:, h, 0:8], in_values=cand[:, h, :], imm_value=-1e30), reads=[cand, f16], writes=[wk])
                P.op("dve", lambda e, h=h: e.max(out=f16[:, h, 8:16], in_=wk[:]), reads=[wk], writes=[f16])
            P.op("dve", lambda e: e.tensor_reduce(out=thr[:], in_=f16[:], axis=AX.X, op=ALU.min), reads=[f16], writes=[thr])
            P.op("dve", lambda e: e.tensor_scalar(out=thr[:], in0=thr[:], scalar1=-2e-5, scalar2=None, op0=ALU.add), reads=[thr], writes=[thr])
            t16v = t16[:].rearrange("p (h two) k -> p h two k", two=2)
            P.op("dve", lambda e, t16v=t16v: e.tensor_tensor(out=fmax[:], in0=t16v[:, :, 0, 0], in1=t16v[:, :, 1, 0], op=ALU.add), reads=[t16], writes=[fmax])
            P.op("dve", lambda e: e.tensor_tensor(out=ex[:], in0=f16[:], in1=fmax[:].unsqueeze(2).to_broadcast([128, 8, 16]), op=ALU.subtract), reads=[f16, fmax], writes=[ex])
            P.op("act", lambda e: e.activation(out=ex[:], in_=ex[:], func=AF.Exp), reads=[ex], writes=[ex])
            P.op("dve", lambda e: e.tensor_reduce(out=Z[:], in_=ex[:], axis=AX.X, op=ALU.add), reads=[ex], writes=[Z])
            P.op("dve", lambda e: e.reciprocal(out=rZ[:], in_=Z[:]), reads=[Z], writes=[rZ])
            S4 = St[:].rearrange("p (h two) k -> p h two k", two=2)
            P.op("dve", lambda e, S4=S4, Ct=Ct: e.tensor_tensor(out=Ct[:], in0=thr[:].unsqueeze(2).to_broadcast([128, 8, 128]), in1=S4[:, :, 0, :], op=ALU.subtract), reads=[thr, St], writes=[Ct])
            P.op("dve", lambda e, S4=S4, W1t=W1t, t16v=t16v: e.tensor_tensor(out=W1t[:], in0=S4[:, :, 0, :], in1=t16v[:, :, 0, 0:1].to_broadcast([128, 8, 128]), op=ALU.subtract), reads=[St, t16], writes=[W1t])
            P.op("act", lambda e, W1t=W1t: e.activation(out=W1t[:], in_=W1t[:], func=AF.Exp), reads=[W1t], writes=[W1t])
            P.op("dve", lambda e, W1t=W1t: e.tensor_tensor(out=W1t[:], in0=W1t[:], in1=rZ[:].unsqueeze(2).to_broadcast([128, 8, 128]), op=ALU.mult), reads=[W1t, rZ], writes=[W1t])
            P.op("dve", lambda e, S4=S4, E2t=E2t, t16v=t16v: e.tensor_tensor(out=E2t[:], in0=S4[:, :, 1, :], in1=t16v[:, :, 1, 0:1].to_broadcast([128, 8, 128]), op=ALU.subtract), reads=[St, t16], writes=[E2t])
            P.op("act", lambda e, E2t=E2t: e.activation(out=E2t[:], in_=E2t[:], func=AF.Exp), reads=[E2t], writes=[E2t])
            if tl == 0 and gi == 0:
                dump('S', St, St[:], [128, 16, 128]); dump('t16', t16, t16[:], [128, 16, 16]); dump('f16', f16, f16[:], [128, 8, 16]); dump('thr', thr, thr[:], [128, 8]); dump('fmax', fmax, fmax[:], [128, 8]); dump('rZ', rZ, rZ[:], [128, 8])
                dump('C', Ct, Ct[:], [128, 8, 128]); dump('W1', W1t, W1t[:], [128, 8, 128]); dump('E2', E2t, E2t[:], [128, 8, 128]); dump('cand', cand, cand[:], [128, 8, 256])
        def g1(ec):
            i0 = ec * NI
            for tl in range(2):
                St = S[tl]; Ct = Cc[tl]
                S4 = St[:].rearrange("p (h two) k -> p h two k", two=2)
                AAt = AA[tl]
                P.op("dve", lambda e, AAt=AAt, S4=S4, Ct=Ct, i0=i0: e.tensor_tensor(out=AAt[:], in0=S4[:, :, 1, :].unsqueeze(2).to_broadcast([128, 8, NI, 128]), in1=Ct[:, :, i0:i0 + NI].unsqueeze(3).to_broadcast([128, 8, NI, 128]), op=ALU.is_ge), reads=[St, Ct], writes=[AAt])
            for tl in range(2):
                E2t = E2[tl]; AAt = AA[tl]
                P.op("pool", lambda e, AAt=AAt, E2t=E2t: e.tensor_tensor(out=AAt[:], in0=AAt[:], in1=E2t[:].unsqueeze(2).to_broadcast([128, 8, NI, 128]), op=ALU.mult), reads=[AAt, E2t], writes=[AAt])

        def g1b(ec):
            i0 = ec * NI
            for tl in range(2):
                W1t = W1[tl]; AAt = AA[tl]
                for h in range(8):
                    for ii in range(NI):
                        P.op("act", lambda e, AAt=AAt, W1t=W1t, h=h, ii=ii, i0=i0: e.activation(out=AAt[:, h, ii, :], in_=AAt[:, h, ii, :], func=AF.Copy, scale=W1t[:, h, i0 + ii:i0 + ii + 1]), reads=[AAt, W1t], writes=[AAt])

        def g2(par):
            for tl in range(2):
                AAt = AA[tl]; GB = Gb[par][tl]
                P.op("dve", lambda e, AAt=AAt, GB=GB: e.tensor_reduce(out=GB[:], in_=AAt[:].rearrange("p h i j -> p i j h"), axis=AX.X, op=ALU.add), reads=[AAt], writes=[GB])
        g1(0); g1b(0); g2(0)
        Yv = XY[:].rearrange("p c t -> p (c t)").rearrange("p (tl d) -> p tl d", tl=2)

        def wload(ec):
            P.dma("pool", ub[ec % 2][:], uv[:, :, ec * EC:(ec + 1) * EC], writes=[ub[ec % 2]])
            P.dma("pool", vb[ec % 2][:], vt[ec * EC:(ec + 1) * EC, :].rearrange("(b p) d -> p b d", p=128), writes=[vb[ec % 2]])
        wload(0)
        for ec in range(nec):
            par = ec % 2
            UB = ub[par]; VB = vb[par]; CT = coefT[par]
            if ec + 1 < nec:
                wload(ec + 1)
                g1(ec + 1)
            for tl in range(2):
                tsl = slice(tl * 128, (tl + 1) * 128)
                pa = ps4[tl]
                for c in range(KC):
                    P.op("pe", lambda e, c=c, pa=pa, UB=UB, tsl=tsl: e.matmul(pa[:, 0:EC], lhsT=NB[:, c, tsl], rhs=UB[:, c, :], start=(c == 0), stop=(c == KC - 1)), reads=[NB, UB], writes=[pa])
            for tl in range(2):
                pa = ps4[tl]; GL = gel[tl]
                P.op("act", lambda e, pa=pa, GL=GL: e.activation(out=GL[:], in_=pa[:, 0:EC], func=AF.Gelu), reads=[pa], writes=[GL])
            for tl in range(2):
                GL = gel[tl]; GB = Gb[par][tl]; CF = coef[tl]
                P.op("dve", lambda e, GL=GL, GB=GB, CF=CF: e.tensor_tensor(out=CF[:], in0=GL[:], in1=GB[:].rearrange("p a b -> p (a b)"), op=ALU.mult), reads=[GL, GB], writes=[CF])
                if tl == 0 and gi == 0 and ec == 0:
                    dump('GL', GL, GL[:], [128, EC]); dump('GB', GB, GB[:], [128, NI, 128])
            for tl in range(2):
                CF = coef[tl]
                for b in range(NI):
                    P.op("pe", lambda e, b=b, CF=CF, tl=tl: e.transpose(out=ps_tr[:, tl, b, :], in_=CF[:, b * 128:(b + 1) * 128], identity=ident[:]), reads=[CF, ident], writes=[ps_tr])
            P.op("act", lambda e, CT=CT: e.activation(out=CT[:].rearrange("p b (tl t) -> p tl b t", tl=2), in_=ps_tr[:], func=AF.Copy), reads=[ps_tr], writes=[CT])
            if ec + 1 < nec:
                g1b(ec + 1)
            k_ = 0
            for tl in range(2):
                tsl = slice(tl * 128, (tl + 1) * 128)
                for d4 in range(4):
                    py = ps4[2 + k_ % 2]; k_ += 1
                    for b in range(NI):
                        P.op("pe", lambda e, b=b, d4=d4, py=py, VB=VB, CT=CT, tsl=tsl: e.matmul(py[:], lhsT=CT[:, b, tsl], rhs=VB[:, b, d4 * 512:(d4 + 1) * 512], start=(b == 0), stop=(b == NI - 1)), reads=[VB, CT], writes=[py])
                    ysl = Yv[:, tl, d4 * 512:(d4 + 1) * 512]
                    if ec == 0:
                        P.op("act", lambda e, py=py, ysl=ysl: e.activation(out=ysl, in_=py[:], func=AF.Copy), reads=[py], writes=[XY])
                    else:
                        P.op("dve", lambda e, py=py, ysl=ysl: e.tensor_tensor(out=ysl, in0=ysl, in1=py[:], op=ALU.add), reads=[py, XY], writes=[XY])
            if ec + 1 < nec:
                g2(1 - par)
        if gi == 0:
            dump('Y', XY, XY[:], [128, KC, TG])
        k_ = 0
        for tl in range(2):
            tsl = slice(tl * 128, (tl + 1) * 128)
            for dc4 in range(4):
                py = ps4[k_ % 4]; k_ += 1
                for q4 in range(4):
                    dc = dc4 * 4 + q4
                    P.op("pe", lambda e, py=py, q4=q4, dc=dc, tl=tl: e.transpose(out=py[:, q4 * 128:(q4 + 1) * 128], in_=Yv[:, tl, dc * 128:(dc + 1) * 128], identity=identf[:]), reads=[XY, identf], writes=[py])
                P.op("dve", lambda e, py=py, dc4=dc4, tsl=tsl: e.tensor_tensor(out=H[:, dc4 * 4:(dc4 + 1) * 4, tsl], in0=H[:, dc4 * 4:(dc4 + 1) * 4, tsl], in1=py[:].rearrange("p (a b) -> p a b", a=4), op=ALU.add), reads=[py, H], writes=[H])
        rmsnorm(H, 1, None, NB)
        for n in range(16):
            WG = wgb[n_w % 2]; GA = gate[n_w % 2]; GT = gtmp[n_w % 2]; n_w += 1
            P.dma("pool", WG[:], wgv[:, :, n * 128:(n + 1) * 128], writes=[WG])
            pg = ps_a if n % 2 == 0 else ps_b
            for c in range(KC):
                P.op("pe", lambda e, c=c, pg=pg, WG=WG: e.matmul(pg[:, 0:TG], lhsT=WG[:, c, :], rhs=NB[:, c, :], start=(c == 0), stop=(c == KC - 1)), reads=[WG, NB], writes=[pg])
            for c in range(2):
                P.op("pe", lambda e, c=c, pg=pg, n=n: e.matmul(pg[:, TG:2 * TG], lhsT=WPI[:, c, n * 128:(n + 1) * 128], rhs=PT[:, c, :], start=(c == 0), stop=(c == 1)), reads=[WPI, PT], writes=[pg])
            P.op("act", lambda e, pg=pg, GA=GA: e.activation(out=GA[:], in_=pg[:, 0:TG], func=AF.Sigmoid), reads=[pg], writes=[GA])
            P.op("dve", lambda e, pg=pg, GA=GA, GT=GT: e.tensor_tensor(out=GT[:], in0=GA[:], in1=pg[:, TG:2 * TG], op=ALU.mult), reads=[pg, GA], writes=[GT])
            P.op("pool", lambda e, n=n, GT=GT: e.tensor_tensor(out=H[:, n, :], in0=H[:, n, :], in1=GT[:], op=ALU.add), reads=[H, GT], writes=[H])
        P.dma("sp", h3v[:, :, gs], H[:], reads=[H])
        if want_hn:
            rmsnorm(H, 2, None, HN)
            P.dma("sp", hnv[:, :, gs], HN[:], reads=[HN])
    return


def host_P(hT_list, layer, z, want_next):
    maps = []
    ident = np.eye(128, dtype=np.float32).astype(BF)
    gnext = z['norm_mix'][layer + 1] if want_next else np.ones(D, np.float32)
    gn = np.ascontiguousarray(np.stack([z['norm_ffn'][layer].reshape(KC, 128).T, z['norm_ple'][layer].reshape(KC, 128).T, gnext.reshape(KC, 128).T], axis=1))
    keysT = np.ascontiguousarray(z['peer_keys'][layer].reshape(16, 128, 128).transpose(2, 0, 1))
    uT = np.ascontiguousarray(z['peer_u'][layer].T)
    for core in range(8):
        b = core // 4; t0 = (core % 4) * 2048
        maps.append({"hT": hT_list[core], "gn": gn, "w_q": z['peer_w_q'][layer], "keysT": keysT, "uT": uT, "vt": z['peer_v'][layer],
                     "wg": z['ple_w_gate'][layer], "wpi": z['ple_w_in'][layer], "pT": np.ascontiguousarray(z['p'][layer, b, t0:t0 + 2048].T), "ident": ident})
    return maps


EPS = 1e-6; KC = 16; ST = 256; NW = 3088

def emit_D(P, io, nst=32, maxphase=9):
    hnT = io("hnT", [8192, 2048], BF16, "ExternalInput")
    wc = io("wc", [2048, NW], F32, "ExternalInput")
    convw = io("convw", [128, 16, 4], F32, "ExternalInput")
    hc = io("hc", [128, 2, 8], F32, "ExternalInput")
    gnrow = io("gnrow", [128, 128], F32, "ExternalInput")
    masks = io("masks", [128, 6, 128], F32, "ExternalInput")
    og = io("og", [1024, 8192], BF16, "ExternalOutput")

    W = P.sb([128, KC, NW], BF16)
    wv = wc.rearrange("(c p) n -> p c n", p=128)
    for c0 in range(0, KC, 4):
        for n0 in range(0, NW, 512):
            n1 = min(NW, n0 + 512)
            P.dma("pool", W[:, c0:c0 + 4, n0:n1], wv[:, c0:c0 + 4, n0:n1], writes=[W])
    CW = P.sb([128, 16, 4], F32); HC = P.sb([128, 2, 8], F32); GN = P.sb([128, 128], F32); MK = P.sb([128, 6, 128], F32)
    P.dma("sp", CW[:], convw[:, :, :], writes=[CW]); P.dma("sp", HC[:], hc[:, :, :], writes=[HC]); P.dma("sp", GN[:], gnrow[:, :], writes=[GN]); P.dma("sp", MK[:], masks[:, :, :], writes=[MK])
    ident = MK[:, 0, :]; Lm = MK[:, 1, :]; Bo = MK[:, 2, :]; Um = MK[:, 3, :]; NUs = MK[:, 4, :]; cmask = MK[:, 5, 0:2]
    ones_f = P.sb([128, 128], F32); epsb = P.sb([128, 1], F32); nega = P.sb([128, 8], F32); one1 = P.sb([128, 1], F32)
    P.op("dve", lambda e: e.memset(ones_f[:], 1.0), writes=[ones_f])
    P.op("dve", lambda e: e.memset(epsb[:], EPS), writes=[epsb])
    P.op("dve", lambda e: e.memset(one1[:], 1.0), writes=[one1])
    P.op("act", lambda e: e.activation(out=nega[:], in_=HC[:, 0, :], func=AF.Exp), reads=[HC], writes=[nega])
    P.op("dve", lambda e: e.tensor_scalar(out=nega[:], in0=nega[:], scalar1=-1.0, scalar2=None, op0=ALU.mult), reads=[nega], writes=[nega])

    HN = [P.sb([128, KC, ST], BF16) for _ in range(1)]
    PJ = [P.sb([128, ST + 3], F32) for _ in range(2)]
    HALO = P.sb([128, 16, 3], F32)
    CO = [P.sb([128, ST], F32) for _ in range(16)]
    P.op("pool", lambda e: e.memset(HALO[:], 0.0), writes=[HALO])
    sqt = [P.sb([128, ST], F32) for _ in range(1)]; rst = [P.sb([128, ST], F32) for _ in range(1)]
    ZS = [P.sb([128, 1024], F32) for _ in range(1)]
    BA = [P.sb([128, 16], F32) for _ in range(2)]; BETA = [P.sb([128, 8], F32) for _ in range(2)]; GG = [P.sb([128, 8], F32) for _ in range(2)]
    GC = P.sb([128, 8], F32); GL = P.sb([128, 8], F32); gsel = P.sb([128, 8, 2], F32); EGL = P.sb([128, 16], F32)
    EGC = P.sb([128, 8], F32); BG = P.sb([128, 8], F32); KD = P.sb([128, 8], F32)
    KTM = P.sb([128, 4, 128], F32); VTM = P.sb([128, 8, 128], F32)
    KK = [P.sb([128, 128], F32) for _ in range(4)]; QK = [P.sb([128, 128], F32) for _ in range(4)]
    def mk2(n=2): return [P.sb([128, 128], F32) for _ in range(n)]
    DGB = [P.sb([128, 256], F32) for _ in range(2)]
    DTt = mk2(); DT = mk2(); EGR = mk2(); BR = mk2(); T1 = mk2(); T2 = T1
    XA = mk2(4); XAT = mk2(4); XB = mk2(4); XBT = mk2(4); RR = mk2(4)
    VB = mk2(4); KBG = mk2(4)
    QKm = mk2(8); Usb = mk2(8); WT = mk2(8); QG = mk2(8); KG = mk2(8); VN = mk2(8)
    Sst = mk2(8)
    for h in range(8):
        P.op("pool", lambda e, h=h: e.memset(Sst[h][:], 0.0), writes=[Sst[h]])
    Osb = P.sb([128, 8, 128], F32);  MS = P.sb([128, 8], F32); O2 = P.sb([128, 8, 128], F32); SQo = O2
    OB = [P.sb([128, 1024], BF16) for _ in range(1)]
    OT = P.sb([128, 8, 128], BF16); identb = P.sb([128, 128], BF16)
    P.op("dve", lambda e: e.tensor_copy(out=identb[:], in_=ident), reads=[MK], writes=[identb])
    bankT = [P.ps([128, 512]) for _ in range(8)]
    pjps = [TV(bankT[0], bankT[0][:, 0:256], "pjA"), TV(bankT[1], bankT[1][:, 0:256], "pjB")]
    zps = bankT[2]
    smalls = [TV(bankT[3], bankT[3][:, i * 128:(i + 1) * 128], f"sm{i}") for i in range(4)]
    rowsps = [TV(bankT[4], bankT[4][:, 0:256], "rowsA"), TV(bankT[5], bankT[5][:, 0:256], "rowsB")]
    rotl = [TV(bankT[6 + i % 2], bankT[6 + i % 2][:, (i // 2) * 128:(i // 2 + 1) * 128], f"rot{i}") for i in range(8)]
    rc = [0]; sc = [0]
    def rot():
        t = rotl[rc[0] % 8]; rc[0] += 1; return t
    def sm():
        t = smalls[sc[0] % 4]; sc[0] += 1; return t

    def mm(o, oap, lhsT, rhs, reads, start=True, stop=True):
        P.op("pe", lambda e: e.matmul(oap, lhsT=lhsT, rhs=rhs, start=start, stop=stop), reads=reads, writes=[o])
    def tp(o, oap, in_, reads):
        P.op("pe", lambda e: e.transpose(out=oap, in_=in_, identity=ident), reads=reads + [MK], writes=[o])
    def cp(eng, o, oap, iap, reads):
        if eng == "act":
            P.op("act", lambda e: e.activation(out=oap, in_=iap, func=AF.Copy), reads=reads, writes=[o])
        else:
            P.op(eng, lambda e: e.tensor_copy(out=oap, in_=iap), reads=reads, writes=[o])
    def act(o, oap, iap, func, reads, bias=None, scale=None):
        kw = {}
        if bias is not None: kw["bias"] = bias
        if scale is not None: kw["scale"] = scale
        P.op("act", lambda e: e.activation(out=oap, in_=iap, func=func, **kw), reads=reads, writes=[o])
    def tt(eng, o, oap, a, b, op, reads):
        P.op(eng, lambda e: e.tensor_tensor(out=oap, in0=a, in1=b, op=op), reads=reads, writes=[o])
    def ts(eng, o, oap, a, s1, s2, op0, op1, reads):
        if op1 is None:
            P.op(eng, lambda e: e.tensor_scalar(out=oap, in0=a, scalar1=s1, scalar2=None, op0=op0), reads=reads, writes=[o])
        else:
            P.op(eng, lambda e: e.tensor_scalar(out=oap, in0=a, scalar1=s1, scalar2=s2, op0=op0, op1=op1), reads=reads, writes=[o])
    def stt(eng, o, oap, a, s, b, op0, op1, reads):
        P.op(eng, lambda e: e.scalar_tensor_tensor(out=oap, in0=a, scalar=s, in1=b, op0=op0, op1=op1), reads=reads, writes=[o])

    for st in range(nst):
        H = HN[0]
        for tl_ in range(2):
            tile_ = st * 2 + tl_; j_ = tile_ // 4; r_ = tile_ % 4
            for k_ in range(8):
                P.dma("sp", H[:, 2 * k_:2 * k_ + 2, tl_ * 128:(tl_ + 1) * 128], hnT[k_ * 1024 + r_ * 256:k_ * 1024 + (r_ + 1) * 256, j_ * 128:(j_ + 1) * 128].rearrange("(two p) t -> p two t", p=128), writes=[H])
        for ch in range(16):
            pj = pjps[ch % 2]; pjt = PJ[ch % 2]; co = CO[ch]
            for c in range(KC):
                mm(pj, pj[:], W[:, c, ch * 128:(ch + 1) * 128], H[:, c, :], [W, H], start=(c == 0), stop=(c == KC - 1))
            cp("pool", pjt, pjt[:, 0:3], HALO[:, ch, :], [HALO])
            cp("act", pjt, pjt[:, 3:ST + 3], pj[:], [pj])
            ts("dve", co, co[:], pjt[:, 3:ST + 3], CW[:, ch, 3:4], None, ALU.mult, None, [pjt, CW])
            for j in (2, 1, 0):
                stt("dve", co, co[:], pjt[:, j:j + ST], CW[:, ch, j:j + 1], co[:], ALU.mult, ALU.add, [pjt, CW, co])
            cp("pool", HALO, HALO[:, ch, :], pjt[:, ST:ST + 3], [pjt])
            act(co, co[:], co[:], AF.Silu, [co])
            if ch < 8:
                sq = sqt[0]; rs = rst[0]; ss = rowsps[ch % 2]
                act(sq, sq[:], co[:], AF.Square, [co])
                mm(ss, ss[:], ones_f[:], sq[:], [ones_f, sq])
                act(rs, rs[:], ss[:], AF.Sqrt, [ss, epsb], bias=epsb[:, 0:1], scale=1.0)
                P.op("dve", lambda e, rs=rs: e.reciprocal(out=rs[:], in_=rs[:]), reads=[rs], writes=[rs])
                if ch < 4:
                    stt("dve", co, co[:], co[:], 128 ** -0.5, rs[:], ALU.mult, ALU.mult, [co, rs])
                else:
                    tt("dve", co, co[:], co[:], rs[:], ALU.mult, [co, rs])
        if maxphase < 2: continue
        for tl in range(2):
            cs = slice(tl * 128, (tl + 1) * 128)
            q = sm()
            for c in range(KC):
                mm(q, q[:, 0:16], H[:, c, cs], W[:, c, 3072:3088], [W, H], start=(c == 0), stop=(c == KC - 1))
            cp("act", BA[tl], BA[tl][:], q[:, 0:16], [q])
            act(BETA[tl], BETA[tl][:], BA[tl][:, 0:8], AF.Sigmoid, [BA[tl]])
            tt("dve", GG[tl], GG[tl][:], BA[tl][:, 8:16], HC[:, 1, :], ALU.add, [BA[tl], HC])
            act(GG[tl], GG[tl][:], GG[tl][:], AF.Exp, [GG[tl]])
            act(GG[tl], GG[tl][:], GG[tl][:], AF.Ln, [GG[tl], one1], bias=one1[:, 0:1], scale=1.0)
            tt("dve", GG[tl], GG[tl][:], GG[tl][:], nega[:], ALU.mult, [GG[tl], nega])
        if maxphase < 3: continue
        for tl in range(2):
            cs = slice(tl * 128, (tl + 1) * 128)
            g = GG[tl]; beta = BETA[tl]
            q = sm(); mm(q, q[:, 0:8], Lm, g[:], [MK, g]); cp("act", GC, GC[:], q[:, 0:8], [q])
            q = sm(); mm(q, q[:, 0:8], Bo, g[:], [MK, g]); cp("act", GL, GL[:], q[:, 0:8], [q])
            tt("dve", gsel, gsel[:], g[:].unsqueeze(2).to_broadcast([128, 8, 2]), cmask.unsqueeze(1).to_broadcast([128, 8, 2]), ALU.mult, [g, MK])
            q = sm(); mm(q, q[:, 0:16], ones_f[:], gsel[:].rearrange("p a b -> p (a b)"), [ones_f, gsel]); act(EGL, EGL[:], q[:, 0:16], AF.Exp, [q])
            act(EGC, EGC[:], GC[:], AF.Exp, [GC])
            tt("dve", BG, BG[:], beta[:], EGC[:], ALU.mult, [beta, EGC])
            tt("dve", KD, KD[:], GL[:], GC[:], ALU.subtract, [GL, GC])
            act(KD, KD[:], KD[:], AF.Exp, [KD])
            if maxphase < 4: continue
            for hq in range(4):
                r = rot(); tp(r, r[:], CO[4 + hq][:, cs], [CO[4 + hq]]); cp("act", KTM, KTM[:, hq, :], r[:], [r])
            for hv_ in range(8):
                r = rot(); tp(r, r[:], CO[8 + hv_][:, cs], [CO[8 + hv_]]); cp("dve" if hv_ % 2 else "act", VTM, VTM[:, hv_, :], r[:], [r])
            for hq in range(4):
                r = rot(); mm(r, r[:], CO[4 + hq][:, cs], CO[4 + hq][:, cs], [CO[4 + hq]]); cp("act", KK[hq], KK[hq][:], r[:], [r])
                r = rot(); mm(r, r[:], CO[4 + hq][:, cs], CO[hq][:, cs], [CO[4 + hq], CO[hq]]); cp("dve", QK[hq], QK[hq][:], r[:], [r])
            if maxphase < 5: continue
            for hb in range(2):
                heads = list(range(4 * hb, 4 * hb + 4))
                for h in heads:
                    hq = h // 2; par = h % 2; b4 = h % 4
                    dgb = DGB[par]; rows = rowsps[par]
                    ts("dve", dgb, dgb[:, 0:128], ident, GC[:, h:h + 1], None, ALU.mult, None, [MK, GC])
                    ts("dve", dgb, dgb[:, 128:256], ident, beta[:, h:h + 1], None, ALU.mult, None, [MK, beta])
                    mm(rows, rows[:], ones_f[:], dgb[:], [ones_f, dgb])
                    ts("dve", DTt[par], DTt[par][:], rows[:, 0:128], GC[:, h:h + 1], 0.0, ALU.subtract, ALU.min, [rows, GC])
                    act(DTt[par], DTt[par][:], DTt[par][:], AF.Exp, [DTt[par]])
                    tt("pool", DT[par], DT[par][:], DTt[par][:], Um, ALU.mult, [DTt[par], MK])
                    act(EGR[par], EGR[par][:], rows[:, 0:128], AF.Exp, [rows])
                    cp("act", BR[par], BR[par][:], rows[:, 128:256], [rows])
                    tt("pool", T1[par], T1[par][:], KK[hq][:], BR[par][:], ALU.mult, [KK[hq], BR[par]])
                    tt("dve", T2[par], T2[par][:], T1[par][:], DT[par][:], ALU.mult, [T1[par], DT[par]])
                    x0 = XA[b4]; x0t = XAT[b4]; R = RR[b4]
                    tt("pool", x0, x0[:], T2[par][:], NUs, ALU.mult, [T2[par], MK])
                    tt("dve", QKm[h], QKm[h][:], QK[hq][:], DT[par][:], ALU.mult, [QK[hq], DT[par]])
                    r = rot(); tp(r, r[:], x0[:], [x0]); cp("act", x0t, x0t[:], r[:], [r])
                    tt("pool", R, R[:], x0[:], ident, ALU.add, [x0, MK])
                    ts("dve", VB[b4], VB[b4][:], VTM[:, h, :], beta[:, h:h + 1], None, ALU.mult, None, [VTM, beta])
                    ts("dve", KBG[b4], KBG[b4][:], KTM[:, hq, :], BG[:, h:h + 1], None, ALU.mult, None, [KTM, BG])
                    tt("pool", QG[h], QG[h][:], CO[hq][:, cs], EGR[par][:], ALU.mult, [CO[hq], EGR[par]])
                    ts("dve", KG[h], KG[h][:], KTM[:, hq, :], KD[:, h:h + 1], None, ALU.mult, None, [KTM, KD])
                for n in range(5):
                    last = n == 4
                    for h in heads:
                        b4 = h % 4
                        xc, xct = ((XA, XAT) if n % 2 == 0 else (XB, XBT))
                        xn, xnt = ((XB, XBT) if n % 2 == 0 else (XA, XAT))
                        xc = xc[b4]; xct = xct[b4]; xn = xn[b4]; xnt = xnt[b4]; R = RR[b4]
                        if not last:
                            r = rot(); mm(r, r[:], xct[:], xc[:], [xct, xc]); cp("act", xn, xn[:], r[:], [r])
                        r = rot(); mm(r, r[:], xc[:], xct[:], [xct, xc]); cp("dve", xnt, xnt[:], r[:], [r])
                    for h in heads:
                        b4 = h % 4
                        xnt = ((XBT) if n % 2 == 0 else (XAT))[b4]; R = RR[b4]
                        r = rot(); mm(r, r[:], xnt[:], R[:], [xnt, R]); tt("dve", R, R[:], R[:], r[:], ALU.add, [R, r])
                for h in heads:
                    b4 = h % 4; R = RR[b4]
                    r = rot(); mm(r, r[:], R[:], VB[b4][:], [R, VB[b4]]); cp("act", Usb[h], Usb[h][:], r[:], [r])
                    r = rot(); mm(r, r[:], KBG[b4][:], R[:], [R, KBG[b4]]); cp("act", WT[h], WT[h][:], r[:], [r])
            if maxphase < 6: continue
            for i in range(2):
                rr = slice(64 * i, 64 * i + 64)
                p1s = []
                for h in range(8):
                    r = rot(); mm(r, r[:], WT[h][:], Sst[h][:], [WT[h], Sst[h]]); p1s.append(r)
                for h in range(8):
                    tt("dve", VN[h], VN[h][rr, :], Usb[h][rr, :], p1s[h][rr, :], ALU.subtract, [Usb[h], p1s[h]])
                p2s = []
                for h in range(8):
                    r = rot()
                    mm(r, r[:], QG[h][:], Sst[h][:], [QG[h], Sst[h]], start=True, stop=False)
                    mm(r, r[:], QKm[h][rr, :], VN[h][rr, :], [QKm[h], VN[h]], start=False, stop=True)
                    p2s.append(r)
                for h in range(8):
                    cp("act", Osb, Osb[rr, h, :], p2s[h][rr, :], [p2s[h]])
                p3s = []
                for h in range(8):
                    r = rot(); mm(r, r[:], KG[h][rr, :], VN[h][rr, :], [KG[h], VN[h]]); p3s.append(r)
                for h in range(8):
                    stt("dve", Sst[h], Sst[h][:], Sst[h][:], EGL[:, 2 * h + i:2 * h + i + 1], p3s[h][:], ALU.mult, ALU.add, [Sst[h], EGL, p3s[h]])
            if maxphase < 7: continue
            for zc in range(2):
                for c in range(KC):
                    mm(zps, zps[:], H[:, c, cs], W[:, c, 2048 + zc * 512:2048 + (zc + 1) * 512], [W, H], start=(c == 0), stop=(c == KC - 1))
                act(ZS[0], ZS[0][:, zc * 512:(zc + 1) * 512], zps[:], AF.Silu, [zps])
            tt("pool", SQo, SQo[:], Osb[:], Osb[:], ALU.mult, [Osb])
            P.op("dve", lambda e: e.tensor_reduce(out=MS[:], in_=SQo[:], axis=AX.X, op=ALU.add), reads=[SQo], writes=[MS])
            act(MS, MS[:], MS[:], AF.Sqrt, [MS, epsb], bias=epsb[:, 0:1], scale=1.0 / 128)
            P.op("dve", lambda e: e.reciprocal(out=MS[:], in_=MS[:]), reads=[MS], writes=[MS])
            tt("dve", O2, O2[:], Osb[:], MS[:].unsqueeze(2).to_broadcast([128, 8, 128]), ALU.mult, [Osb, MS])
            tt("pool", O2, O2[:], O2[:], GN[:].unsqueeze(1).to_broadcast([128, 8, 128]), ALU.mult, [O2, GN])
            ob = OB[0]
            tt("dve", ob, ob[:], O2[:].rearrange("p a b -> p (a b)"), ZS[0][:], ALU.mult, [O2, ZS[0]])
            t0 = st * ST + tl * 128
            for h in range(8):
                r = rot(); rb = r[:].bitcast(BF16)
                P.op("pe", lambda e, rb=rb, h=h: e.transpose(out=rb[:, 0:128], in_=ob[:, h * 128:(h + 1) * 128], identity=identb[:]), reads=[ob, identb], writes=[r])
                cp("act" if h % 2 else "dve", OT, OT[:, h, :], rb[:, 0:128], [r])
            P.dma("sp", og[:, t0:t0 + 128].rearrange("(h f) t -> f h t", f=128), OT[:], reads=[OT])
    return


def host_D(hn_full, z):
    w_in = z['dn_w_in'][0]; conv = z['dn_conv'][0]
    s = np.arange(128)[:, None]; c = np.arange(128)[None, :]
    same = (s // 64) == (c // 64)
    masks = np.zeros((128, 6, 128), np.float32)
    masks[:, 0] = np.eye(128); masks[:, 1] = (same & (s <= c)); masks[:, 2] = same; masks[:, 3] = (same & (c >= s)); masks[:, 4] = -(same & (c > s)).astype(np.float32)
    masks[:, 5, 0] = (np.arange(128) < 64); masks[:, 5, 1] = (np.arange(128) >= 64)
    maps = []
    for core in range(8):
        b = core // 4; hg = core % 4
        qc = np.arange(hg * 512, hg * 512 + 512); kc = 2048 + qc; vc = 4096 + np.arange(hg * 1024, hg * 1024 + 1024); zc = 8192 + np.arange(hg * 1024, hg * 1024 + 1024)
        bc = 12288 + np.arange(hg * 8, hg * 8 + 8); ac = 12320 + np.arange(hg * 8, hg * 8 + 8)
        cols = np.concatenate([qc, kc, vc, zc, bc, ac])
        wcc = np.ascontiguousarray(w_in[:, cols])
        cch = np.concatenate([qc, kc, vc])
        convw = np.ascontiguousarray(conv[:, cch].T.reshape(16, 128, 4).transpose(1, 0, 2))
        hcc = np.zeros((128, 2, 8), np.float32)
        hcc[:, 0, :] = z['dn_a_log'][0][hg * 8:hg * 8 + 8][None]; hcc[:, 1, :] = z['dn_dt_bias'][0][hg * 8:hg * 8 + 8][None]
        gnrow = np.ascontiguousarray(np.broadcast_to(z['dn_norm'][0][None, :], (128, 128))).astype(np.float32)
        maps.append({"hnT": hn_full[b], "wc": wcc, "convw": convw, "hc": hcc, "gnrow": gnrow, "masks": masks})
    return maps


def emit_O1p(P, io):
    ogT = io("ogT", [1024, 8192], BF16, "ExternalInput")
    w = io("w", [1024, 2048], F32, "ExternalInput")
    pb = io("pb", [8192, 2048], F32, "ExternalOutput")
    wb = P.sb([128, 8, 2048], BF16)
    wv = w.rearrange("(c p) n -> p c n", p=128)
    for n0 in range(0, 2048, 512):
        P.dma("pool", wb[:, :, n0:n0 + 512], wv[:, :, n0:n0 + 512], writes=[wb])
    a = [P.sb([128, 8, 512], BF16) for _ in range(2)]
    ob = [P.sb([128, 512], F32) for _ in range(3)]
    ps = [P.ps([128, 512]) for _ in range(3)]
    av = ogT.rearrange("(c p) t -> p c t", p=128)
    pbv = pb.rearrange("(k r p) c -> k p r c", k=16, r=4, p=128)
    n_ = 0
    for s in range(16):
        A_ = a[s % 2]
        P.dma("sp", A_[:], av[:, :, s * 512:(s + 1) * 512], writes=[A_])
        for n in range(16):
            p_ = ps[n_ % 3]; O_ = ob[n_ % 3]; n_ += 1
            for c in range(8):
                P.op("pe", lambda e, c=c, p_=p_, n=n, A_=A_: e.matmul(p_[:], lhsT=wb[:, c, n * 128:(n + 1) * 128], rhs=A_[:, c, :], start=(c == 0), stop=(c == 7)), reads=[wb, A_], writes=[p_])
            if n % 2:
                P.op("act", lambda e, p_=p_, O_=O_: e.activation(out=O_[:], in_=p_[:], func=AF.Copy), reads=[p_], writes=[O_])
            else:
                P.op("dve", lambda e, p_=p_, O_=O_: e.tensor_copy(out=O_[:], in_=p_[:]), reads=[p_], writes=[O_])
            P.dma("act" if n % 2 else "sp", pbv[n][:, :, s * 128:(s + 1) * 128], O_[:].rearrange("p (r t) -> p r t", r=4), reads=[O_])
    return

_G4 = [[0, 1, 2, 3], [4, 5, 6, 7]]


_INFO = {}


def build_fused(upto=None):
    P = Prog()
    AG = "AllGather"
    s_qT = P.scratch("s_qT", [2048, 2048], BF16); s_kT = P.scratch("s_kT", [512, 2048], BF16); s_v = P.scratch("s_v", [2048, 512], BF16)
    s_iqT = P.scratch("s_iqT", [1024, 2048], F32); s_ikT = P.scratch("s_ikT", [64, 2048], F32); s_iw = P.scratch("s_iw", [2048, 16], F32)
    g_kT = P.scratch("g_kT", [2048, 2048], BF16); g_v = P.scratch("g_v", [8192, 512], BF16); g_ik = P.scratch("g_ik", [256, 2048], F32)
    s_oT = P.scratch("s_oT", [2048, 2048], BF16); s_h1 = P.scratch("s_h1", [2048, 2048], F32); s_h3 = P.scratch("s_h3", [2048, 2048], F32)
    s_hn = P.scratch("s_hn", [2048, 2048], BF16); g_hn = P.scratch("g_hn", [8192, 2048], BF16)
    s_og = P.scratch("s_og", [1024, 8192], BF16); s_pb = P.scratch("s_pb", [8192, 2048], F32); s_mix = P.scratch("s_mix", [2048, 2048], F32)
    P.begin_phase(); emit_A(P, IO(P, "A_", {"qT": s_qT, "kT": s_kT, "v": s_v, "iqT": s_iqT, "ikT": s_ikT, "iw": s_iw})); P.end_phase()
    for k in range(2):
        P.coll(AG, ALU.bypass, _G4, s_kT[k * 256:(k + 1) * 256, :], g_kT[k * 1024:(k + 1) * 1024, :])
    for k in range(2):
        P.coll(AG, ALU.bypass, _G4, s_v[k * 1024:(k + 1) * 1024, :], g_v[k * 4096:(k + 1) * 4096, :])
    P.coll(AG, ALU.bypass, _G4, s_ikT, g_ik)
    bB = {"qTc": s_qT, "kT": g_kT, "v": g_v, "ikT": g_ik, "iqTc": s_iqT, "iwc": s_iw, "oT": s_oT}
    if upto == "B":
        del bB["oT"]
    P.begin_phase(); emit_B(P, IO(P, "B_", bB)); P.end_phase()
    if upto == "B":
        _INFO["in"] = list(P.ext_in); _INFO["out"] = list(P.ext_out)
        return P.finish()
    bO = {"aT": s_oT, "hT": s_h1}
    if upto == "O":
        del bO["hT"]
    P.begin_phase(); emit_O(P, IO(P, "O0_", bO), 2048); P.end_phase()
    if upto == "O":
        _INFO["in"] = list(P.ext_in); _INFO["out"] = list(P.ext_out)
        return P.finish()
    P.begin_phase(); emit_P(P, IO(P, "P0_", {"hT": s_h1, "h3T": s_h3, "hnT": s_hn}), True); P.end_phase()
    for k in range(8):
        P.coll(AG, ALU.bypass, _G4, s_hn[k * 256:(k + 1) * 256, :], g_hn[k * 1024:(k + 1) * 1024, :])
    P.begin_phase(); emit_D(P, IO(P, "D_", {"hnT": g_hn, "og": s_og})); P.end_phase()
    P.begin_phase(); emit_O1p(P, IO(P, "Q_", {"ogT": s_og, "pb": s_pb})); P.end_phase()
    for k in range(16):
        P.coll("ReduceScatter", ALU.add, _G4, s_pb[k * 512:(k + 1) * 512, :], s_mix[k * 128:(k + 1) * 128, :])
    P.begin_phase(); emit_P(P, IO(P, "P1_", {"hT": s_h3, "mixT": s_mix}), False, add=True); P.end_phase()
    print("fused stats", P.stats(), flush=True)
    _INFO["in"] = list(P.ext_in); _INFO["out"] = list(P.ext_out)
    return P.finish()


def own_pos(c):
    return np.concatenate([np.arange((4 * j + c) * 128, (4 * j + c + 1) * 128) for j in range(16)])


def host_fused(z):
    x = z['x']
    ident = np.eye(128, dtype=np.float32).astype(BF)
    uT = [np.ascontiguousarray(z['peer_u'][l].T) for l in range(2)]
    keysT = [np.ascontiguousarray(z['peer_keys'][l].reshape(16, 128, 128).transpose(2, 0, 1)) for l in range(2)]
    gns = []
    for l in range(2):
        gnext = z['norm_mix'][l + 1] if l == 0 else np.ones(D, np.float32)
        gns.append(np.ascontiguousarray(np.stack([z['norm_ffn'][l].reshape(KC, 128).T, z['norm_ple'][l].reshape(KC, 128).T, gnext.reshape(KC, 128).T], axis=1)))
    dmaps = host_D([None, None], z)
    maps = []
    for core in range(8):
        b = core // 4; c = core % 4
        pos = own_pos(c)
        csq, csi, perm = rope_tables(pos)
        xT = np.ascontiguousarray(x[b, pos, :].T)
        r = np.arange(128)[:, None]; t = np.arange(512)[None, :]
        m = {"A_xT": xT, "A_gmix": np.ascontiguousarray(z['norm_mix'][0].reshape(KC, 128).T), "A_w_in": z['attn_w_in'][0],
             "A_qkg": np.ascontiguousarray(np.stack([z['attn_q_norm'][0], z['attn_k_norm'][0]], axis=1)), "A_csq": csq, "A_csi": csi, "A_perm": perm,
             "B_cbias": np.where(t <= c * 128 + r, 0.0, -1e5).astype(np.float32), "B_ident": ident,
             "O0_resT": xT, "O0_w": z['attn_w_out'][0]}
        for l, pre in ((0, "P0_"), (1, "P1_")):
            m.update({pre + "gn": gns[l], pre + "w_q": z['peer_w_q'][l], pre + "keysT": keysT[l], pre + "uT": uT[l], pre + "vt": z['peer_v'][l],
                      pre + "wg": z['ple_w_gate'][l], pre + "wpi": z['ple_w_in'][l], pre + "pT": np.ascontiguousarray(z['p'][l, b, pos, :].T), pre + "ident": ident})
        for k in ("wc", "convw", "hc", "gnrow", "masks"):
            m["D_" + k] = dmaps[core][k]
        m["Q_w"] = np.ascontiguousarray(z['dn_w_out'][0][c * 1024:(c + 1) * 1024, :])
        maps.append(m)
    return maps


def kernel(**inputs):
    z = {k: np.ascontiguousarray(np.asarray(v)) for k, v in inputs.items()}
    nc = build_fused()
    maps = [{k: m[k] for k in _INFO["in"]} for m in host_fused(z)]
    res = run_bass_kernel_spmd(nc, maps, core_ids=list(range(8))).results
    out = np.zeros((2, 8192, 2048), np.float32)
    for core in range(8):
        b = core // 4; c = core % 4
        out[b, own_pos(c), :] = res[core]["P1_h3T"].T
    return out
```

```python
import numpy as np
from contextlib import ExitStack
import concourse.bass as bass
import concourse.mybir as mybir
from concourse.bass_utils import run_bass_kernel_spmd

F32 = mybir.dt.float32
BF16 = mybir.dt.bfloat16
I32 = mybir.dt.int32
U32 = mybir.dt.uint32
AF = mybir.ActivationFunctionType
ALU = mybir.AluOpType
AX = mybir.AxisListType

ENGS = ("pe", "act", "dve", "pool", "sp")


class T:
    __slots__ = ("t", "name", "lw", "rd")

    def __init__(self, t, name):
        self.t = t
        self.name = name
        self.lw = None
        self.rd = []

    def __getitem__(self, idx):
        return self.t[idx]


class TV:
    __slots__ = ("t", "name", "par")

    def __init__(self, par, ap, name):
        self.par = par
        self.t = ap
        self.name = name

    def __getitem__(self, idx):
        return self.t[idx]

    @property
    def lw(self):
        return self.par.lw

    @lw.setter
    def lw(self, v):
        self.par.lw = v

    @property
    def rd(self):
        return self.par.rd

    @rd.setter
    def rd(self, v):
        self.par.rd = v


class Prog:
    def __init__(self, name="k", ndma=6, same_eng_sync=True):
        self.nc = bass.Bass("TRN2", target_bir_lowering=False)
        self.es = ExitStack()
        self.ops = {e: [] for e in ENGS}
        self.cnt = {e: 0 for e in ENGS}
        self.known = {e: {} for e in ENGS}
        self.sem = {}
        for e in ENGS:
            self.sem[e] = self.es.enter_context(self.nc.semaphore("c_" + e))
        self.ndma = ndma
        self.dsem = {}
        self.dcnt = {}
        for q in ("sp", "act", "pool"):
            self.dsem[q] = [self.es.enter_context(self.nc.semaphore(f"d_{q}{i}")) for i in range(ndma)]
            self.dcnt[q] = 0
        self.same = same_eng_sync
        self.n_t = 0
        self.phase_es = None
        self.dram_dep = {}
        self.ccsem = self.es.enter_context(self.nc.semaphore("ccsem"))
        self.ccn = 0
        self.ext_in = []
        self.ext_out = []

    def dram(self, name, shape, dt, kind):
        return self.nc.dram_tensor(name, list(shape), dt, kind=kind).ap()

    def sb(self, shape, dt, name=None):
        self.n_t += 1
        name = name or f"sb{self.n_t}"
        t = (self.phase_es or self.es).enter_context(self.nc.sbuf_tensor(name, list(shape), dt))
        return T(t, name)

    def ps(self, shape, dt=F32, name=None):
        self.n_t += 1
        name = name or f"ps{self.n_t}"
        t = (self.phase_es or self.es).enter_context(self.nc.psum_tensor(name, list(shape), dt))
        return T(t, name)

    def scratch(self, name, shape, dt):
        t = self.nc.dram_tensor(name, list(shape), dt)
        self.dram_dep[name] = T(None, name)
        return t.ap()

    def begin_phase(self):
        self.phase_es = ExitStack()

    def end_phase(self):
        self.barrier()
        self.phase_es.close()
        self.phase_es = None

    def barrier(self):
        for e in ENGS:
            toks = []
            for f in ENGS:
                if f != e and self.cnt[f] > 0:
                    toks.append((f, self.sem[f], self.cnt[f], f))
            for q in ("sp", "act", "pool"):
                n = self.dcnt[q]
                for slot in range(min(n, self.ndma)):
                    last_i = ((n - 1 - slot) // self.ndma) * self.ndma + slot
                    toks.append((f"d_{q}{slot}", self.dsem[q][slot], 16 * (last_i // self.ndma + 1), "dma"))
            if self.ccn:
                toks.append(("cc", self.ccsem, self.ccn, "dma"))
            waits = self._waits(e, toks)
            self.ops[e].append((waits, None, None))

    def coll(self, kind, op, groups, src, dst):
        reads = [self.dram_dep[src.name]]
        writes = [self.dram_dep[dst.name]]
        waits = self._waits("pool", self._deps(reads, writes))
        self.ccn += 1
        tok = ("cc", self.ccsem, self.ccn, "dma")

        def fn(e, kind=kind, op=op, groups=groups, src=src, dst=dst):
            return e.collective_compute(kind, op, replica_groups=groups, ins=[src.opt()], outs=[dst.opt()])
        self.ops["pool"].append((waits, fn, (self.ccsem, 1)))
        self._commit(tok, reads, writes)

    def ext(self, name):
        return T(None, name)

    def _deps(self, reads, writes):
        toks = []
        for r in reads:
            if r.lw is not None:
                toks.append(r.lw)
        for w in writes:
            if w.lw is not None:
                toks.append(w.lw)
            toks.extend(w.rd)
        return toks

    def _waits(self, eng, toks):
        kn = self.known[eng]
        need = {}
        for (key, semh, val, src_eng) in toks:
            if src_eng == eng and (not self.same or eng == "pe"):
                continue
            if kn.get(key, 0) >= val:
                continue
            if key not in need or need[key][1] < val:
                need[key] = (semh, val)
        out = []
        for key, (semh, val) in need.items():
            kn[key] = val
            out.append((semh, val))
        return out

    def _commit(self, tok, reads, writes):
        for w in writes:
            w.lw = tok
            w.rd = []
        for r in reads:
            if r in writes:
                continue
            r.rd.append(tok)
            if len(r.rd) > 24:
                best = {}
                for t in r.rd:
                    if t[0] not in best or best[t[0]][2] < t[2]:
                        best[t[0]] = t
                r.rd = list(best.values())

    def op(self, eng, fn, reads=(), writes=()):
        reads = [r for r in reads if r is not None]
        writes = [w for w in writes if w is not None]
        waits = self._waits(eng, self._deps(reads, writes))
        self.cnt[eng] += 1
        seq = self.cnt[eng]
        tok = (eng, self.sem[eng], seq, eng)
        self.ops[eng].append((waits, fn, (self.sem[eng], 1)))
        self._commit(tok, reads, writes)
        return tok

    def dma(self, q, out, in_, reads=(), writes=(), **kw):
        reads = [r for r in reads if r is not None]
        writes = [w for w in writes if w is not None]
        nm = getattr(in_, "name", None)
        if nm in self.dram_dep:
            reads.append(self.dram_dep[nm])
        nm = getattr(out, "name", None)
        if nm in self.dram_dep:
            writes.append(self.dram_dep[nm])
        i = self.dcnt[q]
        self.dcnt[q] += 1
        slot = i % self.ndma
        val = 16 * (i // self.ndma + 1)
        semh = self.dsem[q][slot]
        key = f"d_{q}{slot}"
        toks = self._deps(reads, writes)
        if i >= self.ndma:
            toks.append((key, semh, val - 16, "dma"))
        waits = self._waits(q, toks)
        tok = (key, semh, val, "dma")

        def fn(e, out=out, in_=in_, kw=kw):
            return e.dma_start(out=out, in_=in_, **kw)
        self.ops[q].append((waits, fn, (semh, 16)))
        self._commit(tok, reads, writes)
        return tok

    def finish(self, final_toks=()):
        nc = self.nc
        fin = []
        for q in ("sp", "act", "pool"):
            n = self.dcnt[q]
            for slot in range(min(n, self.ndma)):
                last_i = ((n - 1 - slot) // self.ndma) * self.ndma + slot
                fin.append((f"d_{q}{slot}", self.dsem[q][slot], 16 * (last_i // self.ndma + 1), "dma"))
        for e in ENGS:
            if e != "sp" and self.cnt[e] > 0:
                fin.append((e, self.sem[e], self.cnt[e], e))
        if self.ccn:
            fin.append(("cc", self.ccsem, self.ccn, "dma"))
        fwaits = self._waits("sp", fin)
        ops = self.ops

        def run(e, lst, extra=()):
            for waits, fn, inc in lst:
                for (s, v) in waits:
                    e.wait_ge(s, v)
                if fn is not None:
                    fn(e).then_inc(inc[0], inc[1])
            for (s, v) in extra:
                e.wait_ge(s, v)

        with nc.Block() as block:
            @block.sync
            def _(e):
                run(e, ops["sp"], fwaits)

            @block.tensor
            def _(e):
                run(e, ops["pe"])

            @block.scalar
            def _(e):
                run(e, ops["act"])

            @block.vector
            def _(e):
                run(e, ops["dve"])

            @block.gpsimd
            def _(e):
                run(e, ops["pool"])
        self.es.close()
        return nc

    def stats(self):
        return {e: len(self.ops[e]) for e in ENGS}


class IO:
    def __init__(self, P, prefix, bind=None):
        self.P = P
        self.prefix = prefix
        self.bind = bind or {}

    def __call__(self, name, shape, dt, kind):
        if name in self.bind:
            ap = self.bind[name]
            assert list(ap.shape) == list(shape), (name, ap.shape, shape)
            return ap
        full = self.prefix + name
        if kind == "ExternalInput":
            self.P.ext_in.append(full)
        else:
            self.P.ext_out.append(full)
        return self.P.dram(full, shape, dt, kind)


import ml_dtypes
BF = ml_dtypes.bfloat16


D = 2048; NT = 2048; TT = 512; KC = 16
EPS = 1e-6

def emit_A(P, io):
    xT = io("xT", [D, NT], F32, "ExternalInput")
    gmix = io("gmix", [128, KC], F32, "ExternalInput")
    w_in = io("w_in", [D, 4176], F32, "ExternalInput")
    qkg = io("qkg", [128, 2], F32, "ExternalInput")
    csq = io("csq", [32, 2, NT], F32, "ExternalInput")
    csi = io("csi", [128, 2, NT], F32, "ExternalInput")
    perm = io("perm", [128, 2, 128], F32, "ExternalInput")
    qT = io("qT", [2048, NT], BF16, "ExternalOutput")
    kT = io("kT", [512, NT], BF16, "ExternalOutput")
    vv = io("v", [NT, 512], BF16, "ExternalOutput")
    iqT = io("iqT", [1024, NT], F32, "ExternalOutput")
    ikT = io("ikT", [64, NT], F32, "ExternalOutput")
    iw = io("iw", [NT, 16], F32, "ExternalOutput")

    gm = P.sb([128, KC], F32); qk = P.sb([128, 2], F32); pm = P.sb([128, 2, 128], F32)
    ones_b = P.sb([128, 128], BF16); ones_f = P.sb([128, 128], F32); epsb = P.sb([128, 1], F32)
    P.dma("sp", gm[:], gmix[:, :], writes=[gm]); P.dma("sp", qk[:], qkg[:, :], writes=[qk]); P.dma("sp", pm[:], perm[:, :, :], writes=[pm])
    P.op("dve", lambda e: e.memset(ones_b[:], 1.0), writes=[ones_b])
    P.op("dve", lambda e: e.memset(ones_f[:], 1.0 / 128), writes=[ones_f])
    P.op("dve", lambda e: e.memset(epsb[:], EPS), writes=[epsb])

    xs = P.sb([128, KC, TT], F32)
    sq = P.sb([128, KC, TT], BF16)
    hb = P.sb([128, KC, TT], BF16)
    rstd = P.sb([128, TT], F32)
    cq = P.sb([32, 2, TT], F32); ci = P.sb([128, 2, TT], F32)
    w32 = [P.sb([128, KC, 512], F32) for _ in range(2)]
    wb = [P.sb([128, KC, 512], BF16) for _ in range(2)]
    ps_ms = P.ps([128, TT]); ps_pj = [P.ps([128, TT]) for _ in range(2)]; ps_ms2 = P.ps([128, TT]); ps_rot = P.ps([128, TT])
    sq2 = [P.sb([128, TT], F32) for _ in range(2)]
    r2 = [P.sb([128, TT], F32) for _ in range(2)]
    qn = [P.sb([128, TT], F32) for _ in range(2)]
    t1 = [P.sb([128, TT], F32) for _ in range(2)]
    t2 = [P.sb([128, TT], F32) for _ in range(2)]
    ob = [P.sb([128, TT], BF16) for _ in range(2)]
    of = [P.sb([128, TT], F32) for _ in range(2)]
    vb = [P.sb([128, 512], BF16) for _ in range(2)]
    iwb = [P.sb([128, 16], F32) for _ in range(2)]
    xTv = xT.rearrange("(c p) t -> p c t", p=128)
    wv = w_in.rearrange("(c p) n -> p c n", p=128)
    cnt = [0]
    for tt in range(NT // TT):
        ts = slice(tt * TT, (tt + 1) * TT)
        P.dma("sp", xs[:], xTv[:, :, ts], writes=[xs])
        P.dma("act", cq[:], csq[:, :, ts], writes=[cq])
        P.dma("act", ci[:], csi[:, :, ts], writes=[ci])
        P.op("act", lambda e: e.activation(out=sq[:], in_=xs[:], func=AF.Square), reads=[xs], writes=[sq])
        for c in range(KC):
            P.op("pe", lambda e, c=c: e.matmul(ps_ms[:], lhsT=ones_b[:], rhs=sq[:, c, :], start=(c == 0), stop=(c == KC - 1)), reads=[ones_b, sq], writes=[ps_ms])
        P.op("act", lambda e: e.activation(out=rstd[:], in_=ps_ms[:], func=AF.Sqrt, bias=epsb[:, 0:1], scale=1.0 / D), reads=[ps_ms, epsb], writes=[rstd])
        P.op("dve", lambda e: e.reciprocal(out=rstd[:], in_=rstd[:]), reads=[rstd], writes=[rstd])
        for c in range(KC):
            P.op("dve", lambda e, c=c: e.scalar_tensor_tensor(out=xs[:, c, :], in0=xs[:, c, :], scalar=gm[:, c:c + 1], in1=rstd[:], op0=ALU.mult, op1=ALU.mult), reads=[xs, gm, rstd], writes=[xs])
        P.op("pool", lambda e: e.tensor_copy(out=hb[:], in_=xs[:]), reads=[xs], writes=[hb])
        for g in range(9):
            n0 = g * 512; ncol = 512 if g < 8 else 80
            bi = cnt[0] % 2; cnt[0] += 1
            W32 = w32[bi]; WB = wb[bi]
            P.dma("sp", W32[:, :, 0:ncol], wv[:, :, n0:n0 + ncol], writes=[W32])
            if g <= 5:
                P.op("pool", lambda e, W32=W32, WB=WB: e.tensor_copy(out=WB[:], in_=W32[:]), reads=[W32], writes=[WB])
            if g <= 4:
                for j in range(4):
                    ch = g * 4 + j
                    pj = ps_pj[ch % 2]; S2 = sq2[ch % 2]; R2 = r2[ch % 2]; QN = qn[ch % 2]; T1 = t1[ch % 2]; T2 = t2[ch % 2]; OB = ob[ch % 2]
                    for c in range(KC):
                        P.op("pe", lambda e, c=c, j=j, pj=pj, WB=WB: e.matmul(pj[:], lhsT=WB[:, c, j * 128:(j + 1) * 128], rhs=hb[:, c, :], start=(c == 0), stop=(c == KC - 1)), reads=[WB, hb], writes=[pj])
                    P.op("act", lambda e, pj=pj, S2=S2: e.activation(out=S2[:], in_=pj[:], func=AF.Square), reads=[pj], writes=[S2])
                    P.op("pe", lambda e, S2=S2: e.matmul(ps_ms2[:], lhsT=ones_f[:], rhs=S2[:], start=True, stop=True), reads=[ones_f, S2], writes=[ps_ms2])
                    P.op("act", lambda e, R2=R2: e.activation(out=R2[:], in_=ps_ms2[:], func=AF.Sqrt, bias=epsb[:, 0:1], scale=1.0), reads=[ps_ms2, epsb], writes=[R2])
                    P.op("dve", lambda e, R2=R2: e.reciprocal(out=R2[:], in_=R2[:]), reads=[R2], writes=[R2])
                    gi = 0 if g < 4 else 1
                    P.op("dve", lambda e, pj=pj, QN=QN, R2=R2, gi=gi: e.scalar_tensor_tensor(out=QN[:], in0=pj[:], scalar=qk[:, gi:gi + 1], in1=R2[:], op0=ALU.mult, op1=ALU.mult), reads=[pj, qk, R2], writes=[QN])
                    P.op("pe", lambda e, QN=QN: e.matmul(ps_rot[0:32, :], lhsT=pm[0:32, 0, 0:32], rhs=QN[0:32, :], start=True, stop=True), reads=[pm, QN], writes=[ps_rot])
                    P.op("pool", lambda e, QN=QN, T1=T1: e.tensor_tensor(out=T1[0:32, :], in0=QN[0:32, :], in1=cq[:, 0, :], op=ALU.mult), reads=[QN, cq], writes=[T1])
                    P.op("dve", lambda e, T2=T2: e.tensor_tensor(out=T2[0:32, :], in0=ps_rot[0:32, :], in1=cq[:, 1, :], op=ALU.mult), reads=[ps_rot, cq], writes=[T2])
                    P.op("pool", lambda e, QN=QN, T1=T1, T2=T2: e.tensor_tensor(out=QN[0:32, :], in0=T1[0:32, :], in1=T2[0:32, :], op=ALU.add), reads=[T1, T2], writes=[QN])
                    P.op("act", lambda e, QN=QN, OB=OB: e.activation(out=OB[:], in_=QN[:], func=AF.Copy), reads=[QN], writes=[OB])
                    dst = qT[ch * 128:(ch + 1) * 128, ts] if g < 4 else kT[j * 128:(j + 1) * 128, ts]
                    P.dma("pool", dst, OB[:], reads=[OB])
            elif g == 5:
                for s in range(4):
                    pj = ps_pj[s % 2]; VB = vb[s % 2]
                    for c in range(KC):
                        P.op("pe", lambda e, c=c, s=s, pj=pj, WB=WB: e.matmul(pj[:], lhsT=hb[:, c, s * 128:(s + 1) * 128], rhs=WB[:, c, :], start=(c == 0), stop=(c == KC - 1)), reads=[WB, hb], writes=[pj])
                    P.op("act", lambda e, pj=pj, VB=VB: e.activation(out=VB[:], in_=pj[:], func=AF.Copy), reads=[pj], writes=[VB])
                    P.dma("pool", vv[tt * TT + s * 128: tt * TT + (s + 1) * 128, :], VB[:], reads=[VB])
            else:
                nch = 4 if g < 8 else 1
                for j in range(nch):
                    M = 128 if g < 8 else 64
                    ch = (g - 6) * 4 + j
                    pj = ps_pj[j % 2]; QN = qn[j % 2]; T1 = t1[j % 2]; T2 = t2[j % 2]; OF = of[j % 2]
                    for c in range(KC):
                        P.op("pe", lambda e, c=c, j=j, pj=pj, W32=W32, M=M: e.matmul(pj[0:M, :], lhsT=W32[:, c, j * 128:j * 128 + M], rhs=xs[:, c, :], start=(c == 0), stop=(c == KC - 1)), reads=[W32, xs], writes=[pj])
                    P.op("act", lambda e, pj=pj, QN=QN, M=M: e.activation(out=QN[0:M, :], in_=pj[0:M, :], func=AF.Copy), reads=[pj], writes=[QN])
                    P.op("pe", lambda e, QN=QN, M=M: e.matmul(ps_rot[0:M, :], lhsT=pm[0:M, 1, 0:M], rhs=QN[0:M, :], start=True, stop=True), reads=[pm, QN], writes=[ps_rot])
                    P.op("pool", lambda e, QN=QN, T1=T1, M=M: e.tensor_tensor(out=T1[0:M, :], in0=QN[0:M, :], in1=ci[0:M, 0, :], op=ALU.mult), reads=[QN, ci], writes=[T1])
                    P.op("dve", lambda e, T2=T2, M=M: e.tensor_tensor(out=T2[0:M, :], in0=ps_rot[0:M, :], in1=ci[0:M, 1, :], op=ALU.mult), reads=[ps_rot, ci], writes=[T2])
                    P.op("pool", lambda e, OF=OF, T1=T1, T2=T2, M=M: e.tensor_tensor(out=OF[0:M, :], in0=T1[0:M, :], in1=T2[0:M, :], op=ALU.add), reads=[T1, T2], writes=[OF])
                    dst = iqT[ch * 128:(ch + 1) * 128, ts] if g < 8 else ikT[:, ts]
                    P.dma("pool", dst, OF[0:M, :], reads=[OF])
                if g == 8:
                    for s in range(4):
                        pj = ps_pj[s % 2]; IW = iwb[s % 2]
                        for c in range(KC):
                            P.op("pe", lambda e, c=c, s=s, pj=pj, W32=W32: e.matmul(pj[:, 0:16], lhsT=xs[:, c, s * 128:(s + 1) * 128], rhs=W32[:, c, 64:80], start=(c == 0), stop=(c == KC - 1)), reads=[W32, xs], writes=[pj])
                        P.op("act", lambda e, pj=pj, IW=IW: e.activation(out=IW[:], in_=pj[:, 0:16], func=AF.Copy), reads=[pj], writes=[IW])
                        P.dma("pool", iw[tt * TT + s * 128: tt * TT + (s + 1) * 128, :], IW[:], reads=[IW])
    return


def rope_tables(pos):
    pos = pos.astype(np.float32)
    theta = np.float32(500000.0)
    f16 = (theta ** (-np.arange(16, dtype=np.float32) * np.float32(2.0 / 32))).astype(np.float32)
    ang = pos[None, :] * f16[:, None]
    csq = np.zeros((32, 2, len(pos)), np.float32)
    csq[0:16, 0] = np.cos(ang); csq[16:32, 0] = np.cos(ang)
    csq[0:16, 1] = np.sin(ang); csq[16:32, 1] = np.sin(ang)
    f8 = (theta ** (-np.arange(8, dtype=np.float32) * np.float32(2.0 / 16))).astype(np.float32)
    ang8 = pos[None, :] * f8[:, None]
    csi = np.zeros((128, 2, len(pos)), np.float32)
    csi[:, 0] = 1.0
    for h in range(2):
        for half in range(2):
            csi[h * 64 + half * 8: h * 64 + half * 8 + 8, 0] = np.cos(ang8)
            csi[h * 64 + half * 8: h * 64 + half * 8 + 8, 1] = np.sin(ang8)
    perm = np.zeros((128, 2, 128), np.float32)
    for m in range(16):
        perm[m + 16, 0, m] = -1.0
        perm[m, 0, m + 16] = 1.0
    for h in range(2):
        for m in range(8):
            perm[h * 64 + m + 8, 1, h * 64 + m] = -1.0
            perm[h * 64 + m, 1, h * 64 + m + 8] = 1.0
    return csq, csi, perm


def host_A(x, norm_mix0, attn_w_in, qn, kn):
    maps = []
    for core in range(8):
        b = core // 4; t0 = (core % 4) * NT
        pos = np.arange(t0, t0 + NT)
        csq, csi, perm = rope_tables(pos)
        maps.append({
            "xT": np.ascontiguousarray(x[b, t0:t0 + NT, :].T),
            "gmix": np.ascontiguousarray(norm_mix0.reshape(KC, 128).T),
            "w_in": attn_w_in,
            "qkg": np.ascontiguousarray(np.stack([qn, kn], axis=1)),
            "csq": csq, "csi": csi, "perm": perm,
        })
    return maps


TKEYS = 8192; NQT = 16
NBIS = 24; LO0 = -2048.0; TOPK = 256.0

def emit_B(P, io, nqt=NQT):
    n_sc = 0; n_v = 0; n_pl = 0
    qTc = io("qTc", [2048, 2048], BF16, "ExternalInput")
    kT = io("kT", [2048, 2048], BF16, "ExternalInput")
    vv = io("v", [TKEYS, 512], BF16, "ExternalInput")
    ikT = io("ikT", [256, 2048], F32, "ExternalInput")
    iqTc = io("iqTc", [1024, 2048], F32, "ExternalInput")
    iwc = io("iwc", [2048, 16], F32, "ExternalInput")
    cbias = io("cbias", [128, 512], F32, "ExternalInput")
    identd = io("ident", [128, 128], BF16, "ExternalInput")
    oT = io("oT", [2048, 2048], BF16, "ExternalOutput")

    kt = P.sb([128, 4, TKEYS], BF16); ik = P.sb([64, TKEYS], F32)
    iw = P.sb([128, NQT, 16], F32); cb = P.sb([128, 512], F32); ident = P.sb([128, 128], BF16)
    ones_b = P.sb([128, 128], BF16)
    for g in range(4):
        for r in range(4):
            P.dma("sp", kt[:, g, :].rearrange("d (j r t) -> d j r t", r=4, t=128)[:, :, r, :], kT[(g // 2) * 1024 + r * 256 + (g % 2) * 128:(g // 2) * 1024 + r * 256 + (g % 2) * 128 + 128, :].rearrange("d (j t) -> d j t", t=128), writes=[kt])
    for r in range(4):
        P.dma("act", ik[:].rearrange("d (j r t) -> d j r t", r=4, t=128)[:, :, r, :], ikT[r * 64:(r + 1) * 64, :].rearrange("d (j t) -> d j t", t=128), writes=[ik])
    P.dma("act", iw[:], iwc.rearrange("(j p) h -> p j h", p=128), writes=[iw])
    P.dma("act", cb[:], cbias[:, :], writes=[cb]); P.dma("act", ident[:], identd[:, :], writes=[ident])
    P.op("dve", lambda e: e.memset(ones_b[:], 1.0), writes=[ones_b])

    score = P.sb([128, TKEYS], F32); junk = P.sb([128, TKEYS], BF16)
    maskT = P.sb([128, 64, 128], BF16)
    qt = [P.sb([128, 16, 128], BF16) for _ in range(2)]
    iq = [P.sb([64, 16, 128], F32) for _ in range(1)]
    rl = [P.sb([128, 512], F32) for _ in range(4)]
    mk_ = [P.sb([128, 512], BF16) for _ in range(2)]
    vt = [P.sb([128, 4, 128], BF16) for _ in range(3)]
    pe_ = [P.sb([128, 512], BF16) for _ in range(3)]
    pmk = [P.sb([128, 512], BF16) for _ in range(3)]
    lo = P.sb([128, 1], F32); mid = P.sb([128, 1], F32); cntall = P.sb([128, NBIS], F32); tmp = P.sb([128, 1], F32)
    rs = P.sb([128, 512], F32); ob = [P.sb([128, 512], BF16) for _ in range(2)]
    ps_sc = [P.ps([128, 512]) for _ in range(2)]
    ps_tr = P.ps([128, 4, 128], BF16)
    ps_pl = [P.ps([128, 512]) for _ in range(2)]
    ps_o = P.ps([128, 512]); ps_s = P.ps([128, 512])
    sc_rot = [ps_sc[0], ps_sc[1], ps_pl[0], ps_pl[1]]
    qv = qTc.rearrange("(h d) q -> d h q", d=128)
    iqv = iqTc.rearrange("(h d) q -> d h q", d=64)
    SCALE = 128 ** -0.5
    for j in range(nqt):
        NCH = j + 1
        QT = qt[j % 2]; IQ = iq[0]
        P.dma("sp", QT[:], qv[:, :, j * 128:(j + 1) * 128], writes=[QT])
        P.dma("sp", IQ[:], iqv[:, :, j * 128:(j + 1) * 128], writes=[IQ])
        for ch in range(NCH):
            cs = slice(ch * 512, (ch + 1) * 512)
            for h in range(16):
                ps = sc_rot[n_sc % 4]; RL = rl[n_sc % 4]; n_sc += 1
                P.op("pe", lambda e, ps=ps, h=h, cs=cs, IQ=IQ: e.matmul(ps[:], lhsT=IQ[:, h, :], rhs=ik[:, cs], start=True, stop=True), reads=[IQ, ik], writes=[ps])
                P.op("act", lambda e, ps=ps, RL=RL: e.activation(out=RL[:], in_=ps[:], func=AF.Relu), reads=[ps], writes=[RL])
                if h == 0:
                    P.op("dve", lambda e, RL=RL, cs=cs, j=j, h=h: e.tensor_scalar(out=score[:, cs], in0=RL[:], scalar1=iw[:, j, h:h + 1], scalar2=None, op0=ALU.mult), reads=[RL, iw], writes=[score])
                else:
                    P.op("dve", lambda e, RL=RL, cs=cs, j=j, h=h: e.scalar_tensor_tensor(out=score[:, cs], in0=RL[:], scalar=iw[:, j, h:h + 1], in1=score[:, cs], op0=ALU.mult, op1=ALU.add), reads=[RL, iw, score], writes=[score])
            if ch == NCH - 1:
                P.op("dve", lambda e, cs=cs: e.tensor_tensor(out=score[:, cs], in0=score[:, cs], in1=cb[:], op=ALU.add), reads=[score, cb], writes=[score])
        S = NCH * 512
        P.op("dve", lambda e: e.memset(cntall[:], 0.0), writes=[cntall])
        step = -LO0
        P.op("dve", lambda e: e.memset(mid[:], 0.0), writes=[mid])
        for it in range(NBIS):
            st = step
            P.op("dve", lambda e, S=S, it=it: e.tensor_scalar(out=junk[:, 0:S], in0=score[:, 0:S], scalar1=mid[:, 0:1], scalar2=0.0, op0=ALU.is_ge, op1=ALU.add, accum_out=cntall[:, it:it + 1]), reads=[score, mid, cntall], writes=[junk, cntall])
            P.op("dve", lambda e, it=it: e.tensor_scalar(out=tmp[:], in0=cntall[:, it:it + 1], scalar1=TOPK, scalar2=0.5, op0=ALU.is_ge, op1=ALU.subtract), reads=[cntall], writes=[tmp])
            P.op("dve", lambda e, st=st: e.scalar_tensor_tensor(out=mid[:], in0=tmp[:], scalar=st, in1=mid[:], op0=ALU.mult, op1=ALU.add), reads=[tmp, mid], writes=[mid])
            step *= 0.5
        fs = step
        P.op("dve", lambda e, fs=fs: e.tensor_scalar(out=lo[:], in0=mid[:], scalar1=-fs, scalar2=None, op0=ALU.add), reads=[mid], writes=[lo])
        for ch in range(NCH):
            cs = slice(ch * 512, (ch + 1) * 512)
            MK = mk_[ch % 2]
            P.op("dve", lambda e, MK=MK, cs=cs: e.tensor_scalar(out=MK[:], in0=score[:, cs], scalar1=lo[:, 0:1], scalar2=None, op0=ALU.is_ge), reads=[score, lo], writes=[MK])
            for t in range(4):
                P.op("pe", lambda e, MK=MK, t=t: e.transpose(out=ps_tr[:, t, :], in_=MK[:, t * 128:(t + 1) * 128], identity=ident[:]), reads=[MK, ident], writes=[ps_tr])
            P.op("act", lambda e, ch=ch: e.activation(out=maskT[:, ch * 4:(ch + 1) * 4, :], in_=ps_tr[:], func=AF.Copy), reads=[ps_tr], writes=[maskT])
        for g in range(4):
            nst = NCH * 4
            tiles = [(ch, t) for ch in range(NCH) for t in range(4)]
            vts = {}

            def issue_qk(idx, g=g, QT=QT):
                nonlocal n_v
                ch, t = tiles[idx]; st_ = ch * 4 + t
                if t == 0:
                    VT = vt[n_v % 3]; n_v += 1
                    P.dma("sp", VT[:], vv[(ch // 8) * 4096:(ch // 8 + 1) * 4096, :].rearrange("(r n) c -> n r c", r=4)[(ch % 8) * 128:(ch % 8 + 1) * 128, :, g * 128:(g + 1) * 128], writes=[VT])
                    vts[ch] = VT
                pl = ps_pl[idx % 2]
                P.op("pe", lambda e, pl=pl, st_=st_: e.matmul(pl[:], lhsT=kt[:, g, st_ * 128:(st_ + 1) * 128], rhs=QT[:, g * 4:(g + 1) * 4, :], start=True, stop=True), reads=[kt, QT], writes=[pl])
                return pl
            nxt = issue_qk(0)
            for idx in range(len(tiles)):
                pl = nxt
                if idx + 1 < len(tiles):
                    nxt = issue_qk(idx + 1)
                ch, t = tiles[idx]; st_ = ch * 4 + t; VT = vts[ch]
                PE_ = pe_[n_pl % 3]; PM = pmk[n_pl % 3]; n_pl += 1
                P.op("act", lambda e, pl=pl, PE_=PE_: e.activation(out=PE_[:], in_=pl[:], func=AF.Exp, scale=SCALE), reads=[pl], writes=[PE_])
                P.op("pool", lambda e, PE_=PE_, PM=PM, st_=st_: e.tensor_tensor(out=PM[:].rearrange("p (h q) -> p h q", h=4), in0=PE_[:].rearrange("p (h q) -> p h q", h=4), in1=maskT[:, st_:st_ + 1, :].to_broadcast([128, 4, 128]), op=ALU.mult), reads=[PE_, maskT], writes=[PM])
                P.op("pe", lambda e, VT=VT, t=t, PM=PM, st_=st_, nst=nst: e.matmul(ps_o[:], lhsT=VT[:, t, :], rhs=PM[:], start=(st_ == 0), stop=(st_ == nst - 1)), reads=[VT, PM], writes=[ps_o])
                P.op("pe", lambda e, PM=PM, st_=st_, nst=nst: e.matmul(ps_s[:], lhsT=ones_b[:], rhs=PM[:], start=(st_ == 0), stop=(st_ == nst - 1)), reads=[ones_b, PM], writes=[ps_s])
            OB = ob[g % 2]
            P.op("dve", lambda e: e.reciprocal(out=rs[:], in_=ps_s[:]), reads=[ps_s], writes=[rs])
            P.op("dve", lambda e, OB=OB: e.tensor_tensor(out=OB[:], in0=ps_o[:], in1=rs[:], op=ALU.mult), reads=[ps_o, rs], writes=[OB])
            P.dma("act", oT[g * 512:(g + 1) * 512, j * 128:(j + 1) * 128].rearrange("(h d) q -> d h q", d=128), OB[:].rearrange("p (h q) -> p h q", h=4), reads=[OB])
    return


def host_B(Aout, nqt=NQT):
    maps = []
    ident = np.eye(128, dtype=np.float32).astype(BF)
    for core in range(8):
        b = core // 4; c = core % 4
        cores_b = [b * 4 + i for i in range(4)]
        qT_full = np.concatenate([Aout[i]["qT"] for i in cores_b], axis=1)
        kT_full = np.concatenate([Aout[i]["kT"] for i in cores_b], axis=1)
        v_full = np.concatenate([Aout[i]["v"] for i in cores_b], axis=0)
        ik_full = np.concatenate([Aout[i]["ikT"] for i in cores_b], axis=1)
        iq_full = np.concatenate([Aout[i]["iqT"] for i in cores_b], axis=1)
        iw_full = np.concatenate([Aout[i]["iw"] for i in cores_b], axis=0)
        sel = np.concatenate([np.arange((4 * j + c) * 128, (4 * j + c + 1) * 128) for j in range(16)])
        r = np.arange(128)[:, None]; t = np.arange(512)[None, :]
        cbias = np.where(t <= c * 128 + r, 0.0, -1e5).astype(np.float32)
        maps.append({"qTc": np.ascontiguousarray(qT_full[:, sel]), "kT": kT_full, "v": v_full, "ikT": ik_full,
                     "iqTc": np.ascontiguousarray(iq_full[:, sel]), "iwc": np.ascontiguousarray(iw_full[sel]),
                     "cbias": cbias, "ident": ident})
    return maps


def emit_O(P, io, K):
    KC = K // 128
    aT = io("aT", [K, 2048], BF16, "ExternalInput")
    resT = io("resT", [2048, 2048], F32, "ExternalInput")
    w = io("w", [K, 2048], F32, "ExternalInput")
    hT = io("hT", [2048, 2048], F32, "ExternalOutput")
    a = P.sb([128, KC, 2048], BF16)
    av = aT.rearrange("(c p) t -> p c t", p=128)
    for c0 in range(0, KC, 8):
        P.dma("sp", a[:, c0:c0 + 8, :], av[:, c0:c0 + 8, :], writes=[a])
    wb = [P.sb([128, KC, 256], BF16) for _ in range(2)]
    rs = [P.sb([128, 512], F32) for _ in range(3)]
    ob = [P.sb([128, 512], F32) for _ in range(3)]
    ps = [P.ps([128, 512]) for _ in range(3)]
    wv = w.rearrange("(c p) n -> p c n", p=128)
    n = 0
    for nc2 in range(8):
        WB = wb[nc2 % 2]
        P.dma("pool", WB[:], wv[:, :, nc2 * 256:(nc2 + 1) * 256], writes=[WB])
        for sub in range(2):
            r0 = nc2 * 256 + sub * 128
            for tt in range(4):
                ts = slice(tt * 512, (tt + 1) * 512)
                p_ = ps[n % 3]; R = rs[n % 3]; O = ob[n % 3]; n += 1
                P.dma("act", R[:], resT[r0:r0 + 128, ts], writes=[R])
                for c in range(KC):
                    P.op("pe", lambda e, c=c, p_=p_, WB=WB, sub=sub, ts=ts: e.matmul(p_[:], lhsT=WB[:, c, sub * 128:(sub + 1) * 128], rhs=a[:, c, ts], start=(c == 0), stop=(c == KC - 1)), reads=[WB, a], writes=[p_])
                P.op("dve", lambda e, p_=p_, R=R, O=O: e.tensor_tensor(out=O[:], in0=p_[:], in1=R[:], op=ALU.add), reads=[p_, R], writes=[O])
                P.dma("sp", hT[r0:r0 + 128, ts], O[:], reads=[O])
    return


def assemble_o(Bout):
    o_full = np.zeros((2, 2048, 8192), dtype=BF)
    for core in range(8):
        b = core // 4; c = core % 4
        for j in range(16):
            o_full[b][:, (4 * j + c) * 128:(4 * j + c + 1) * 128] = Bout[core][:, j * 128:(j + 1) * 128]
    return o_full


EPS = 1e-6; D = 2048; KC = 16; TG = 256; NE = 16384; EC = 256

def emit_P(P, io, want_hn, add=False, ngroups=8, nec=NE // EC, dbg=False):
    NE = nec * EC if dbg else 16384
    DBG = {}
    def dump(name, t, ap, shape):
        if not dbg or name in DBG: return
        DBG[name] = io('dbg_' + name, shape, F32, 'ExternalOutput')
        P.dma('sp', DBG[name], ap, reads=[t])
    hT = io("hT", [D, 2048], F32, "ExternalInput")
    gn = io("gn", [128, 3, KC], F32, "ExternalInput")
    w_q = io("w_q", [D, D], F32, "ExternalInput")
    keysT = io("keysT", [128, 16, 128], F32, "ExternalInput")
    uT = io("uT", [D, NE], F32, "ExternalInput")
    vt = io("vt", [NE, D], F32, "ExternalInput")
    wg = io("wg", [D, D], F32, "ExternalInput")
    wpi = io("wpi", [256, D], F32, "ExternalInput")
    pT = io("pT", [256, 2048], F32, "ExternalInput")
    identd = io("ident", [128, 128], BF16, "ExternalInput")
    h3T = io("h3T", [D, 2048], F32, "ExternalOutput")
    mixT = io("mixT", [D, 2048], F32, "ExternalInput") if add else None
    hnT = io("hnT", [D, 2048], BF16, "ExternalOutput") if want_hn else None

    G = P.sb([128, 3, KC], F32); KT = P.sb([128, 16, 128], F32); ident = P.sb([128, 128], BF16)
    WPI = P.sb([128, 2, D], BF16); ones_b = P.sb([128, 128], BF16); epsb = P.sb([128, 1], F32)
    P.dma("sp", G[:], gn[:, :, :], writes=[G]); P.dma("sp", KT[:], keysT[:, :, :], writes=[KT]); P.dma("sp", ident[:], identd[:, :], writes=[ident])
    P.dma("pool", WPI[:], wpi.rearrange("(c p) n -> p c n", p=128), writes=[WPI])
    P.op("dve", lambda e: e.memset(ones_b[:], 1.0), writes=[ones_b])
    P.op("dve", lambda e: e.memset(epsb[:], EPS), writes=[epsb])

    H = P.sb([128, KC, TG], F32); XY = P.sb([128, KC, TG], F32); QP = P.sb([128, 16, TG], F32); NB = P.sb([128, KC, TG], BF16)
    rstd = P.sb([128, TG], F32)
    S = [P.sb([128, 16, 128], F32) for _ in range(2)]
    Cc = [P.sb([128, 8, 128], F32) for _ in range(2)]
    W1 = [P.sb([128, 8, 128], F32) for _ in range(2)]
    E2 = [P.sb([128, 8, 128], F32) for _ in range(2)]
    t16 = P.sb([128, 16, 16], F32); wk = P.sb([128, 256], F32); cand = P.sb([128, 8, 256], F32); f16 = P.sb([128, 8, 16], F32)
    thr = P.sb([128, 8], F32); fmax = P.sb([128, 8], F32); ex = P.sb([128, 8, 16], F32); Z = P.sb([128, 8], F32); rZ = P.sb([128, 8], F32)
    ub = [P.sb([128, KC, EC], BF16) for _ in range(2)]
    vb = [P.sb([128, EC // 128, D], BF16) for _ in range(2)]
    gel = [P.sb([128, EC], F32) for _ in range(2)]
    NI = EC // 128
    AA = [P.sb([128, 8, NI, 128], F32) for _ in range(2)]
    Gb = [[P.sb([128, NI, 128], F32) for _ in range(2)] for _ in range(2)]
    identf = P.sb([128, 128], F32)
    P.op("dve", lambda e: e.tensor_copy(out=identf[:], in_=ident[:]), reads=[ident], writes=[identf])
    coef = [P.sb([128, EC], BF16) for _ in range(2)]
    coefT = [P.sb([128, NI, TG], BF16) for _ in range(2)]
    wq = P.sb([128, KC, 128], F32)
    wgb = [P.sb([128, KC, 128], BF16) for _ in range(2)]
    PT = P.sb([128, 2, TG], BF16)
    gate = [P.sb([128, TG], F32) for _ in range(2)]; gtmp = [P.sb([128, TG], F32) for _ in range(2)]
    HN = NB if want_hn else None
    ps_ms = P.ps([128, 512]); ps_a = P.ps([128, 512]); ps_b = P.ps([128, 512]); ps_tr = P.ps([128, 2, NI, 128], BF16)
    ps4 = [P.ps([128, 512]) for _ in range(4)]
    hv = hT.rearrange("(c p) t -> p c t", p=128)
    h3v = h3T.rearrange("(c p) t -> p c t", p=128)
    hnv = hnT.rearrange("(c p) t -> p c t", p=128) if want_hn else None
    wqv = w_q.rearrange("(c p) n -> p c n", p=128)
    wgv = wg.rearrange("(c p) n -> p c n", p=128)
    uv = uT.rearrange("(c p) e -> p c e", p=128)
    ptv = pT.rearrange("(c p) t -> p c t", p=128)

    def rmsnorm(src, which, out32, outb):
        P.op("act", lambda e: e.activation(out=outb[:], in_=src[:], func=AF.Square), reads=[src], writes=[outb])
        for c in range(KC):
            P.op("pe", lambda e, c=c: e.matmul(ps_ms[:, 0:TG], lhsT=ones_b[:], rhs=outb[:, c, :], start=(c == 0), stop=(c == KC - 1)), reads=[ones_b, outb], writes=[ps_ms])
        P.op("act", lambda e: e.activation(out=rstd[:], in_=ps_ms[:, 0:TG], func=AF.Sqrt, bias=epsb[:, 0:1], scale=1.0 / D), reads=[ps_ms, epsb], writes=[rstd])
        P.op("dve", lambda e: e.reciprocal(out=rstd[:], in_=rstd[:]), reads=[rstd], writes=[rstd])
        for c in range(KC):
            if out32 is not None:
                P.op("dve", lambda e, c=c: e.scalar_tensor_tensor(out=out32[:, c, :], in0=src[:, c, :], scalar=G[:, which, c:c + 1], in1=rstd[:], op0=ALU.mult, op1=ALU.mult), reads=[src, G, rstd], writes=[out32])
            else:
                P.op("dve", lambda e, c=c: e.scalar_tensor_tensor(out=outb[:, c, :], in0=src[:, c, :], scalar=G[:, which, c:c + 1], in1=rstd[:], op0=ALU.mult, op1=ALU.mult), reads=[src, G, rstd], writes=[outb])
        if out32 is not None:
            P.op("act", lambda e: e.activation(out=outb[:], in_=out32[:], func=AF.Copy), reads=[out32], writes=[outb])

    n_w = 0
    for gi in range(ngroups):
        gs = slice(gi * TG, (gi + 1) * TG)
        P.dma("sp", H[:], hv[:, :, gs], writes=[H])
        P.dma("pool", PT[:], ptv[:, :, gs], writes=[PT])
        if add:
            P.dma("act", QP[:], mixT.rearrange("(c p) t -> p c t", p=128)[:, :, gs], writes=[QP])
            P.op("dve", lambda e: e.tensor_tensor(out=H[:], in0=H[:], in1=QP[:], op=ALU.add), reads=[H, QP], writes=[H])
        rmsnorm(H, 0, XY, NB)
        for n in range(16):
            P.dma("sp", wq[:], wqv[:, :, n * 128:(n + 1) * 128], writes=[wq])
            pq = ps_a if n % 2 == 0 else ps_b
            for c in range(KC):
                P.op("pe", lambda e, c=c, pq=pq: e.matmul(pq[:, 0:TG], lhsT=wq[:, c, :], rhs=XY[:, c, :], start=(c == 0), stop=(c == KC - 1)), reads=[wq, XY], writes=[pq])
            P.op("act", lambda e, n=n, pq=pq: e.activation(out=QP[:, n, :], in_=pq[:, 0:TG], func=AF.Copy), reads=[pq], writes=[QP])
        for tl in range(2):
            tsl = slice(tl * 128, (tl + 1) * 128)
            St = S[tl]; Ct = Cc[tl]; W1t = W1[tl]; E2t = E2[tl]
            for hp in range(16):
                pb = ps4[hp // 4]
                P.op("pe", lambda e, hp=hp, pb=pb, tsl=tsl: e.matmul(pb[:, (hp % 4) * 128:(hp % 4 + 1) * 128], lhsT=QP[:, hp, tsl], rhs=KT[:, hp, :], start=True, stop=True), reads=[QP, KT], writes=[pb])
            for q4 in range(4):
                P.op("act", lambda e, q4=q4, St=St: e.activation(out=St[:, q4 * 4:(q4 + 1) * 4, :], in_=ps4[q4][:].rearrange("p (a b) -> p a b", a=4), func=AF.Copy), reads=[ps4[q4]], writes=[St])
            for hp in range(16):
                P.op("dve", lambda e, hp=hp, St=St: e.max(out=t16[:, hp, 0:8], in_=St[:, hp, :]), reads=[St], writes=[t16])
                P.op("dve", lambda e, hp=hp, St=St: e.match_replace(out=wk[:, 0:128], in_to_replace=t16[:, hp, 0:8], in_values=St[:, hp, :], imm_value=-1e30), reads=[St, t16], writes=[wk])
                P.op("dve", lambda e, hp=hp: e.max(out=t16[:, hp, 8:16], in_=wk[:, 0:128]), reads=[wk], writes=[t16])
            for h in range(8):
                P.op("dve", lambda e, h=h: e.tensor_tensor(out=cand[:, h, :].rearrange("p (a b) -> p a b", a=16), in0=t16[:, 2 * h, :].unsqueeze(2).to_broadcast([128, 16, 16]), in1=t16[:, 2 * h + 1, :].unsqueeze(1).to_broadcast([128, 16, 16]), op=ALU.add), reads=[t16], writes=[cand])
            for h in range(8):
                P.op("dve", lambda e, h=h: e.max(out=f16[:, h, 0:8], in_=cand[:, h, :]), reads=[cand], writes=[f16])
                P.op("dve", lambda e, h=h: e.match_replace(out=wk[:], in_to_replace=f16[:, h, 0:8], in_values=cand[:, h, :], imm_value=-1e30), reads=[cand, f16], writes=[wk])
                P.op("dve", lambda e, h=h: e.max(out=f16[:, h, 8:16], in_=wk[:]), reads=[wk], writes=[f16])
            P.op("dve", lambda e: e.tensor_reduce(out=thr[:], in_=f16[:], axis=AX.X, op=ALU.min), reads=[f16], writes=[thr])
            P.op("dve", lambda e: e.tensor_scalar(out=thr[:], in0=thr[:], scalar1=-2e-5, scalar2=None, op0=ALU.add), reads=[thr], writes=[thr])
            t16v = t16[:].rearrange("p (h two) k -> p h two k", two=2)
            P.op("dve", lambda e, t16v=t16v: e.tensor_tensor(out=fmax[:], in0=t16v[:, :, 0, 0], in1=t16v[:, :, 1, 0], op=ALU.add), reads=[t16], writes=[fmax])
            P.op("dve", lambda e: e.tensor_tensor(out=ex[:], in0=f16[:], in1=fmax[:].unsqueeze(2).to_broadcast([128, 8, 16]), op=ALU.subtract), reads=[f16, fmax], writes=[ex])
            P.op("act", lambda e: e.activation(out=ex[:], in_=ex[:], func=AF.Exp), reads=[ex], writes=[ex])
            P.op("dve", lambda e: e.tensor_reduce(out=Z[:], in_=ex[:], axis=AX.X, op=ALU.add), reads=[ex], writes=[Z])
            P.op("dve", lambda e: e.reciprocal(out=rZ[:], in_=Z[:]), reads=[Z], writes=[rZ])
            S4 = St[:].rearrange("p (h two) k -> p h two k", two=2)
            P.op("dve", lambda e, S4=S4, Ct=Ct: e.tensor_tensor(out=Ct[:], in0=thr[:].unsqueeze(2).to_broadcast([128, 8, 128]), in1=S4[:, :, 0, :], op=ALU.subtract), reads=[thr, St], writes=[Ct])
            P.op("dve", lambda e, S4=S4, W1t=W1t, t16v=t16v: e.tensor_tensor(out=W1t[:], in0=S4[:, :, 0, :], in1=t16v[:, :, 0, 0:1].to_broadcast([128, 8, 128]), op=ALU.subtract), reads=[St, t16], writes=[W1t])
            P.op("act", lambda e, W1t=W1t: e.activation(out=W1t[:], in_=W1t[:], func=AF.Exp), reads=[W1t], writes=[W1t])
            P.op("dve", lambda e, W1t=W1t: e.tensor_tensor(out=W1t[:], in0=W1t[:], in1=rZ[:].unsqueeze(2).to_broadcast([128, 8, 128]), op=ALU.mult), reads=[W1t, rZ], writes=[W1t])
            P.op("dve", lambda e, S4=S4, E2t=E2t, t16v=t16v: e.tensor_tensor(out=E2t[:], in0=S4[:, :, 1, :], in1=t16v[:, :, 1, 0:1].to_broadcast([128, 8, 128]), op=ALU.subtract), reads=[St, t16], writes=[E2t])
            P.op("act", lambda e, E2t=E2t: e.activation(out=E2t[:], in_=E2t[:], func=AF.Exp), reads=[E2t], writes=[E2t])
            if tl == 0 and gi == 0:
                dump('S', St, St[:], [128, 16, 128]); dump('t16', t16, t16[:], [128, 16, 16]); dump('f16', f16, f16[:], [128, 8, 16]); dump('thr', thr, thr[:], [128, 8]); dump('fmax', fmax, fmax[:], [128, 8]); dump('rZ', rZ, rZ[:], [128, 8])
                dump('C', Ct, Ct[:], [128, 8, 128]); dump('W1', W1t, W1t[:], [128, 8, 128]); dump('E2', E2t, E2t[:], [128, 8, 128]); dump('cand', cand, cand[:], [128, 8, 256])
        def g1(ec):
            i0 = ec * NI
            for tl in range(2):
                St = S[tl]; Ct = Cc[tl]
                S4 = St[:].rearrange("p (h two) k -> p h two k", two=2)
                AAt = AA[tl]
                P.op("dve", lambda e, AAt=AAt, S4=S4, Ct=Ct, i0=i0: e.tensor_tensor(out=AAt[:], in0=S4[:, :, 1, :].unsqueeze(2).to_broadcast([128, 8, NI, 128]), in1=Ct[:, :, i0:i0 + NI].unsqueeze(3).to_broadcast([128, 8, NI, 128]), op=ALU.is_ge), reads=[St, Ct], writes=[AAt])
            for tl in range(2):
                W1t = W1[tl]; E2t = E2[tl]; AAt = AA[tl]
                P.op("dve", lambda e, AAt=AAt, E2t=E2t: e.tensor_tensor(out=AAt[:], in0=AAt[:], in1=E2t[:].unsqueeze(2).to_broadcast([128, 8, NI, 128]), op=ALU.mult), reads=[AAt, E2t], writes=[AAt])
                P.op("dve", lambda e, AAt=AAt, W1t=W1t, i0=i0: e.tensor_tensor(out=AAt[:], in0=AAt[:], in1=W1t[:, :, i0:i0 + NI].unsqueeze(3).to_broadcast([128, 8, NI, 128]), op=ALU.mult), reads=[AAt, W1t], writes=[AAt])

        def g2(par):
            for tl in range(2):
                AAt = AA[tl]; GB = Gb[par][tl]
                P.op("dve", lambda e, AAt=AAt, GB=GB: e.tensor_reduce(out=GB[:], in_=AAt[:].rearrange("p h i j -> p i j h"), axis=AX.X, op=ALU.add), reads=[AAt], writes=[GB])
        g1(0); g2(0)
        Yv = XY[:].rearrange("p c t -> p (c t)").rearrange("p (tl d) -> p tl d", tl=2)

        def wload(ec):
            P.dma("pool", ub[ec % 2][:], uv[:, :, ec * EC:(ec + 1) * EC], writes=[ub[ec % 2]])
            P.dma("pool", vb[ec % 2][:], vt[ec * EC:(ec + 1) * EC, :].rearrange("(b p) d -> p b d", p=128), writes=[vb[ec % 2]])
        wload(0)
        for ec in range(nec):
            par = ec % 2
            UB = ub[par]; VB = vb[par]; CT = coefT[par]
            if ec + 1 < nec:
                wload(ec + 1)
                g1(ec + 1)
            for tl in range(2):
                tsl = slice(tl * 128, (tl + 1) * 128)
                pa = ps4[tl]
                for c in range(KC):
                    P.op("pe", lambda e, c=c, pa=pa, UB=UB, tsl=tsl: e.matmul(pa[:, 0:EC], lhsT=NB[:, c, tsl], rhs=UB[:, c, :], start=(c == 0), stop=(c == KC - 1)), reads=[NB, UB], writes=[pa])
            for tl in range(2):
                pa = ps4[tl]; GL = gel[tl]
                P.op("act", lambda e, pa=pa, GL=GL: e.activation(out=GL[:], in_=pa[:, 0:EC], func=AF.Gelu), reads=[pa], writes=[GL])
            for tl in range(2):
                GL = gel[tl]; GB = Gb[par][tl]; CF = coef[tl]
                P.op("dve", lambda e, GL=GL, GB=GB, CF=CF: e.tensor_tensor(out=CF[:], in0=GL[:], in1=GB[:].rearrange("p a b -> p (a b)"), op=ALU.mult), reads=[GL, GB], writes=[CF])
                if tl == 0 and gi == 0 and ec == 0:
                    dump('GL', GL, GL[:], [128, EC]); dump('GB', GB, GB[:], [128, NI, 128])
            for tl in range(2):
                CF = coef[tl]
                for b in range(NI):
                    P.op("pe", lambda e, b=b, CF=CF, tl=tl: e.transpose(out=ps_tr[:, tl, b, :], in_=CF[:, b * 128:(b + 1) * 128], identity=ident[:]), reads=[CF, ident], writes=[ps_tr])
            P.op("act", lambda e, CT=CT: e.activation(out=CT[:].rearrange("p b (tl t) -> p tl b t", tl=2), in_=ps_tr[:], func=AF.Copy), reads=[ps_tr], writes=[CT])
            k_ = 0
            for tl in range(2):
                tsl = slice(tl * 128, (tl + 1) * 128)
                for d4 in range(4):
                    py = ps4[2 + k_ % 2]; k_ += 1
                    for b in range(NI):
                        P.op("pe", lambda e, b=b, d4=d4, py=py, VB=VB, CT=CT, tsl=tsl: e.matmul(py[:], lhsT=CT[:, b, tsl], rhs=VB[:, b, d4 * 512:(d4 + 1) * 512], start=(b == 0), stop=(b == NI - 1)), reads=[VB, CT], writes=[py])
                    ysl = Yv[:, tl, d4 * 512:(d4 + 1) * 512]
                    if ec == 0:
                        P.op("act", lambda e, py=py, ysl=ysl: e.activation(out=ysl, in_=py[:], func=AF.Copy), reads=[py], writes=[XY])
                    else:
                        P.op("dve", lambda e, py=py, ysl=ysl: e.tensor_tensor(out=ysl, in0=ysl, in1=py[:], op=ALU.add), reads=[py, XY], writes=[XY])
            if ec + 1 < nec:
                g2(1 - par)
        if gi == 0:
            dump('Y', XY, XY[:], [128, KC, TG])
        k_ = 0
        for tl in range(2):
            tsl = slice(tl * 128, (tl + 1) * 128)
            for dc4 in range(4):
                py = ps4[k_ % 4]; k_ += 1
                for q4 in range(4):
                    dc = dc4 * 4 + q4
                    P.op("pe", lambda e, py=py, q4=q4, dc=dc, tl=tl: e.transpose(out=py[:, q4 * 128:(q4 + 1) * 128], in_=Yv[:, tl, dc * 128:(dc + 1) * 128], identity=identf[:]), reads=[XY, identf], writes=[py])
                P.op("dve", lambda e, py=py, dc4=dc4, tsl=tsl: e.tensor_tensor(out=H[:, dc4 * 4:(dc4 + 1) * 4, tsl], in0=H[:, dc4 * 4:(dc4 + 1) * 4, tsl], in1=py[:].rearrange("p (a b) -> p a b", a=4), op=ALU.add), reads=[py, H], writes=[H])
        rmsnorm(H, 1, None, NB)
        for n in range(16):
            WG = wgb[n_w % 2]; GA = gate[n_w % 2]; GT = gtmp[n_w % 2]; n_w += 1
            P.dma("pool", WG[:], wgv[:, :, n * 128:(n + 1) * 128], writes=[WG])
            pg = ps_a if n % 2 == 0 else ps_b
            for c in range(KC):
                P.op("pe", lambda e, c=c, pg=pg, WG=WG: e.matmul(pg[:, 0:TG], lhsT=WG[:, c, :], rhs=NB[:, c, :], start=(c == 0), stop=(c == KC - 1)), reads=[WG, NB], writes=[pg])
            for c in range(2):
                P.op("pe", lambda e, c=c, pg=pg, n=n: e.matmul(pg[:, TG:2 * TG], lhsT=WPI[:, c, n * 128:(n + 1) * 128], rhs=PT[:, c, :], start=(c == 0), stop=(c == 1)), reads=[WPI, PT], writes=[pg])
            P.op("act", lambda e, pg=pg, GA=GA: e.activation(out=GA[:], in_=pg[:, 0:TG], func=AF.Sigmoid), reads=[pg], writes=[GA])
            P.op("dve", lambda e, pg=pg, GA=GA, GT=GT: e.tensor_tensor(out=GT[:], in0=GA[:], in1=pg[:, TG:2 * TG], op=ALU.mult), reads=[pg, GA], writes=[GT])
            P.op("pool", lambda e, n=n, GT=GT: e.tensor_tensor(out=H[:, n, :], in0=H[:, n, :], in1=GT[:], op=ALU.add), reads=[H, GT], writes=[H])
        P.dma("sp", h3v[:, :, gs], H[:], reads=[H])
        if want_hn:
            rmsnorm(H, 2, None, HN)
            P.dma("sp", hnv[:, :, gs], HN[:], reads=[HN])
    return


def host_P(hT_list, layer, z, want_next):
    maps = []
    ident = np.eye(128, dtype=np.float32).astype(BF)
    gnext = z['norm_mix'][layer + 1] if want_next else np.ones(D, np.float32)
    gn = np.ascontiguousarray(np.stack([z['norm_ffn'][layer].reshape(KC, 128).T, z['norm_ple'][layer].reshape(KC, 128).T, gnext.reshape(KC, 128).T], axis=1))
    keysT = np.ascontiguousarray(z['peer_keys'][layer].reshape(16, 128, 128).transpose(2, 0, 1))
    uT = np.ascontiguousarray(z['peer_u'][layer].T)
    for core in range(8):
        b = core // 4; t0 = (core % 4) * 2048
        maps.append({"hT": hT_list[core], "gn": gn, "w_q": z['peer_w_q'][layer], "keysT": keysT, "uT": uT, "vt": z['peer_v'][layer],
                     "wg": z['ple_w_gate'][layer], "wpi": z['ple_w_in'][layer], "pT": np.ascontiguousarray(z['p'][layer, b, t0:t0 + 2048].T), "ident": ident})
    return maps


EPS = 1e-6; KC = 16; ST = 256; NW = 3088

def emit_D(P, io, nst=32, maxphase=9):
    hnT = io("hnT", [8192, 2048], BF16, "ExternalInput")
    wc = io("wc", [2048, NW], F32, "ExternalInput")
    convw = io("convw", [128, 16, 4], F32, "ExternalInput")
    hc = io("hc", [128, 2, 8], F32, "ExternalInput")
    gnrow = io("gnrow", [128, 128], F32, "ExternalInput")
    masks = io("masks", [128, 6, 128], F32, "ExternalInput")
    og = io("og", [1024, 8192], BF16, "ExternalOutput")

    W = P.sb([128, KC, NW], BF16)
    wv = wc.rearrange("(c p) n -> p c n", p=128)
    for c0 in range(0, KC, 4):
        for n0 in range(0, NW, 512):
            n1 = min(NW, n0 + 512)
            P.dma("pool", W[:, c0:c0 + 4, n0:n1], wv[:, c0:c0 + 4, n0:n1], writes=[W])
    CW = P.sb([128, 16, 4], F32); HC = P.sb([128, 2, 8], F32); GN = P.sb([128, 128], F32); MK = P.sb([128, 6, 128], F32)
    P.dma("sp", CW[:], convw[:, :, :], writes=[CW]); P.dma("sp", HC[:], hc[:, :, :], writes=[HC]); P.dma("sp", GN[:], gnrow[:, :], writes=[GN]); P.dma("sp", MK[:], masks[:, :, :], writes=[MK])
    ident = MK[:, 0, :]; Lm = MK[:, 1, :]; Bo = MK[:, 2, :]; Um = MK[:, 3, :]; NUs = MK[:, 4, :]; cmask = MK[:, 5, 0:2]
    ones_f = P.sb([128, 128], F32); epsb = P.sb([128, 1], F32); nega = P.sb([128, 8], F32); one1 = P.sb([128, 1], F32)
    P.op("dve", lambda e: e.memset(ones_f[:], 1.0), writes=[ones_f])
    P.op("dve", lambda e: e.memset(epsb[:], EPS), writes=[epsb])
    P.op("dve", lambda e: e.memset(one1[:], 1.0), writes=[one1])
    P.op("act", lambda e: e.activation(out=nega[:], in_=HC[:, 0, :], func=AF.Exp), reads=[HC], writes=[nega])
    P.op("dve", lambda e: e.tensor_scalar(out=nega[:], in0=nega[:], scalar1=-1.0, scalar2=None, op0=ALU.mult), reads=[nega], writes=[nega])

    HN = [P.sb([128, KC, ST], BF16) for _ in range(1)]
    PJ = [P.sb([128, ST + 3], F32) for _ in range(2)]
    HALO = P.sb([128, 16, 3], F32)
    CO = [P.sb([128, ST], F32) for _ in range(16)]
    P.op("pool", lambda e: e.memset(HALO[:], 0.0), writes=[HALO])
    sqt = [P.sb([128, ST], F32) for _ in range(1)]; rst = [P.sb([128, ST], F32) for _ in range(1)]
    ZS = [P.sb([128, 1024], F32) for _ in range(1)]
    BA = [P.sb([128, 16], F32) for _ in range(2)]; BETA = [P.sb([128, 8], F32) for _ in range(2)]; GG = [P.sb([128, 8], F32) for _ in range(2)]
    GC = P.sb([128, 8], F32); GL = P.sb([128, 8], F32); gsel = P.sb([128, 8, 2], F32); EGL = P.sb([128, 16], F32)
    EGC = P.sb([128, 8], F32); BG = P.sb([128, 8], F32); KD = P.sb([128, 8], F32)
    KTM = P.sb([128, 4, 128], F32); VTM = P.sb([128, 8, 128], F32)
    KK = [P.sb([128, 128], F32) for _ in range(4)]; QK = [P.sb([128, 128], F32) for _ in range(4)]
    def mk2(n=2): return [P.sb([128, 128], F32) for _ in range(n)]
    DGB = [P.sb([128, 256], F32) for _ in range(2)]
    DTt = mk2(); DT = mk2(); EGR = mk2(); BR = mk2(); T1 = mk2(); T2 = T1
    XA = mk2(4); XAT = mk2(4); XB = mk2(4); XBT = mk2(4); RR = mk2(4)
    VB = mk2(4); KBG = mk2(4)
    QKm = mk2(8); Usb = mk2(8); WT = mk2(8); QG = mk2(8); KG = mk2(8); VN = mk2(8)
    Sst = mk2(8)
    for h in range(8):
        P.op("pool", lambda e, h=h: e.memset(Sst[h][:], 0.0), writes=[Sst[h]])
    Osb = P.sb([128, 8, 128], F32);  MS = P.sb([128, 8], F32); O2 = P.sb([128, 8, 128], F32); SQo = O2
    OB = [P.sb([128, 1024], BF16) for _ in range(1)]
    OT = P.sb([128, 8, 128], BF16); identb = P.sb([128, 128], BF16)
    P.op("dve", lambda e: e.tensor_copy(out=identb[:], in_=ident), reads=[MK], writes=[identb])
    bankT = [P.ps([128, 512]) for _ in range(8)]
    pjps = [TV(bankT[0], bankT[0][:, 0:256], "pjA"), TV(bankT[1], bankT[1][:, 0:256], "pjB")]
    zps = bankT[2]
    smalls = [TV(bankT[3], bankT[3][:, i * 128:(i + 1) * 128], f"sm{i}") for i in range(4)]
    rowsps = [TV(bankT[4], bankT[4][:, 0:256], "rowsA"), TV(bankT[5], bankT[5][:, 0:256], "rowsB")]
    rotl = [TV(bankT[6 + i % 2], bankT[6 + i % 2][:, (i // 2) * 128:(i // 2 + 1) * 128], f"rot{i}") for i in range(8)]
    rc = [0]; sc = [0]
    def rot():
        t = rotl[rc[0] % 8]; rc[0] += 1; return t
    def sm():
        t = smalls[sc[0] % 4]; sc[0] += 1; return t

    def mm(o, oap, lhsT, rhs, reads, start=True, stop=True):
        P.op("pe", lambda e: e.matmul(oap, lhsT=lhsT, rhs=rhs, start=start, stop=stop), reads=reads, writes=[o])
    def tp(o, oap, in_, reads):
        P.op("pe", lambda e: e.transpose(out=oap, in_=in_, identity=ident), reads=reads + [MK], writes=[o])
    def cp(eng, o, oap, iap, reads):
        if eng == "act":
            P.op("act", lambda e: e.activation(out=oap, in_=iap, func=AF.Copy), reads=reads, writes=[o])
        else:
            P.op(eng, lambda e: e.tensor_copy(out=oap, in_=iap), reads=reads, writes=[o])
    def act(o, oap, iap, func, reads, bias=None, scale=None):
        kw = {}
        if bias is not None: kw["bias"] = bias
        if scale is not None: kw["scale"] = scale
        P.op("act", lambda e: e.activation(out=oap, in_=iap, func=func, **kw), reads=reads, writes=[o])
    def tt(eng, o, oap, a, b, op, reads):
        P.op(eng, lambda e: e.tensor_tensor(out=oap, in0=a, in1=b, op=op), reads=reads, writes=[o])
    def ts(eng, o, oap, a, s1, s2, op0, op1, reads):
        if op1 is None:
            P.op(eng, lambda e: e.tensor_scalar(out=oap, in0=a, scalar1=s1, scalar2=None, op0=op0), reads=reads, writes=[o])
        else:
            P.op(eng, lambda e: e.tensor_scalar(out=oap, in0=a, scalar1=s1, scalar2=s2, op0=op0, op1=op1), reads=reads, writes=[o])
    def stt(eng, o, oap, a, s, b, op0, op1, reads):
        P.op(eng, lambda e: e.scalar_tensor_tensor(out=oap, in0=a, scalar=s, in1=b, op0=op0, op1=op1), reads=reads, writes=[o])

    for st in range(nst):
        H = HN[0]
        for tl_ in range(2):
            tile_ = st * 2 + tl_; j_ = tile_ // 4; r_ = tile_ % 4
            for k_ in range(8):
                P.dma("sp", H[:, 2 * k_:2 * k_ + 2, tl_ * 128:(tl_ + 1) * 128], hnT[k_ * 1024 + r_ * 256:k_ * 1024 + (r_ + 1) * 256, j_ * 128:(j_ + 1) * 128].rearrange("(two p) t -> p two t", p=128), writes=[H])
        for ch in range(16):
            pj = pjps[ch % 2]; pjt = PJ[ch % 2]; co = CO[ch]
            for c in range(KC):
                mm(pj, pj[:], W[:, c, ch * 128:(ch + 1) * 128], H[:, c, :], [W, H], start=(c == 0), stop=(c == KC - 1))
            cp("pool", pjt, pjt[:, 0:3], HALO[:, ch, :], [HALO])
            cp("act", pjt, pjt[:, 3:ST + 3], pj[:], [pj])
            ts("dve", co, co[:], pjt[:, 3:ST + 3], CW[:, ch, 3:4], None, ALU.mult, None, [pjt, CW])
            for j in (2, 1, 0):
                stt("dve", co, co[:], pjt[:, j:j + ST], CW[:, ch, j:j + 1], co[:], ALU.mult, ALU.add, [pjt, CW, co])
            cp("pool", HALO, HALO[:, ch, :], pjt[:, ST:ST + 3], [pjt])
            act(co, co[:], co[:], AF.Silu, [co])
            if ch < 8:
                sq = sqt[0]; rs = rst[0]; ss = rowsps[ch % 2]
                act(sq, sq[:], co[:], AF.Square, [co])
                mm(ss, ss[:], ones_f[:], sq[:], [ones_f, sq])
                act(rs, rs[:], ss[:], AF.Sqrt, [ss, epsb], bias=epsb[:, 0:1], scale=1.0)
                P.op("dve", lambda e, rs=rs: e.reciprocal(out=rs[:], in_=rs[:]), reads=[rs], writes=[rs])
                if ch < 4:
                    stt("dve", co, co[:], co[:], 128 ** -0.5, rs[:], ALU.mult, ALU.mult, [co, rs])
                else:
                    tt("dve", co, co[:], co[:], rs[:], ALU.mult, [co, rs])
        if maxphase < 2: continue
        for tl in range(2):
            cs = slice(tl * 128, (tl + 1) * 128)
            q = sm()
            for c in range(KC):
                mm(q, q[:, 0:16], H[:, c, cs], W[:, c, 3072:3088], [W, H], start=(c == 0), stop=(c == KC - 1))
            cp("act", BA[tl], BA[tl][:], q[:, 0:16], [q])
            act(BETA[tl], BETA[tl][:], BA[tl][:, 0:8], AF.Sigmoid, [BA[tl]])
            tt("dve", GG[tl], GG[tl][:], BA[tl][:, 8:16], HC[:, 1, :], ALU.add, [BA[tl], HC])
            act(GG[tl], GG[tl][:], GG[tl][:], AF.Exp, [GG[tl]])
            act(GG[tl], GG[tl][:], GG[tl][:], AF.Ln, [GG[tl], one1], bias=one1[:, 0:1], scale=1.0)
            tt("dve", GG[tl], GG[tl][:], GG[tl][:], nega[:], ALU.mult, [GG[tl], nega])
        if maxphase < 3: continue
        for tl in range(2):
            cs = slice(tl * 128, (tl + 1) * 128)
            g = GG[tl]; beta = BETA[tl]
            q = sm(); mm(q, q[:, 0:8], Lm, g[:], [MK, g]); cp("act", GC, GC[:], q[:, 0:8], [q])
            q = sm(); mm(q, q[:, 0:8], Bo, g[:], [MK, g]); cp("act", GL, GL[:], q[:, 0:8], [q])
            tt("dve", gsel, gsel[:], g[:].unsqueeze(2).to_broadcast([128, 8, 2]), cmask.unsqueeze(1).to_broadcast([128, 8, 2]), ALU.mult, [g, MK])
            q = sm(); mm(q, q[:, 0:16], ones_f[:], gsel[:].rearrange("p a b -> p (a b)"), [ones_f, gsel]); act(EGL, EGL[:], q[:, 0:16], AF.Exp, [q])
            act(EGC, EGC[:], GC[:], AF.Exp, [GC])
            tt("dve", BG, BG[:], beta[:], EGC[:], ALU.mult, [beta, EGC])
            tt("dve", KD, KD[:], GL[:], GC[:], ALU.subtract, [GL, GC])
            act(KD, KD[:], KD[:], AF.Exp, [KD])
            if maxphase < 4: continue
            for hq in range(4):
                r = rot(); tp(r, r[:], CO[4 + hq][:, cs], [CO[4 + hq]]); cp("act", KTM, KTM[:, hq, :], r[:], [r])
            for hv_ in range(8):
                r = rot(); tp(r, r[:], CO[8 + hv_][:, cs], [CO[8 + hv_]]); cp("dve" if hv_ % 2 else "act", VTM, VTM[:, hv_, :], r[:], [r])
            for hq in range(4):
                r = rot(); mm(r, r[:], CO[4 + hq][:, cs], CO[4 + hq][:, cs], [CO[4 + hq]]); cp("act", KK[hq], KK[hq][:], r[:], [r])
                r = rot(); mm(r, r[:], CO[4 + hq][:, cs], CO[hq][:, cs], [CO[4 + hq], CO[hq]]); cp("dve", QK[hq], QK[hq][:], r[:], [r])
            if maxphase < 5: continue
            for hb in range(2):
                heads = list(range(4 * hb, 4 * hb + 4))
                for h in heads:
                    hq = h // 2; par = h % 2; b4 = h % 4
                    dgb = DGB[par]; rows = rowsps[par]
                    ts("dve", dgb, dgb[:, 0:128], ident, GC[:, h:h + 1], None, ALU.mult, None, [MK, GC])
                    ts("dve", dgb, dgb[:, 128:256], ident, beta[:, h:h + 1], None, ALU.mult, None, [MK, beta])
                    mm(rows, rows[:], ones_f[:], dgb[:], [ones_f, dgb])
                    ts("dve", DTt[par], DTt[par][:], rows[:, 0:128], GC[:, h:h + 1], 0.0, ALU.subtract, ALU.min, [rows, GC])
                    act(DTt[par], DTt[par][:], DTt[par][:], AF.Exp, [DTt[par]])
                    tt("pool", DT[par], DT[par][:], DTt[par][:], Um, ALU.mult, [DTt[par], MK])
                    act(EGR[par], EGR[par][:], rows[:, 0:128], AF.Exp, [rows])
                    cp("act", BR[par], BR[par][:], rows[:, 128:256], [rows])
                    tt("pool", T1[par], T1[par][:], KK[hq][:], BR[par][:], ALU.mult, [KK[hq], BR[par]])
                    tt("dve", T2[par], T2[par][:], T1[par][:], DT[par][:], ALU.mult, [T1[par], DT[par]])
                    x0 = XA[b4]; x0t = XAT[b4]; R = RR[b4]
                    tt("pool", x0, x0[:], T2[par][:], NUs, ALU.mult, [T2[par], MK])
                    tt("dve", QKm[h], QKm[h][:], QK[hq][:], DT[par][:], ALU.mult, [QK[hq], DT[par]])
                    r = rot(); tp(r, r[:], x0[:], [x0]); cp("act", x0t, x0t[:], r[:], [r])
                    tt("pool", R, R[:], x0[:], ident, ALU.add, [x0, MK])
                    ts("dve", VB[b4], VB[b4][:], VTM[:, h, :], beta[:, h:h + 1], None, ALU.mult, None, [VTM, beta])
                    ts("dve", KBG[b4], KBG[b4][:], KTM[:, hq, :], BG[:, h:h + 1], None, ALU.mult, None, [KTM, BG])
                    tt("pool", QG[h], QG[h][:], CO[hq][:, cs], EGR[par][:], ALU.mult, [CO[hq], EGR[par]])
                    ts("dve", KG[h], KG[h][:], KTM[:, hq, :], KD[:, h:h + 1], None, ALU.mult, None, [KTM, KD])
                for n in range(5):
                    last = n == 4
                    for h in heads:
                        b4 = h % 4
                        xc, xct = ((XA, XAT) if n % 2 == 0 else (XB, XBT))
                        xn, xnt = ((XB, XBT) if n % 2 == 0 else (XA, XAT))
                        xc = xc[b4]; xct = xct[b4]; xn = xn[b4]; xnt = xnt[b4]; R = RR[b4]
                        if not last:
                            r = rot(); mm(r, r[:], xct[:], xc[:], [xct, xc]); cp("act", xn, xn[:], r[:], [r])
                        r = rot(); mm(r, r[:], xc[:], xct[:], [xct, xc]); cp("dve", xnt, xnt[:], r[:], [r])
                    for h in heads:
                        b4 = h % 4
                        xnt = ((XBT) if n % 2 == 0 else (XAT))[b4]; R = RR[b4]
                        r = rot(); mm(r, r[:], xnt[:], R[:], [xnt, R]); tt("dve", R, R[:], R[:], r[:], ALU.add, [R, r])
                for h in heads:
                    b4 = h % 4; R = RR[b4]
                    r = rot(); mm(r, r[:], R[:], VB[b4][:], [R, VB[b4]]); cp("act", Usb[h], Usb[h][:], r[:], [r])
                    r = rot(); mm(r, r[:], KBG[b4][:], R[:], [R, KBG[b4]]); cp("act", WT[h], WT[h][:], r[:], [r])
            if maxphase < 6: continue
            for i in range(2):
                rr = slice(64 * i, 64 * i + 64)
                p1s = []
                for h in range(8):
                    r = rot(); mm(r, r[:], WT[h][:], Sst[h][:], [WT[h], Sst[h]]); p1s.append(r)
                for h in range(8):
                    tt("dve", VN[h], VN[h][rr, :], Usb[h][rr, :], p1s[h][rr, :], ALU.subtract, [Usb[h], p1s[h]])
                p2s = []
                for h in range(8):
                    r = rot()
                    mm(r, r[:], QG[h][:], Sst[h][:], [QG[h], Sst[h]], start=True, stop=False)
                    mm(r, r[:], QKm[h][rr, :], VN[h][rr, :], [QKm[h], VN[h]], start=False, stop=True)
                    p2s.append(r)
                for h in range(8):
                    cp("act", Osb, Osb[rr, h, :], p2s[h][rr, :], [p2s[h]])
                p3s = []
                for h in range(8):
                    r = rot(); mm(r, r[:], KG[h][rr, :], VN[h][rr, :], [KG[h], VN[h]]); p3s.append(r)
                for h in range(8):
                    stt("dve", Sst[h], Sst[h][:], Sst[h][:], EGL[:, 2 * h + i:2 * h + i + 1], p3s[h][:], ALU.mult, ALU.add, [Sst[h], EGL, p3s[h]])
            if maxphase < 7: continue
            for zc in range(2):
                for c in range(KC):
                    mm(zps, zps[:], H[:, c, cs], W[:, c, 2048 + zc * 512:2048 + (zc + 1) * 512], [W, H], start=(c == 0), stop=(c == KC - 1))
                act(ZS[0], ZS[0][:, zc * 512:(zc + 1) * 512], zps[:], AF.Silu, [zps])
            tt("pool", SQo, SQo[:], Osb[:], Osb[:], ALU.mult, [Osb])
            P.op("dve", lambda e: e.tensor_reduce(out=MS[:], in_=SQo[:], axis=AX.X, op=ALU.add), reads=[SQo], writes=[MS])
            act(MS, MS[:], MS[:], AF.Sqrt, [MS, epsb], bias=epsb[:, 0:1], scale=1.0 / 128)
            P.op("dve", lambda e: e.reciprocal(out=MS[:], in_=MS[:]), reads=[MS], writes=[MS])
            tt("dve", O2, O2[:], Osb[:], MS[:].unsqueeze(2).to_broadcast([128, 8, 128]), ALU.mult, [Osb, MS])
            tt("pool", O2, O2[:], O2[:], GN[:].unsqueeze(1).to_broadcast([128, 8, 128]), ALU.mult, [O2, GN])
            ob = OB[0]
            tt("dve", ob, ob[:], O2[:].rearrange("p a b -> p (a b)"), ZS[0][:], ALU.mult, [O2, ZS[0]])
            t0 = st * ST + tl * 128
            for h in range(8):
                r = rot(); rb = r[:].bitcast(BF16)
                P.op("pe", lambda e, rb=rb, h=h: e.transpose(out=rb[:, 0:128], in_=ob[:, h * 128:(h + 1) * 128], identity=identb[:]), reads=[ob, identb], writes=[r])
                cp("act" if h % 2 else "dve", OT, OT[:, h, :], rb[:, 0:128], [r])
            P.dma("sp", og[:, t0:t0 + 128].rearrange("(h f) t -> f h t", f=128), OT[:], reads=[OT])
    return


def host_D(hn_full, z):
    w_in = z['dn_w_in'][0]; conv = z['dn_conv'][0]
    s = np.arange(128)[:, None]; c = np.arange(128)[None, :]
    same = (s // 64) == (c // 64)
    masks = np.zeros((128, 6, 128), np.float32)
    masks[:, 0] = np.eye(128); masks[:, 1] = (same & (s <= c)); masks[:, 2] = same; masks[:, 3] = (same & (c >= s)); masks[:, 4] = -(same & (c > s)).astype(np.float32)
    masks[:, 5, 0] = (np.arange(128) < 64); masks[:, 5, 1] = (np.arange(128) >= 64)
    maps = []
    for core in range(8):
        b = core // 4; hg = core % 4
        qc = np.arange(hg * 512, hg * 512 + 512); kc = 2048 + qc; vc = 4096 + np.arange(hg * 1024, hg * 1024 + 1024); zc = 8192 + np.arange(hg * 1024, hg * 1024 + 1024)
        bc = 12288 + np.arange(hg * 8, hg * 8 + 8); ac = 12320 + np.arange(hg * 8, hg * 8 + 8)
        cols = np.concatenate([qc, kc, vc, zc, bc, ac])
        wcc = np.ascontiguousarray(w_in[:, cols])
        cch = np.concatenate([qc, kc, vc])
        convw = np.ascontiguousarray(conv[:, cch].T.reshape(16, 128, 4).transpose(1, 0, 2))
        hcc = np.zeros((128, 2, 8), np.float32)
        hcc[:, 0, :] = z['dn_a_log'][0][hg * 8:hg * 8 + 8][None]; hcc[:, 1, :] = z['dn_dt_bias'][0][hg * 8:hg * 8 + 8][None]
        gnrow = np.ascontiguousarray(np.broadcast_to(z['dn_norm'][0][None, :], (128, 128))).astype(np.float32)
        maps.append({"hnT": hn_full[b], "wc": wcc, "convw": convw, "hc": hcc, "gnrow": gnrow, "masks": masks})
    return maps


def emit_O1p(P, io):
    ogT = io("ogT", [1024, 8192], BF16, "ExternalInput")
    w = io("w", [1024, 2048], F32, "ExternalInput")
    pb = io("pb", [8192, 2048], F32, "ExternalOutput")
    wb = P.sb([128, 8, 2048], BF16)
    wv = w.rearrange("(c p) n -> p c n", p=128)
    for n0 in range(0, 2048, 512):
        P.dma("pool", wb[:, :, n0:n0 + 512], wv[:, :, n0:n0 + 512], writes=[wb])
    a = [P.sb([128, 8, 512], BF16) for _ in range(2)]
    ob = [P.sb([128, 512], F32) for _ in range(3)]
    ps = [P.ps([128, 512]) for _ in range(3)]
    av = ogT.rearrange("(c p) t -> p c t", p=128)
    pbv = pb.rearrange("(k r p) c -> k p r c", k=16, r=4, p=128)
    n_ = 0
    for s in range(16):
        A_ = a[s % 2]
        P.dma("sp", A_[:], av[:, :, s * 512:(s + 1) * 512], writes=[A_])
        for n in range(16):
            p_ = ps[n_ % 3]; O_ = ob[n_ % 3]; n_ += 1
            for c in range(8):
                P.op("pe", lambda e, c=c, p_=p_, n=n, A_=A_: e.matmul(p_[:], lhsT=wb[:, c, n * 128:(n + 1) * 128], rhs=A_[:, c, :], start=(c == 0), stop=(c == 7)), reads=[wb, A_], writes=[p_])
            if n % 2:
                P.op("act", lambda e, p_=p_, O_=O_: e.activation(out=O_[:], in_=p_[:], func=AF.Copy), reads=[p_], writes=[O_])
            else:
                P.op("dve", lambda e, p_=p_, O_=O_: e.tensor_copy(out=O_[:], in_=p_[:]), reads=[p_], writes=[O_])
            P.dma("act" if n % 2 else "sp", pbv[n][:, :, s * 128:(s + 1) * 128], O_[:].rearrange("p (r t) -> p r t", r=4), reads=[O_])
    return

_G4 = [[0, 1, 2, 3], [4, 5, 6, 7]]


_INFO = {}


def build_fused(upto=None):
    P = Prog()
    AG = "AllGather"
    s_qT = P.scratch("s_qT", [2048, 2048], BF16); s_kT = P.scratch("s_kT", [512, 2048], BF16); s_v = P.scratch("s_v", [2048, 512], BF16)
    s_iqT = P.scratch("s_iqT", [1024, 2048], F32); s_ikT = P.scratch("s_ikT", [64, 2048], F32); s_iw = P.scratch("s_iw", [2048, 16], F32)
    g_kT = P.scratch("g_kT", [2048, 2048], BF16); g_v = P.scratch("g_v", [8192, 512], BF16); g_ik = P.scratch("g_ik", [256, 2048], F32)
    s_oT = P.scratch("s_oT", [2048, 2048], BF16); s_h1 = P.scratch("s_h1", [2048, 2048], F32); s_h3 = P.scratch("s_h3", [2048, 2048], F32)
    s_hn = P.scratch("s_hn", [2048, 2048], BF16); g_hn = P.scratch("g_hn", [8192, 2048], BF16)
    s_og = P.scratch("s_og", [1024, 8192], BF16); s_pb = P.scratch("s_pb", [8192, 2048], F32); s_mix = P.scratch("s_mix", [2048, 2048], F32)
    P.begin_phase(); emit_A(P, IO(P, "A_", {"qT": s_qT, "kT": s_kT, "v": s_v, "iqT": s_iqT, "ikT": s_ikT, "iw": s_iw})); P.end_phase()
    for k in range(2):
        P.coll(AG, ALU.bypass, _G4, s_kT[k * 256:(k + 1) * 256, :], g_kT[k * 1024:(k + 1) * 1024, :])
    for k in range(2):
        P.coll(AG, ALU.bypass, _G4, s_v[k * 1024:(k + 1) * 1024, :], g_v[k * 4096:(k + 1) * 4096, :])
    P.coll(AG, ALU.bypass, _G4, s_ikT, g_ik)
    bB = {"qTc": s_qT, "kT": g_kT, "v": g_v, "ikT": g_ik, "iqTc": s_iqT, "iwc": s_iw, "oT": s_oT}
    if upto == "B":
        del bB["oT"]
    P.begin_phase(); emit_B(P, IO(P, "B_", bB)); P.end_phase()
    if upto == "B":
        _INFO["in"] = list(P.ext_in); _INFO["out"] = list(P.ext_out)
        return P.finish()
    bO = {"aT": s_oT, "hT": s_h1}
    if upto == "O":
        del bO["hT"]
    P.begin_phase(); emit_O(P, IO(P, "O0_", bO), 2048); P.end_phase()
    if upto == "O":
        _INFO["in"] = list(P.ext_in); _INFO["out"] = list(P.ext_out)
        return P.finish()
    P.begin_phase(); emit_P(P, IO(P, "P0_", {"hT": s_h1, "h3T": s_h3, "hnT": s_hn}), True); P.end_phase()
    for k in range(8):
        P.coll(AG, ALU.bypass, _G4, s_hn[k * 256:(k + 1) * 256, :], g_hn[k * 1024:(k + 1) * 1024, :])
    P.begin_phase(); emit_D(P, IO(P, "D_", {"hnT": g_hn, "og": s_og})); P.end_phase()
    P.begin_phase(); emit_O1p(P, IO(P, "Q_", {"ogT": s_og, "pb": s_pb})); P.end_phase()
    for k in range(16):
        P.coll("ReduceScatter", ALU.add, _G4, s_pb[k * 512:(k + 1) * 512, :], s_mix[k * 128:(k + 1) * 128, :])
    P.begin_phase(); emit_P(P, IO(P, "P1_", {"hT": s_h3, "mixT": s_mix}), False, add=True); P.end_phase()
    print("fused stats", P.stats(), flush=True)
    _INFO["in"] = list(P.ext_in); _INFO["out"] = list(P.ext_out)
    return P.finish()


def own_pos(c):
    return np.concatenate([np.arange((4 * j + c) * 128, (4 * j + c + 1) * 128) for j in range(16)])


def host_fused(z):
    x = z['x']
    ident = np.eye(128, dtype=np.float32).astype(BF)
    uT = [np.ascontiguousarray(z['peer_u'][l].T) for l in range(2)]
    keysT = [np.ascontiguousarray(z['peer_keys'][l].reshape(16, 128, 128).transpose(2, 0, 1)) for l in range(2)]
    gns = []
    for l in range(2):
        gnext = z['norm_mix'][l + 1] if l == 0 else np.ones(D, np.float32)
        gns.append(np.ascontiguousarray(np.stack([z['norm_ffn'][l].reshape(KC, 128).T, z['norm_ple'][l].reshape(KC, 128).T, gnext.reshape(KC, 128).T], axis=1)))
    dmaps = host_D([None, None], z)
    maps = []
    for core in range(8):
        b = core // 4; c = core % 4
        pos = own_pos(c)
        csq, csi, perm = rope_tables(pos)
        xT = np.ascontiguousarray(x[b, pos, :].T)
        r = np.arange(128)[:, None]; t = np.arange(512)[None, :]
        m = {"A_xT": xT, "A_gmix": np.ascontiguousarray(z['norm_mix'][0].reshape(KC, 128).T), "A_w_in": z['attn_w_in'][0],
             "A_qkg": np.ascontiguousarray(np.stack([z['attn_q_norm'][0], z['attn_k_norm'][0]], axis=1)), "A_csq": csq, "A_csi": csi, "A_perm": perm,
             "B_cbias": np.where(t <= c * 128 + r, 0.0, -1e5).astype(np.float32), "B_ident": ident,
             "O0_resT": xT, "O0_w": z['attn_w_out'][0]}
        for l, pre in ((0, "P0_"), (1, "P1_")):
            m.update({pre + "gn": gns[l], pre + "w_q": z['peer_w_q'][l], pre + "keysT": keysT[l], pre + "uT": uT[l], pre + "vt": z['peer_v'][l],
                      pre + "wg": z['ple_w_gate'][l], pre + "wpi": z['ple_w_in'][l], pre + "pT": np.ascontiguousarray(z['p'][l, b, pos, :].T), pre + "ident": ident})
        for k in ("wc", "convw", "hc", "gnrow", "masks"):
            m["D_" + k] = dmaps[core][k]
        m["Q_w"] = np.ascontiguousarray(z['dn_w_out'][0][c * 1024:(c + 1) * 1024, :])
        maps.append(m)
    return maps


def kernel(**inputs):
    z = {k: np.ascontiguousarray(np.asarray(v)) for k, v in inputs.items()}
    nc = build_fused()
    maps = [{k: m[k] for k in _INFO["in"]} for m in host_fused(z)]
    res = run_bass_kernel_spmd(nc, maps, core_ids=list(range(8))).results
    out = np.zeros((2, 8192, 2048), np.float32)
    for core in range(8):
        b = core // 4; c = core % 4
        out[b, own_pos(c), :] = res[core]["P1_h3T"].T
    return out
```

```python
import numpy as np
from contextlib import ExitStack
import concourse.bass as bass
import concourse.mybir as mybir
from concourse.bass_utils import run_bass_kernel_spmd

F32 = mybir.dt.float32
BF16 = mybir.dt.bfloat16
I32 = mybir.dt.int32
U32 = mybir.dt.uint32
AF = mybir.ActivationFunctionType
ALU = mybir.AluOpType
AX = mybir.AxisListType

ENGS = ("pe", "act", "dve", "pool", "sp")


class T:
    __slots__ = ("t", "name", "lw", "rd")

    def __init__(self, t, name):
        self.t = t
        self.name = name
        self.lw = None
        self.rd = []

    def __getitem__(self, idx):
        return self.t[idx]


class TV:
    __slots__ = ("t", "name", "par")

    def __init__(self, par, ap, name):
        self.par = par
        self.t = ap
        self.name = name

    def __getitem__(self, idx):
        return self.t[idx]

    @property
    def lw(self):
        return self.par.lw

    @lw.setter
    def lw(self, v):
        self.par.lw = v

    @property
    def rd(self):
        return self.par.rd

    @rd.setter
    def rd(self, v):
        self.par.rd = v


class Prog:
    def __init__(self, name="k", ndma=6, same_eng_sync=True):
        self.nc = bass.Bass("TRN2", target_bir_lowering=False)
        self.es = ExitStack()
        self.ops = {e: [] for e in ENGS}
        self.cnt = {e: 0 for e in ENGS}
        self.known = {e: {} for e in ENGS}
        self.sem = {}
        for e in ENGS:
            self.sem[e] = self.es.enter_context(self.nc.semaphore("c_" + e))
        self.ndma = ndma
        self.dsem = {}
        self.dcnt = {}
        for q in ("sp", "act", "pool"):
            self.dsem[q] = [self.es.enter_context(self.nc.semaphore(f"d_{q}{i}")) for i in range(ndma)]
            self.dcnt[q] = 0
        self.same = same_eng_sync
        self.n_t = 0
        self.phase_es = None
        self.dram_dep = {}
        self.ccsem = self.es.enter_context(self.nc.semaphore("ccsem"))
        self.ccn = 0
        self.ext_in = []
        self.ext_out = []

    def dram(self, name, shape, dt, kind):
        return self.nc.dram_tensor(name, list(shape), dt, kind=kind).ap()

    def sb(self, shape, dt, name=None):
        self.n_t += 1
        name = name or f"sb{self.n_t}"
        t = (self.phase_es or self.es).enter_context(self.nc.sbuf_tensor(name, list(shape), dt))
        return T(t, name)

    def ps(self, shape, dt=F32, name=None):
        self.n_t += 1
        name = name or f"ps{self.n_t}"
        t = (self.phase_es or self.es).enter_context(self.nc.psum_tensor(name, list(shape), dt))
        return T(t, name)

    def scratch(self, name, shape, dt):
        t = self.nc.dram_tensor(name, list(shape), dt)
        self.dram_dep[name] = T(None, name)
        return t.ap()

    def begin_phase(self):
        self.phase_es = ExitStack()

    def end_phase(self):
        self.barrier()
        self.phase_es.close()
        self.phase_es = None

    def barrier(self):
        for e in ENGS:
            toks = []
            for f in ENGS:
                if f != e and self.cnt[f] > 0:
                    toks.append((f, self.sem[f], self.cnt[f], f))
            for q in ("sp", "act", "pool"):
                n = self.dcnt[q]
                for slot in range(min(n, self.ndma)):
                    last_i = ((n - 1 - slot) // self.ndma) * self.ndma + slot
                    toks.append((f"d_{q}{slot}", self.dsem[q][slot], 16 * (last_i // self.ndma + 1), "dma"))
            if self.ccn:
                toks.append(("cc", self.ccsem, self.ccn, "dma"))
            waits = self._waits(e, toks)
            self.ops[e].append((waits, None, None))

    def coll(self, kind, op, groups, src, dst):
        reads = [self.dram_dep[src.name]]
        writes = [self.dram_dep[dst.name]]
        waits = self._waits("pool", self._deps(reads, writes))
        self.ccn += 1
        tok = ("cc", self.ccsem, self.ccn, "dma")

        def fn(e, kind=kind, op=op, groups=groups, src=src, dst=dst):
            return e.collective_compute(kind, op, replica_groups=groups, ins=[src.opt()], outs=[dst.opt()])
        self.ops["pool"].append((waits, fn, (self.ccsem, 1)))
        self._commit(tok, reads, writes)

    def ext(self, name):
        return T(None, name)

    def _deps(self, reads, writes):
        toks = []
        for r in reads:
            if r.lw is not None:
                toks.append(r.lw)
        for w in writes:
            if w.lw is not None:
                toks.append(w.lw)
            toks.extend(w.rd)
        return toks

    def _waits(self, eng, toks):
        kn = self.known[eng]
        need = {}
        for (key, semh, val, src_eng) in toks:
            if src_eng == eng and (not self.same or eng == "pe"):
                continue
            if kn.get(key, 0) >= val:
                continue
            if key not in need or need[key][1] < val:
                need[key] = (semh, val)
        out = []
        for key, (semh, val) in need.items():
            kn[key] = val
            out.append((semh, val))
        return out

    def _commit(self, tok, reads, writes):
        for w in writes:
            w.lw = tok
            w.rd = []
        for r in reads:
            if r in writes:
                continue
            r.rd.append(tok)
            if len(r.rd) > 24:
                best = {}
                for t in r.rd:
                    if t[0] not in best or best[t[0]][2] < t[2]:
                        best[t[0]] = t
                r.rd = list(best.values())

    def op(self, eng, fn, reads=(), writes=()):
        reads = [r for r in reads if r is not None]
        writes = [w for w in writes if w is not None]
        waits = self._waits(eng, self._deps(reads, writes))
        self.cnt[eng] += 1
        seq = self.cnt[eng]
        tok = (eng, self.sem[eng], seq, eng)
        self.ops[eng].append((waits, fn, (self.sem[eng], 1)))
        self._commit(tok, reads, writes)
        return tok

    def dma(self, q, out, in_, reads=(), writes=(), **kw):
        reads = [r for r in reads if r is not None]
        writes = [w for w in writes if w is not None]
        nm = getattr(in_, "name", None)
        if nm in self.dram_dep:
            reads.append(self.dram_dep[nm])
        nm = getattr(out, "name", None)
        if nm in self.dram_dep:
            writes.append(self.dram_dep[nm])
        i = self.dcnt[q]
        self.dcnt[q] += 1
        slot = i % self.ndma
        val = 16 * (i // self.ndma + 1)
        semh = self.dsem[q][slot]
        key = f"d_{q}{slot}"
        toks = self._deps(reads, writes)
        if i >= self.ndma:
            toks.append((key, semh, val - 16, "dma"))
        waits = self._waits(q, toks)
        tok = (key, semh, val, "dma")

        def fn(e, out=out, in_=in_, kw=kw):
            return e.dma_start(out=out, in_=in_, **kw)
        self.ops[q].append((waits, fn, (semh, 16)))
        self._commit(tok, reads, writes)
        return tok

    def finish(self, final_toks=()):
        nc = self.nc
        fin = []
        for q in ("sp", "act", "pool"):
            n = self.dcnt[q]
            for slot in range(min(n, self.ndma)):
                last_i = ((n - 1 - slot) // self.ndma) * self.ndma + slot
                fin.append((f"d_{q}{slot}", self.dsem[q][slot], 16 * (last_i // self.ndma + 1), "dma"))
        for e in ENGS:
            if e != "sp" and self.cnt[e] > 0:
                fin.append((e, self.sem[e], self.cnt[e], e))
        if self.ccn:
            fin.append(("cc", self.ccsem, self.ccn, "dma"))
        fwaits = self._waits("sp", fin)
        ops = self.ops

        def run(e, lst, extra=()):
            for waits, fn, inc in lst:
                for (s, v) in waits:
                    e.wait_ge(s, v)
                if fn is not None:
                    fn(e).then_inc(inc[0], inc[1])
            for (s, v) in extra:
                e.wait_ge(s, v)

        with nc.Block() as block:
            @block.sync
            def _(e):
                run(e, ops["sp"], fwaits)

            @block.tensor
            def _(e):
                run(e, ops["pe"])

            @block.scalar
            def _(e):
                run(e, ops["act"])

            @block.vector
            def _(e):
                run(e, ops["dve"])

            @block.gpsimd
            def _(e):
                run(e, ops["pool"])
        self.es.close()
        return nc

    def stats(self):
        return {e: len(self.ops[e]) for e in ENGS}


class IO:
    def __init__(self, P, prefix, bind=None):
        self.P = P
        self.prefix = prefix
        self.bind = bind or {}

    def __call__(self, name, shape, dt, kind):
        if name in self.bind:
            ap = self.bind[name]
            assert list(ap.shape) == list(shape), (name, ap.shape, shape)
            return ap
        full = self.prefix + name
        if kind == "ExternalInput":
            self.P.ext_in.append(full)
        else:
            self.P.ext_out.append(full)
        return self.P.dram(full, shape, dt, kind)


import ml_dtypes
BF = ml_dtypes.bfloat16


D = 2048; NT = 2048; TT = 512; KC = 16
EPS = 1e-6

def emit_A(P, io):
    xT = io("xT", [D, NT], F32, "ExternalInput")
    gmix = io("gmix", [128, KC], F32, "ExternalInput")
    w_in = io("w_in", [D, 4176], F32, "ExternalInput")
    qkg = io("qkg", [128, 2], F32, "ExternalInput")
    csq = io("csq", [32, 2, NT], F32, "ExternalInput")
    csi = io("csi", [128, 2, NT], F32, "ExternalInput")
    perm = io("perm", [128, 2, 128], F32, "ExternalInput")
    qT = io("qT", [2048, NT], BF16, "ExternalOutput")
    kT = io("kT", [512, NT], BF16, "ExternalOutput")
    vv = io("v", [NT, 512], BF16, "ExternalOutput")
    iqT = io("iqT", [1024, NT], F32, "ExternalOutput")
    ikT = io("ikT", [64, NT], F32, "ExternalOutput")
    iw = io("iw", [NT, 16], F32, "ExternalOutput")

    gm = P.sb([128, KC], F32); qk = P.sb([128, 2], F32); pm = P.sb([128, 2, 128], F32)
    ones_b = P.sb([128, 128], BF16); ones_f = P.sb([128, 128], F32); epsb = P.sb([128, 1], F32)
    P.dma("sp", gm[:], gmix[:, :], writes=[gm]); P.dma("sp", qk[:], qkg[:, :], writes=[qk]); P.dma("sp", pm[:], perm[:, :, :], writes=[pm])
    P.op("dve", lambda e: e.memset(ones_b[:], 1.0), writes=[ones_b])
    P.op("dve", lambda e: e.memset(ones_f[:], 1.0 / 128), writes=[ones_f])
    P.op("dve", lambda e: e.memset(epsb[:], EPS), writes=[epsb])

    xs = P.sb([128, KC, TT], F32)
    sq = P.sb([128, KC, TT], BF16)
    hb = P.sb([128, KC, TT], BF16)
    rstd = P.sb([128, TT], F32)
    cq = P.sb([32, 2, TT], F32); ci = P.sb([128, 2, TT], F32)
    w32 = [P.sb([128, KC, 512], F32) for _ in range(2)]
    wb = [P.sb([128, KC, 512], BF16) for _ in range(2)]
    ps_ms = P.ps([128, TT]); ps_pj = [P.ps([128, TT]) for _ in range(2)]; ps_ms2 = P.ps([128, TT]); ps_rot = P.ps([128, TT])
    sq2 = [P.sb([128, TT], F32) for _ in range(2)]
    r2 = [P.sb([128, TT], F32) for _ in range(2)]
    qn = [P.sb([128, TT], F32) for _ in range(2)]
    t1 = [P.sb([128, TT], F32) for _ in range(2)]
    t2 = [P.sb([128, TT], F32) for _ in range(2)]
    ob = [P.sb([128, TT], BF16) for _ in range(2)]
    of = [P.sb([128, TT], F32) for _ in range(2)]
    vb = [P.sb([128, 512], BF16) for _ in range(2)]
    iwb = [P.sb([128, 16], F32) for _ in range(2)]
    xTv = xT.rearrange("(c p) t -> p c t", p=128)
    wv = w_in.rearrange("(c p) n -> p c n", p=128)
    cnt = [0]
    for tt in range(NT // TT):
        ts = slice(tt * TT, (tt + 1) * TT)
        P.dma("sp", xs[:], xTv[:, :, ts], writes=[xs])
        P.dma("act", cq[:], csq[:, :, ts], writes=[cq])
        P.dma("act", ci[:], csi[:, :, ts], writes=[ci])
        P.op("act", lambda e: e.activation(out=sq[:], in_=xs[:], func=AF.Square), reads=[xs], writes=[sq])
        for c in range(KC):
            P.op("pe", lambda e, c=c: e.matmul(ps_ms[:], lhsT=ones_b[:], rhs=sq[:, c, :], start=(c == 0), stop=(c == KC - 1)), reads=[ones_b, sq], writes=[ps_ms])
        P.op("act", lambda e: e.activation(out=rstd[:], in_=ps_ms[:], func=AF.Sqrt, bias=epsb[:, 0:1], scale=1.0 / D), reads=[ps_ms, epsb], writes=[rstd])
        P.op("dve", lambda e: e.reciprocal(out=rstd[:], in_=rstd[:]), reads=[rstd], writes=[rstd])
        for c in range(KC):
            P.op("dve", lambda e, c=c: e.scalar_tensor_tensor(out=xs[:, c, :], in0=xs[:, c, :], scalar=gm[:, c:c + 1], in1=rstd[:], op0=ALU.mult, op1=ALU.mult), reads=[xs, gm, rstd], writes=[xs])
        P.op("pool", lambda e: e.tensor_copy(out=hb[:], in_=xs[:]), reads=[xs], writes=[hb])
        for g in range(9):
            n0 = g * 512; ncol = 512 if g < 8 else 80
            bi = cnt[0] % 2; cnt[0] += 1
            W32 = w32[bi]; WB = wb[bi]
            P.dma("sp", W32[:, :, 0:ncol], wv[:, :, n0:n0 + ncol], writes=[W32])
            if g <= 5:
                P.op("pool", lambda e, W32=W32, WB=WB: e.tensor_copy(out=WB[:], in_=W32[:]), reads=[W32], writes=[WB])
            if g <= 4:
                for j in range(4):
                    ch = g * 4 + j
                    pj = ps_pj[ch % 2]; S2 = sq2[ch % 2]; R2 = r2[ch % 2]; QN = qn[ch % 2]; T1 = t1[ch % 2]; T2 = t2[ch % 2]; OB = ob[ch % 2]
                    for c in range(KC):
                        P.op("pe", lambda e, c=c, j=j, pj=pj, WB=WB: e.matmul(pj[:], lhsT=WB[:, c, j * 128:(j + 1) * 128], rhs=hb[:, c, :], start=(c == 0), stop=(c == KC - 1)), reads=[WB, hb], writes=[pj])
                    P.op("act", lambda e, pj=pj, S2=S2: e.activation(out=S2[:], in_=pj[:], func=AF.Square), reads=[pj], writes=[S2])
                    P.op("pe", lambda e, S2=S2: e.matmul(ps_ms2[:], lhsT=ones_f[:], rhs=S2[:], start=True, stop=True), reads=[ones_f, S2], writes=[ps_ms2])
                    P.op("act", lambda e, R2=R2: e.activation(out=R2[:], in_=ps_ms2[:], func=AF.Sqrt, bias=epsb[:, 0:1], scale=1.0), reads=[ps_ms2, epsb], writes=[R2])
                    P.op("dve", lambda e, R2=R2: e.reciprocal(out=R2[:], in_=R2[:]), reads=[R2], writes=[R2])
                    gi = 0 if g < 4 else 1
                    P.op("dve", lambda e, pj=pj, QN=QN, R2=R2, gi=gi: e.scalar_tensor_tensor(out=QN[:], in0=pj[:], scalar=qk[:, gi:gi + 1], in1=R2[:], op0=ALU.mult, op1=ALU.mult), reads=[pj, qk, R2], writes=[QN])
                    P.op("pe", lambda e, QN=QN: e.matmul(ps_rot[0:32, :], lhsT=pm[0:32, 0, 0:32], rhs=QN[0:32, :], start=True, stop=True), reads=[pm, QN], writes=[ps_rot])
                    P.op("pool", lambda e, QN=QN, T1=T1: e.tensor_tensor(out=T1[0:32, :], in0=QN[0:32, :], in1=cq[:, 0, :], op=ALU.mult), reads=[QN, cq], writes=[T1])
                    P.op("dve", lambda e, T2=T2: e.tensor_tensor(out=T2[0:32, :], in0=ps_rot[0:32, :], in1=cq[:, 1, :], op=ALU.mult), reads=[ps_rot, cq], writes=[T2])
                    P.op("pool", lambda e, QN=QN, T1=T1, T2=T2: e.tensor_tensor(out=QN[0:32, :], in0=T1[0:32, :], in1=T2[0:32, :], op=ALU.add), reads=[T1, T2], writes=[QN])
                    P.op("act", lambda e, QN=QN, OB=OB: e.activation(out=OB[:], in_=QN[:], func=AF.Copy), reads=[QN], writes=[OB])
                    dst = qT[ch * 128:(ch + 1) * 128, ts] if g < 4 else kT[j * 128:(j + 1) * 128, ts]
                    P.dma("pool", dst, OB[:], reads=[OB])
            elif g == 5:
                for s in range(4):
                    pj = ps_pj[s % 2]; VB = vb[s % 2]
                    for c in range(KC):
                        P.op("pe", lambda e, c=c, s=s, pj=pj, WB=WB: e.matmul(pj[:], lhsT=hb[:, c, s * 128:(s + 1) * 128], rhs=WB[:, c, :], start=(c == 0), stop=(c == KC - 1)), reads=[WB, hb], writes=[pj])
                    P.op("act", lambda e, pj=pj, VB=VB: e.activation(out=VB[:], in_=pj[:], func=AF.Copy), reads=[pj], writes=[VB])
                    P.dma("pool", vv[tt * TT + s * 128: tt * TT + (s + 1) * 128, :], VB[:], reads=[VB])
            else:
                nch = 4 if g < 8 else 1
                for j in range(nch):
                    M = 128 if g < 8 else 64
                    ch = (g - 6) * 4 + j
                    pj = ps_pj[j % 2]; QN = qn[j % 2]; T1 = t1[j % 2]; T2 = t2[j % 2]; OF = of[j % 2]
                    for c in range(KC):
                        P.op("pe", lambda e, c=c, j=j, pj=pj, W32=W32, M=M: e.matmul(pj[0:M, :], lhsT=W32[:, c, j * 128:j * 128 + M], rhs=xs[:, c, :], start=(c == 0), stop=(c == KC - 1)), reads=[W32, xs], writes=[pj])
                    P.op("act", lambda e, pj=pj, QN=QN, M=M: e.activation(out=QN[0:M, :], in_=pj[0:M, :], func=AF.Copy), reads=[pj], writes=[QN])
                    P.op("pe", lambda e, QN=QN, M=M: e.matmul(ps_rot[0:M, :], lhsT=pm[0:M, 1, 0:M], rhs=QN[0:M, :], start=True, stop=True), reads=[pm, QN], writes=[ps_rot])
                    P.op("pool", lambda e, QN=QN, T1=T1, M=M: e.tensor_tensor(out=T1[0:M, :], in0=QN[0:M, :], in1=ci[0:M, 0, :], op=ALU.mult), reads=[QN, ci], writes=[T1])
                    P.op("dve", lambda e, T2=T2, M=M: e.tensor_tensor(out=T2[0:M, :], in0=ps_rot[0:M, :], in1=ci[0:M, 1, :], op=ALU.mult), reads=[ps_rot, ci], writes=[T2])
                    P.op("pool", lambda e, OF=OF, T1=T1, T2=T2, M=M: e.tensor_tensor(out=OF[0:M, :], in0=T1[0:M, :], in1=T2[0:M, :], op=ALU.add), reads=[T1, T2], writes=[OF])
                    dst = iqT[ch * 128:(ch + 1) * 128, ts] if g < 8 else ikT[:, ts]
                    P.dma("pool", dst, OF[0:M, :], reads=[OF])
                if g == 8:
                    for s in range(4):
                        pj = ps_pj[s % 2]; IW = iwb[s % 2]
                        for c in range(KC):
                            P.op("pe", lambda e, c=c, s=s, pj=pj, W32=W32: e.matmul(pj[:, 0:16], lhsT=xs[:, c, s * 128:(s + 1) * 128], rhs=W32[:, c, 64:80], start=(c == 0), stop=(c == KC - 1)), reads=[W32, xs], writes=[pj])
                        P.op("act", lambda e, pj=pj, IW=IW: e.activation(out=IW[:], in_=pj[:, 0:16], func=AF.Copy), reads=[pj], writes=[IW])
                        P.dma("pool", iw[tt * TT + s * 128: tt * TT + (s + 1) * 128, :], IW[:], reads=[IW])
    return


def rope_tables(pos):
    pos = pos.astype(np.float32)
    theta = np.float32(500000.0)
    f16 = (theta ** (-np.arange(16, dtype=np.float32) * np.float32(2.0 / 32))).astype(np.float32)
    ang = pos[None, :] * f16[:, None]
    csq = np.zeros((32, 2, len(pos)), np.float32)
    csq[0:16, 0] = np.cos(ang); csq[16:32, 0] = np.cos(ang)
    csq[0:16, 1] = np.sin(ang); csq[16:32, 1] = np.sin(ang)
    f8 = (theta ** (-np.arange(8, dtype=np.float32) * np.float32(2.0 / 16))).astype(np.float32)
    ang8 = pos[None, :] * f8[:, None]
    csi = np.zeros((128, 2, len(pos)), np.float32)
    csi[:, 0] = 1.0
    for h in range(2):
        for half in range(2):
            csi[h * 64 + half * 8: h * 64 + half * 8 + 8, 0] = np.cos(ang8)
            csi[h * 64 + half * 8: h * 64 + half * 8 + 8, 1] = np.sin(ang8)
    perm = np.zeros((128, 2, 128), np.float32)
    for m in range(16):
        perm[m + 16, 0, m] = -1.0
        perm[m, 0, m + 16] = 1.0
    for h in range(2):
        for m in range(8):
            perm[h * 64 + m + 8, 1, h * 64 + m] = -1.0
            perm[h * 64 + m, 1, h * 64 + m + 8] = 1.0
    return csq, csi, perm


def host_A(x, norm_mix0, attn_w_in, qn, kn):
    maps = []
    for core in range(8):
        b = core // 4; t0 = (core % 4) * NT
        pos = np.arange(t0, t0 + NT)
        csq, csi, perm = rope_tables(pos)
        maps.append({
            "xT": np.ascontiguousarray(x[b, t0:t0 + NT, :].T),
            "gmix": np.ascontiguousarray(norm_mix0.reshape(KC, 128).T),
            "w_in": attn_w_in,
            "qkg": np.ascontiguousarray(np.stack([qn, kn], axis=1)),
            "csq": csq, "csi": csi, "perm": perm,
        })
    return maps


TKEYS = 8192; NQT = 16
NBIS = 24; LO0 = -2048.0; TOPK = 256.0

def emit_B(P, io, nqt=NQT):
    n_sc = 0; n_v = 0; n_pl = 0
    qTc = io("qTc", [2048, 2048], BF16, "ExternalInput")
    kT = io("kT", [2048, 2048], BF16, "ExternalInput")
    vv = io("v", [TKEYS, 512], BF16, "ExternalInput")
    ikT = io("ikT", [256, 2048], F32, "ExternalInput")
    iqTc = io("iqTc", [1024, 2048], F32, "ExternalInput")
    iwc = io("iwc", [2048, 16], F32, "ExternalInput")
    cbias = io("cbias", [128, 512], F32, "ExternalInput")
    identd = io("ident", [128, 128], BF16, "ExternalInput")
    oT = io("oT", [2048, 2048], BF16, "ExternalOutput")

    kt = P.sb([128, 4, TKEYS], BF16); ik = P.sb([64, TKEYS], F32)
    iw = P.sb([128, NQT, 16], F32); cb = P.sb([128, 512], F32); ident = P.sb([128, 128], BF16)
    ones_b = P.sb([128, 128], BF16)
    for g in range(4):
        for r in range(4):
            P.dma("sp", kt[:, g, :].rearrange("d (j r t) -> d j r t", r=4, t=128)[:, :, r, :], kT[(g // 2) * 1024 + r * 256 + (g % 2) * 128:(g // 2) * 1024 + r * 256 + (g % 2) * 128 + 128, :].rearrange("d (j t) -> d j t", t=128), writes=[kt])
    for r in range(4):
        P.dma("act", ik[:].rearrange("d (j r t) -> d j r t", r=4, t=128)[:, :, r, :], ikT[r * 64:(r + 1) * 64, :].rearrange("d (j t) -> d j t", t=128), writes=[ik])
    P.dma("act", iw[:], iwc.rearrange("(j p) h -> p j h", p=128), writes=[iw])
    P.dma("act", cb[:], cbias[:, :], writes=[cb]); P.dma("act", ident[:], identd[:, :], writes=[ident])
    P.op("dve", lambda e: e.memset(ones_b[:], 1.0), writes=[ones_b])

    score = P.sb([128, TKEYS], F32); junk = P.sb([128, TKEYS], BF16)
    maskT = P.sb([128, 64, 128], BF16)
    qt = [P.sb([128, 16, 128], BF16) for _ in range(2)]
    iq = [P.sb([64, 16, 128], F32) for _ in range(1)]
    rl = [P.sb([128, 512], F32) for _ in range(4)]
    mk_ = [P.sb([128, 512], BF16) for _ in range(2)]
    vt = [P.sb([128, 4, 128], BF16) for _ in range(3)]
    pe_ = [P.sb([128, 512], BF16) for _ in range(3)]
    pmk = [P.sb([128, 512], BF16) for _ in range(3)]
    lo = P.sb([128, 1], F32); mid = P.sb([128, 1], F32); cntall = P.sb([128, NBIS], F32); tmp = P.sb([128, 1], F32)
    rs = P.sb([128, 512], F32); ob = [P.sb([128, 512], BF16) for _ in range(2)]
    ps_sc = [P.ps([128, 512]) for _ in range(2)]
    ps_tr = P.ps([128, 4, 128], BF16)
    ps_pl = [P.ps([128, 512]) for _ in range(2)]
    ps_o = P.ps([128, 512]); ps_s = P.ps([128, 512])
    sc_rot = [ps_sc[0], ps_sc[1], ps_pl[0], ps_pl[1]]
    qv = qTc.rearrange("(h d) q -> d h q", d=128)
    iqv = iqTc.rearrange("(h d) q -> d h q", d=64)
    SCALE = 128 ** -0.5
    for j in range(nqt):
        NCH = j + 1
        QT = qt[j % 2]; IQ = iq[0]
        P.dma("sp", QT[:], qv[:, :, j * 128:(j + 1) * 128], writes=[QT])
        P.dma("sp", IQ[:], iqv[:, :, j * 128:(j + 1) * 128], writes=[IQ])
        for ch in range(NCH):
            cs = slice(ch * 512, (ch + 1) * 512)
            for h in range(16):
                ps = sc_rot[n_sc % 4]; RL = rl[n_sc % 4]; n_sc += 1
                P.op("pe", lambda e, ps=ps, h=h, cs=cs, IQ=IQ: e.matmul(ps[:], lhsT=IQ[:, h, :], rhs=ik[:, cs], start=True, stop=True), reads=[IQ, ik], writes=[ps])
                P.op("act", lambda e, ps=ps, RL=RL: e.activation(out=RL[:], in_=ps[:], func=AF.Relu), reads=[ps], writes=[RL])
                if h == 0:
                    P.op("dve", lambda e, RL=RL, cs=cs, j=j, h=h: e.tensor_scalar(out=score[:, cs], in0=RL[:], scalar1=iw[:, j, h:h + 1], scalar2=None, op0=ALU.mult), reads=[RL, iw], writes=[score])
                else:
                    P.op("dve", lambda e, RL=RL, cs=cs, j=j, h=h: e.scalar_tensor_tensor(out=score[:, cs], in0=RL[:], scalar=iw[:, j, h:h + 1], in1=score[:, cs], op0=ALU.mult, op1=ALU.add), reads=[RL, iw, score], writes=[score])
            if ch == NCH - 1:
                P.op("dve", lambda e, cs=cs: e.tensor_tensor(out=score[:, cs], in0=score[:, cs], in1=cb[:], op=ALU.add), reads=[score, cb], writes=[score])
        S = NCH * 512
        P.op("dve", lambda e: e.memset(cntall[:], 0.0), writes=[cntall])
        step = -LO0
        P.op("dve", lambda e: e.memset(mid[:], 0.0), writes=[mid])
        for it in range(NBIS):
            st = step
            P.op("dve", lambda e, S=S, it=it: e.tensor_scalar(out=junk[:, 0:S], in0=score[:, 0:S], scalar1=mid[:, 0:1], scalar2=0.0, op0=ALU.is_ge, op1=ALU.add, accum_out=cntall[:, it:it + 1]), reads=[score, mid, cntall], writes=[junk, cntall])
            P.op("dve", lambda e, it=it: e.tensor_scalar(out=tmp[:], in0=cntall[:, it:it + 1], scalar1=TOPK, scalar2=0.5, op0=ALU.is_ge, op1=ALU.subtract), reads=[cntall], writes=[tmp])
            P.op("dve", lambda e, st=st: e.scalar_tensor_tensor(out=mid[:], in0=tmp[:], scalar=st, in1=mid[:], op0=ALU.mult, op1=ALU.add), reads=[tmp, mid], writes=[mid])
            step *= 0.5
        fs = step
        P.op("dve", lambda e, fs=fs: e.tensor_scalar(out=lo[:], in0=mid[:], scalar1=-fs, scalar2=None, op0=ALU.add), reads=[mid], writes=[lo])
        for ch in range(NCH):
            cs = slice(ch * 512, (ch + 1) * 512)
            MK = mk_[ch % 2]
            P.op("dve", lambda e, MK=MK, cs=cs: e.tensor_scalar(out=MK[:], in0=score[:, cs], scalar1=lo[:, 0:1], scalar2=None, op0=ALU.is_ge), reads=[score, lo], writes=[MK])
            for t in range(4):
                P.op("pe", lambda e, MK=MK, t=t: e.transpose(out=ps_tr[:, t, :], in_=MK[:, t * 128:(t + 1) * 128], identity=ident[:]), reads=[MK, ident], writes=[ps_tr])
            P.op("act", lambda e, ch=ch: e.activation(out=maskT[:, ch * 4:(ch + 1) * 4, :], in_=ps_tr[:], func=AF.Copy), reads=[ps_tr], writes=[maskT])
        for g in range(4):
            nst = NCH * 4
            tiles = [(ch, t) for ch in range(NCH) for t in range(4)]
            vts = {}

            def issue_qk(idx, g=g, QT=QT):
                nonlocal n_v
                ch, t = tiles[idx]; st_ = ch * 4 + t
                if t == 0:
                    VT = vt[n_v % 3]; n_v += 1
                    P.dma("sp", VT[:], vv[(ch // 8) * 4096:(ch // 8 + 1) * 4096, :].rearrange("(r n) c -> n r c", r=4)[(ch % 8) * 128:(ch % 8 + 1) * 128, :, g * 128:(g + 1) * 128], writes=[VT])
                    vts[ch] = VT
                pl = ps_pl[idx % 2]
                P.op("pe", lambda e, pl=pl, st_=st_: e.matmul(pl[:], lhsT=kt[:, g, st_ * 128:(st_ + 1) * 128], rhs=QT[:, g * 4:(g + 1) * 4, :], start=True, stop=True), reads=[kt, QT], writes=[pl])
                return pl
            nxt = issue_qk(0)
            for idx in range(len(tiles)):
                pl = nxt
                if idx + 1 < len(tiles):
                    nxt = issue_qk(idx + 1)
                ch, t = tiles[idx]; st_ = ch * 4 + t; VT = vts[ch]
                PE_ = pe_[n_pl % 3]; PM = pmk[n_pl % 3]; n_pl += 1
                P.op("act", lambda e, pl=pl, PE_=PE_: e.activation(out=PE_[:], in_=pl[:], func=AF.Exp, scale=SCALE), reads=[pl], writes=[PE_])
                P.op("pool", lambda e, PE_=PE_, PM=PM, st_=st_: e.tensor_tensor(out=PM[:].rearrange("p (h q) -> p h q", h=4), in0=PE_[:].rearrange("p (h q) -> p h q", h=4), in1=maskT[:, st_:st_ + 1, :].to_broadcast([128, 4, 128]), op=ALU.mult), reads=[PE_, maskT], writes=[PM])
                P.op("pe", lambda e, VT=VT, t=t, PM=PM, st_=st_, nst=nst: e.matmul(ps_o[:], lhsT=VT[:, t, :], rhs=PM[:], start=(st_ == 0), stop=(st_ == nst - 1)), reads=[VT, PM], writes=[ps_o])
                P.op("pe", lambda e, PM=PM, st_=st_, nst=nst: e.matmul(ps_s[:], lhsT=ones_b[:], rhs=PM[:], start=(st_ == 0), stop=(st_ == nst - 1)), reads=[ones_b, PM], writes=[ps_s])
            OB = ob[g % 2]
            P.op("dve", lambda e: e.reciprocal(out=rs[:], in_=ps_s[:]), reads=[ps_s], writes=[rs])
            P.op("dve", lambda e, OB=OB: e.tensor_tensor(out=OB[:], in0=ps_o[:], in1=rs[:], op=ALU.mult), reads=[ps_o, rs], writes=[OB])
            P.dma("act", oT[g * 512:(g + 1) * 512, j * 128:(j + 1) * 128].rearrange("(h d) q -> d h q", d=128), OB[:].rearrange("p (h q) -> p h q", h=4), reads=[OB])
    return


def host_B(Aout, nqt=NQT):
    maps = []
    ident = np.eye(128, dtype=np.float32).astype(BF)
    for core in range(8):
        b = core // 4; c = core % 4
        cores_b = [b * 4 + i for i in range(4)]
        qT_full = np.concatenate([Aout[i]["qT"] for i in cores_b], axis=1)
        kT_full = np.concatenate([Aout[i]["kT"] for i in cores_b], axis=1)
        v_full = np.concatenate([Aout[i]["v"] for i in cores_b], axis=0)
        ik_full = np.concatenate([Aout[i]["ikT"] for i in cores_b], axis=1)
        iq_full = np.concatenate([Aout[i]["iqT"] for i in cores_b], axis=1)
        iw_full = np.concatenate([Aout[i]["iw"] for i in cores_b], axis=0)
        sel = np.concatenate([np.arange((4 * j + c) * 128, (4 * j + c + 1) * 128) for j in range(16)])
        r = np.arange(128)[:, None]; t = np.arange(512)[None, :]
        cbias = np.where(t <= c * 128 + r, 0.0, -1e5).astype(np.float32)
        maps.append({"qTc": np.ascontiguousarray(qT_full[:, sel]), "kT": kT_full, "v": v_full, "ikT": ik_full,
                     "iqTc": np.ascontiguousarray(iq_full[:, sel]), "iwc": np.ascontiguousarray(iw_full[sel]),
                     "cbias": cbias, "ident": ident})
    return maps


def emit_O(P, io, K):
    KC = K // 128
    aT = io("aT", [K, 2048], BF16, "ExternalInput")
    resT = io("resT", [2048, 2048], F32, "ExternalInput")
    w = io("w", [K, 2048], F32, "ExternalInput")
    hT = io("hT", [2048, 2048], F32, "ExternalOutput")
    a = P.sb([128, KC, 2048], BF16)
    av = aT.rearrange("(c p) t -> p c t", p=128)
    for c0 in range(0, KC, 8):
        P.dma("sp", a[:, c0:c0 + 8, :], av[:, c0:c0 + 8, :], writes=[a])
    wb = [P.sb([128, KC, 256], BF16) for _ in range(2)]
    rs = [P.sb([128, 512], F32) for _ in range(3)]
    ob = [P.sb([128, 512], F32) for _ in range(3)]
    ps = [P.ps([128, 512]) for _ in range(3)]
    wv = w.rearrange("(c p) n -> p c n", p=128)
    n = 0
    for nc2 in range(8):
        WB = wb[nc2 % 2]
        P.dma("pool", WB[:], wv[:, :, nc2 * 256:(nc2 + 1) * 256], writes=[WB])
        for sub in range(2):
            r0 = nc2 * 256 + sub * 128
            for tt in range(4):
                ts = slice(tt * 512, (tt + 1) * 512)
                p_ = ps[n % 3]; R = rs[n % 3]; O = ob[n % 3]; n += 1
                P.dma("act", R[:], resT[r0:r0 + 128, ts], writes=[R])
                for c in range(KC):
                    P.op("pe", lambda e, c=c, p_=p_, WB=WB, sub=sub, ts=ts: e.matmul(p_[:], lhsT=WB[:, c, sub * 128:(sub + 1) * 128], rhs=a[:, c, ts], start=(c == 0), stop=(c == KC - 1)), reads=[WB, a], writes=[p_])
                P.op("dve", lambda e, p_=p_, R=R, O=O: e.tensor_tensor(out=O[:], in0=p_[:], in1=R[:], op=ALU.add), reads=[p_, R], writes=[O])
                P.dma("sp", hT[r0:r0 + 128, ts], O[:], reads=[O])
    return


def assemble_o(Bout):
    o_full = np.zeros((2, 2048, 8192), dtype=BF)
    for core in range(8):
        b = core // 4; c = core % 4
        for j in range(16):
            o_full[b][:, (4 * j + c) * 128:(4 * j + c + 1) * 128] = Bout[core][:, j * 128:(j + 1) * 128]
    return o_full


EPS = 1e-6; D = 2048; KC = 16; TG = 256; NE = 16384; EC = 256

def emit_P(P, io, want_hn, add=False, ngroups=8, nec=NE // EC, dbg=False):
    NE = nec * EC if dbg else 16384
    DBG = {}
    def dump(name, t, ap, shape):
        if not dbg or name in DBG: return
        DBG[name] = io('dbg_' + name, shape, F32, 'ExternalOutput')
        P.dma('sp', DBG[name], ap, reads=[t])
    hT = io("hT", [D, 2048], F32, "ExternalInput")
    gn = io("gn", [128, 3, KC], F32, "ExternalInput")
    w_q = io("w_q", [D, D], F32, "ExternalInput")
    keysT = io("keysT", [128, 16, 128], F32, "ExternalInput")
    uT = io("uT", [D, NE], F32, "ExternalInput")
    vt = io("vt", [NE, D], F32, "ExternalInput")
    wg = io("wg", [D, D], F32, "ExternalInput")
    wpi = io("wpi", [256, D], F32, "ExternalInput")
    pT = io("pT", [256, 2048], F32, "ExternalInput")
    identd = io("ident", [128, 128], BF16, "ExternalInput")
    h3T = io("h3T", [D, 2048], F32, "ExternalOutput")
    mixT = io("mixT", [D, 2048], F32, "ExternalInput") if add else None
    hnT = io("hnT", [D, 2048], BF16, "ExternalOutput") if want_hn else None

    G = P.sb([128, 3, KC], F32); KT = P.sb([128, 16, 128], F32); ident = P.sb([128, 128], BF16)
    WPI = P.sb([128, 2, D], BF16); ones_b = P.sb([128, 128], BF16); epsb = P.sb([128, 1], F32)
    P.dma("sp", G[:], gn[:, :, :], writes=[G]); P.dma("sp", KT[:], keysT[:, :, :], writes=[KT]); P.dma("sp", ident[:], identd[:, :], writes=[ident])
    P.dma("pool", WPI[:], wpi.rearrange("(c p) n -> p c n", p=128), writes=[WPI])
    P.op("dve", lambda e: e.memset(ones_b[:], 1.0), writes=[ones_b])
    P.op("dve", lambda e: e.memset(epsb[:], EPS), writes=[epsb])

    H = P.sb([128, KC, TG], F32); XY = P.sb([128, KC, TG], F32); QP = P.sb([128, 16, TG], F32); NB = P.sb([128, KC, TG], BF16)
    rstd = P.sb([128, TG], F32)
    S = [P.sb([128, 16, 128], F32) for _ in range(2)]
    Cc = [P.sb([128, 8, 128], F32) for _ in range(2)]
    W1 = [P.sb([128, 8, 128], F32) for _ in range(2)]
    E2 = [P.sb([128, 8, 128], F32) for _ in range(2)]
    t16 = P.sb([128, 16, 16], F32); wk = P.sb([128, 256], F32); cand = P.sb([128, 8, 256], F32); f16 = P.sb([128, 8, 16], F32)
    thr = P.sb([128, 8], F32); fmax = P.sb([128, 8], F32); ex = P.sb([128, 8, 16], F32); Z = P.sb([128, 8], F32); rZ = P.sb([128, 8], F32)
    ub = [P.sb([128, KC, EC], BF16) for _ in range(2)]
    vb = [P.sb([128, EC // 128, D], BF16) for _ in range(2)]
    gel = [P.sb([128, EC], F32) for _ in range(2)]
    NI = EC // 128
    AA = [P.sb([128, 8, NI, 128], F32) for _ in range(2)]
    Gb = [[P.sb([128, NI, 128], F32) for _ in range(2)] for _ in range(2)]
    identf = P.sb([128, 128], F32)
    P.op("dve", lambda e: e.tensor_copy(out=identf[:], in_=ident[:]), reads=[ident], writes=[identf])
    coef = [P.sb([128, EC], BF16) for _ in range(2)]
    coefT = [P.sb([128, NI, TG], BF16) for _ in range(2)]
    wq = P.sb([128, KC, 128], F32)
    wgb = [P.sb([128, KC, 128], BF16) for _ in range(2)]
    PT = P.sb([128, 2, TG], BF16)
    gate = [P.sb([128, TG], F32) for _ in range(2)]; gtmp = [P.sb([128, TG], F32) for _ in range(2)]
    HN = NB if want_hn else None
    ps_ms = P.ps([128, 512]); ps_a = P.ps([128, 512]); ps_b = P.ps([128, 512]); ps_tr = P.ps([128, 2, NI, 128], BF16)
    ps4 = [P.ps([128, 512]) for _ in range(4)]
    hv = hT.rearrange("(c p) t -> p c t", p=128)
    h3v = h3T.rearrange("(c p) t -> p c t", p=128)
    hnv = hnT.rearrange("(c p) t -> p c t", p=128) if want_hn else None
    wqv = w_q.rearrange("(c p) n -> p c n", p=128)
    wgv = wg.rearrange("(c p) n -> p c n", p=128)
    uv = uT.rearrange("(c p) e -> p c e", p=128)
    ptv = pT.rearrange("(c p) t -> p c t", p=128)

    def rmsnorm(src, which, out32, outb):
        P.op("act", lambda e: e.activation(out=outb[:], in_=src[:], func=AF.Square), reads=[src], writes=[outb])
        for c in range(KC):
            P.op("pe", lambda e, c=c: e.matmul(ps_ms[:, 0:TG], lhsT=ones_b[:], rhs=outb[:, c, :], start=(c == 0), stop=(c == KC - 1)), reads=[ones_b, outb], writes=[ps_ms])
        P.op("act", lambda e: e.activation(out=rstd[:], in_=ps_ms[:, 0:TG], func=AF.Sqrt, bias=epsb[:, 0:1], scale=1.0 / D), reads=[ps_ms, epsb], writes=[rstd])
        P.op("dve", lambda e: e.reciprocal(out=rstd[:], in_=rstd[:]), reads=[rstd], writes=[rstd])
        for c in range(KC):
            if out32 is not None:
                P.op("dve", lambda e, c=c: e.scalar_tensor_tensor(out=out32[:, c, :], in0=src[:, c, :], scalar=G[:, which, c:c + 1], in1=rstd[:], op0=ALU.mult, op1=ALU.mult), reads=[src, G, rstd], writes=[out32])
            else:
                P.op("dve", lambda e, c=c: e.scalar_tensor_tensor(out=outb[:, c, :], in0=src[:, c, :], scalar=G[:, which, c:c + 1], in1=rstd[:], op0=ALU.mult, op1=ALU.mult), reads=[src, G, rstd], writes=[outb])
        if out32 is not None:
            P.op("act", lambda e: e.activation(out=outb[:], in_=out32[:], func=AF.Copy), reads=[out32], writes=[outb])

    n_w = 0
    for gi in range(ngroups):
        gs = slice(gi * TG, (gi + 1) * TG)
        P.dma("sp", H[:], hv[:, :, gs], writes=[H])
        P.dma("pool", PT[:], ptv[:, :, gs], writes=[PT])
        if add:
            P.dma("act", QP[:], mixT.rearrange("(c p) t -> p c t", p=128)[:, :, gs], writes=[QP])
            P.op("dve", lambda e: e.tensor_tensor(out=H[:], in0=H[:], in1=QP[:], op=ALU.add), reads=[H, QP], writes=[H])
        rmsnorm(H, 0, XY, NB)
        for n in range(16):
            P.dma("sp", wq[:], wqv[:, :, n * 128:(n + 1) * 128], writes=[wq])
            pq = ps_a if n % 2 == 0 else ps_b
            for c in range(KC):
                P.op("pe", lambda e, c=c, pq=pq: e.matmul(pq[:, 0:TG], lhsT=wq[:, c, :], rhs=XY[:, c, :], start=(c == 0), stop=(c == KC - 1)), reads=[wq, XY], writes=[pq])
            P.op("act", lambda e, n=n, pq=pq: e.activation(out=QP[:, n, :], in_=pq[:, 0:TG], func=AF.Copy), reads=[pq], writes=[QP])
        for tl in range(2):
            tsl = slice(tl * 128, (tl + 1) * 128)
            St = S[tl]; Ct = Cc[tl]; W1t = W1[tl]; E2t = E2[tl]
            for hp in range(16):
                pb = ps4[hp // 4]
                P.op("pe", lambda e, hp=hp, pb=pb, tsl=tsl: e.matmul(pb[:, (hp % 4) * 128:(hp % 4 + 1) * 128], lhsT=QP[:, hp, tsl], rhs=KT[:, hp, :], start=True, stop=True), reads=[QP, KT], writes=[pb])
            for q4 in range(4):
                P.op("act", lambda e, q4=q4, St=St: e.activation(out=St[:, q4 * 4:(q4 + 1) * 4, :], in_=ps4[q4][:].rearrange("p (a b) -> p a b", a=4), func=AF.Copy), reads=[ps4[q4]], writes=[St])
            for hp in range(16):
                P.op("dve", lambda e, hp=hp, St=St: e.max(out=t16[:, hp, 0:8], in_=St[:, hp, :]), reads=[St], writes=[t16])
                P.op("dve", lambda e, hp=hp, St=St: e.match_replace(out=wk[:, 0:128], in_to_replace=t16[:, hp, 0:8], in_values=St[:, hp, :], imm_value=-1e30), reads=[St, t16], writes=[wk])
                P.op("dve", lambda e, hp=hp: e.max(out=t16[:, hp, 8:16], in_=wk[:, 0:128]), reads=[wk], writes=[t16])
            for h in range(8):
                P.op("dve", lambda e, h=h: e.tensor_tensor(out=cand[:, h, :].rearrange("p (a b) -> p a b", a=16), in0=t16[:, 2 * h, :].unsqueeze(2).to_broadcast([128, 16, 16]), in1=t16[:, 2 * h + 1, :].unsqueeze(1).to_broadcast([128, 16, 16]), op=ALU.add), reads=[t16], writes=[cand])
            for h in range(8):
                P.op("dve", lambda e, h=h: e.max(out=f16[:, h, 0:8], in_=cand[:, h, :]), reads=[cand], writes=[f16])
                P.op("dve", lambda e, h=h: e.match_replace(out=wk[:], in_to_replace=f16[:, h, 0:8], in_values=cand[:, h, :], imm_value=-1e30), reads=[cand, f16], writes=[wk])
                P.op("dve", lambda e, h=h: e.max(out=f16[:, h, 8:16], in_=wk[:]), reads=[wk], writes=[f16])
            P.op("dve", lambda e: e.tensor_reduce(out=thr[:], in_=f16[:], axis=AX.X, op=ALU.min), reads=[f16], writes=[thr])
            P.op("dve", lambda e: e.tensor_scalar(out=thr[:], in0=thr[:], scalar1=-2e-5, scalar2=None, op0=ALU.add), reads=[thr], writes=[thr])
            t16v = t16[:].rearrange("p (h two) k -> p h two k", two=2)
            P.op("dve", lambda e, t16v=t16v: e.tensor_tensor(out=fmax[:], in0=t16v[:, :, 0, 0], in1=t16v[:, :, 1, 0], op=ALU.add), reads=[t16], writes=[fmax])
            P.op("dve", lambda e: e.tensor_tensor(out=ex[:], in0=f16[:], in1=fmax[:].unsqueeze(2).to_broadcast([128, 8, 16]), op=ALU.subtract), reads=[f16, fmax], writes=[ex])
            P.op("act", lambda e: e.activation(out=ex[:], in_=ex[:], func=AF.Exp), reads=[ex], writes=[ex])
            P.op("dve", lambda e: e.tensor_reduce(out=Z[:], in_=ex[:], axis=AX.X, op=ALU.add), reads=[ex], writes=[Z])
            P.op("dve", lambda e: e.reciprocal(out=rZ[:], in_=Z[:]), reads=[Z], writes=[rZ])
            S4 = St[:].rearrange("p (h two) k -> p h two k", two=2)
            P.op("dve", lambda e, S4=S4, Ct=Ct: e.tensor_tensor(out=Ct[:], in0=thr[:].unsqueeze(2).to_broadcast([128, 8, 128]), in1=S4[:, :, 0, :], op=ALU.subtract), reads=[thr, St], writes=[Ct])
            P.op("dve", lambda e, S4=S4, W1t=W1t, t16v=t16v: e.tensor_tensor(out=W1t[:], in0=S4[:, :, 0, :], in1=t16v[:, :, 0, 0:1].to_broadcast([128, 8, 128]), op=ALU.subtract), reads=[St, t16], writes=[W1t])
            P.op("act", lambda e, W1t=W1t: e.activation(out=W1t[:], in_=W1t[:], func=AF.Exp), reads=[W1t], writes=[W1t])
            P.op("dve", lambda e, W1t=W1t: e.tensor_tensor(out=W1t[:], in0=W1t[:], in1=rZ[:].unsqueeze(2).to_broadcast([128, 8, 128]), op=ALU.mult), reads=[W1t, rZ], writes=[W1t])
            P.op("dve", lambda e, S4=S4, E2t=E2t, t16v=t16v: e.tensor_tensor(out=E2t[:], in0=S4[:, :, 1, :], in1=t16v[:, :, 1, 0:1].to_broadcast([128, 8, 128]), op=ALU.subtract), reads=[St, t16], writes=[E2t])
            P.op("act", lambda e, E2t=E2t: e.activation(out=E2t[:], in_=E2t[:], func=AF.Exp), reads=[E2t], writes=[E2t])
            if tl == 0 and gi == 0:
                dump('S', St, St[:], [128, 16, 128]); dump('t16', t16, t16[:], [128, 16, 16]); dump('f16', f16, f16[:], [128, 8, 16]); dump('thr', thr, thr[:], [128, 8]); dump('fmax', fmax, fmax[:], [128, 8]); dump('rZ', rZ, rZ[:], [128, 8])
                dump('C', Ct, Ct[:], [128, 8, 128]); dump('W1', W1t, W1t[:], [128, 8, 128]); dump('E2', E2t, E2t[:], [128, 8, 128]); dump('cand', cand, cand[:], [128, 8, 256])
        def g1(ec):
            i0 = ec * NI
            for tl in range(2):
                St = S[tl]; Ct = Cc[tl]
                S4 = St[:].rearrange("p (h two) k -> p h two k", two=2)
                AAt = AA[tl]
                P.op("dve", lambda e, AAt=AAt, S4=S4, Ct=Ct, i0=i0: e.tensor_tensor(out=AAt[:], in0=S4[:, :, 1, :].unsqueeze(2).to_broadcast([128, 8, NI, 128]), in1=Ct[:, :, i0:i0 + NI].unsqueeze(3).to_broadcast([128, 8, NI, 128]), op=ALU.is_ge), reads=[St, Ct], writes=[AAt])
            for tl in range(2):
                W1t = W1[tl]; E2t = E2[tl]; AAt = AA[tl]
                P.op("pool", lambda e, AAt=AAt, E2t=E2t: e.tensor_tensor(out=AAt[:], in0=AAt[:], in1=E2t[:].unsqueeze(2).to_broadcast([128, 8, NI, 128]), op=ALU.mult), reads=[AAt, E2t], writes=[AAt])
                P.op("pool", lambda e, AAt=AAt, W1t=W1t, i0=i0: e.tensor_tensor(out=AAt[:], in0=AAt[:], in1=W1t[:, :, i0:i0 + NI].unsqueeze(3).to_broadcast([128, 8, NI, 128]), op=ALU.mult), reads=[AAt, W1t], writes=[AAt])

        def g2(par):
            for tl in range(2):
                AAt = AA[tl]; GB = Gb[par][tl]
                P.op("dve", lambda e, AAt=AAt, GB=GB: e.tensor_reduce(out=GB[:], in_=AAt[:].rearrange("p h i j -> p i j h"), axis=AX.X, op=ALU.add), reads=[AAt], writes=[GB])
        g1(0); g2(0)
        Yv = XY[:].rearrange("p c t -> p (c t)").rearrange("p (tl d) -> p tl d", tl=2)

        def wload(ec):
            P.dma("pool", ub[ec % 2][:], uv[:, :, ec * EC:(ec + 1) * EC], writes=[ub[ec % 2]])
            P.dma("pool", vb[ec % 2][:], vt[ec * EC:(ec + 1) * EC, :].rearrange("(b p) d -> p b d", p=128), writes=[vb[ec % 2]])
        wload(0)
        for ec in range(nec):
            par = ec % 2
            UB = ub[par]; VB = vb[par]; CT = coefT[par]
            if ec + 1 < nec:
                wload(ec + 1)
                g1(ec + 1)
            for tl in range(2):
                tsl = slice(tl * 128, (tl + 1) * 128)
                pa = ps4[tl]
                for c in range(KC):
                    P.op("pe", lambda e, c=c, pa=pa, UB=UB, tsl=tsl: e.matmul(pa[:, 0:EC], lhsT=NB[:, c, tsl], rhs=UB[:, c, :], start=(c == 0), stop=(c == KC - 1)), reads=[NB, UB], writes=[pa])
            for tl in range(2):
                pa = ps4[tl]; GL = gel[tl]
                P.op("act", lambda e, pa=pa, GL=GL: e.activation(out=GL[:], in_=pa[:, 0:EC], func=AF.Gelu), reads=[pa], writes=[GL])
            for tl in range(2):
                GL = gel[tl]; GB = Gb[par][tl]; CF = coef[tl]
                P.op("dve", lambda e, GL=GL, GB=GB, CF=CF: e.tensor_tensor(out=CF[:], in0=GL[:], in1=GB[:].rearrange("p a b -> p (a b)"), op=ALU.mult), reads=[GL, GB], writes=[CF])
                if tl == 0 and gi == 0 and ec == 0:
                    dump('GL', GL, GL[:], [128, EC]); dump('GB', GB, GB[:], [128, NI, 128])
            for tl in range(2):
                CF = coef[tl]
                for b in range(NI):
                    P.op("pe", lambda e, b=b, CF=CF, tl=tl: e.transpose(out=ps_tr[:, tl, b, :], in_=CF[:, b * 128:(b + 1) * 128], identity=ident[:]), reads=[CF, ident], writes=[ps_tr])
            P.op("act", lambda e, CT=CT: e.activation(out=CT[:].rearrange("p b (tl t) -> p tl b t", tl=2), in_=ps_tr[:], func=AF.Copy), reads=[ps_tr], writes=[CT])
            k_ = 0
            for tl in range(2):
                tsl = slice(tl * 128, (tl + 1) * 128)
                for d4 in range(4):
                    py = ps4[2 + k_ % 2]; k_ += 1
                    for b in range(NI):
                        P.op("pe", lambda e, b=b, d4=d4, py=py, VB=VB, CT=CT, tsl=tsl: e.matmul(py[:], lhsT=CT[:, b, tsl], rhs=VB[:, b, d4 * 512:(d4 + 1) * 512], start=(b == 0), stop=(b == NI - 1)), reads=[VB, CT], writes=[py])
                    ysl = Yv[:, tl, d4 * 512:(d4 + 1) * 512]
                    if ec == 0:
                        P.op("act", lambda e, py=py, ysl=ysl: e.activation(out=ysl, in_=py[:], func=AF.Copy), reads=[py], writes=[XY])
                    else:
                        P.op("dve", lambda e, py=py, ysl=ysl: e.tensor_tensor(out=ysl, in0=ysl, in1=py[:], op=ALU.add), reads=[py, XY], writes=[XY])
            if ec + 1 < nec:
                g2(1 - par)
        if gi == 0:
            dump('Y', XY, XY[:], [128, KC, TG])
        k_ = 0
        for tl in range(2):
            tsl = slice(tl * 128, (tl + 1) * 128)
            for dc4 in range(4):
                py = ps4[k_ % 4]; k_ += 1
                for q4 in range(4):
                    dc = dc4 * 4 + q4
                    P.op("pe", lambda e, py=py, q4=q4, dc=dc, tl=tl: e.transpose(out=py[:, q4 * 128:(q4 + 1) * 128], in_=Yv[:, tl, dc * 128:(dc + 1) * 128], identity=identf[:]), reads=[XY, identf], writes=[py])
                P.op("dve", lambda e, py=py, dc4=dc4, tsl=tsl: e.tensor_tensor(out=H[:, dc4 * 4:(dc4 + 1) * 4, tsl], in0=H[:, dc4 * 4:(dc4 + 1) * 4, tsl], in1=py[:].rearrange("p (a b) -> p a b", a=4), op=ALU.add), reads=[py, H], writes=[H])
        rmsnorm(H, 1, None, NB)
        for n in range(16):
            WG = wgb[n_w % 2]; GA = gate[n_w % 2]; GT = gtmp[n_w % 2]; n_w += 1
            P.dma("pool", WG[:], wgv[:, :, n * 128:(n + 1) * 128], writes=[WG])
            pg = ps_a if n % 2 == 0 else ps_b
            for c in range(KC):
                P.op("pe", lambda e, c=c, pg=pg, WG=WG: e.matmul(pg[:, 0:TG], lhsT=WG[:, c, :], rhs=NB[:, c, :], start=(c == 0), stop=(c == KC - 1)), reads=[WG, NB], writes=[pg])
            for c in range(2):
                P.op("pe", lambda e, c=c, pg=pg, n=n: e.matmul(pg[:, TG:2 * TG], lhsT=WPI[:, c, n * 128:(n + 1) * 128], rhs=PT[:, c, :], start=(c == 0), stop=(c == 1)), reads=[WPI, PT], writes=[pg])
            P.op("act", lambda e, pg=pg, GA=GA: e.activation(out=GA[:], in_=pg[:, 0:TG], func=AF.Sigmoid), reads=[pg], writes=[GA])
            P.op("dve", lambda e, pg=pg, GA=GA, GT=GT: e.tensor_tensor(out=GT[:], in0=GA[:], in1=pg[:, TG:2 * TG], op=ALU.mult), reads=[pg, GA], writes=[GT])
            P.op("pool", lambda e, n=n, GT=GT: e.tensor_tensor(out=H[:, n, :], in0=H[:, n, :], in1=GT[:], op=ALU.add), reads=[H, GT], writes=[H])
        P.dma("sp", h3v[:, :, gs], H[:], reads=[H])
        if want_hn:
            rmsnorm(H, 2, None, HN)
            P.dma("sp", hnv[:, :, gs], HN[:], reads=[HN])
    return


def host_P(hT_list, layer, z, want_next):
    maps = []
    ident = np.eye(128, dtype=np.float32).astype(BF)
    gnext = z['norm_mix'][layer + 1] if want_next else np.ones(D, np.float32)
    gn = np.ascontiguousarray(np.stack([z['norm_ffn'][layer].reshape(KC, 128).T, z['norm_ple'][layer].reshape(KC, 128).T, gnext.reshape(KC, 128).T], axis=1))
    keysT = np.ascontiguousarray(z['peer_keys'][layer].reshape(16, 128, 128).transpose(2, 0, 1))
    uT = np.ascontiguousarray(z['peer_u'][layer].T)
    for core in range(8):
        b = core // 4; t0 = (core % 4) * 2048
        maps.append({"hT": hT_list[core], "gn": gn, "w_q": z['peer_w_q'][layer], "keysT": keysT, "uT": uT, "vt": z['peer_v'][layer],
                     "wg": z['ple_w_gate'][layer], "wpi": z['ple_w_in'][layer], "pT": np.ascontiguousarray(z['p'][layer, b, t0:t0 + 2048].T), "ident": ident})
    return maps


EPS = 1e-6; KC = 16; ST = 256; NW = 3088

def emit_D(P, io, nst=32, maxphase=9):
    hnT = io("hnT", [8192, 2048], BF16, "ExternalInput")
    wc = io("wc", [2048, NW], F32, "ExternalInput")
    convw = io("convw", [128, 16, 4], F32, "ExternalInput")
    hc = io("hc", [128, 2, 8], F32, "ExternalInput")
    gnrow = io("gnrow", [128, 128], F32, "ExternalInput")
    masks = io("masks", [128, 6, 128], F32, "ExternalInput")
    og = io("og", [1024, 8192], BF16, "ExternalOutput")

    W = P.sb([128, KC, NW], BF16)
    wv = wc.rearrange("(c p) n -> p c n", p=128)
    for c0 in range(0, KC, 4):
        for n0 in range(0, NW, 512):
            n1 = min(NW, n0 + 512)
            P.dma("pool", W[:, c0:c0 + 4, n0:n1], wv[:, c0:c0 + 4, n0:n1], writes=[W])
    CW = P.sb([128, 16, 4], F32); HC = P.sb([128, 2, 8], F32); GN = P.sb([128, 128], F32); MK = P.sb([128, 6, 128], F32)
    P.dma("sp", CW[:], convw[:, :, :], writes=[CW]); P.dma("sp", HC[:], hc[:, :, :], writes=[HC]); P.dma("sp", GN[:], gnrow[:, :], writes=[GN]); P.dma("sp", MK[:], masks[:, :, :], writes=[MK])
    ident = MK[:, 0, :]; Lm = MK[:, 1, :]; Bo = MK[:, 2, :]; Um = MK[:, 3, :]; NUs = MK[:, 4, :]; cmask = MK[:, 5, 0:2]
    ones_f = P.sb([128, 128], F32); epsb = P.sb([128, 1], F32); nega = P.sb([128, 8], F32); one1 = P.sb([128, 1], F32)
    P.op("dve", lambda e: e.memset(ones_f[:], 1.0), writes=[ones_f])
    P.op("dve", lambda e: e.memset(epsb[:], EPS), writes=[epsb])
    P.op("dve", lambda e: e.memset(one1[:], 1.0), writes=[one1])
    P.op("act", lambda e: e.activation(out=nega[:], in_=HC[:, 0, :], func=AF.Exp), reads=[HC], writes=[nega])
    P.op("dve", lambda e: e.tensor_scalar(out=nega[:], in0=nega[:], scalar1=-1.0, scalar2=None, op0=ALU.mult), reads=[nega], writes=[nega])

    HN = [P.sb([128, KC, ST], BF16) for _ in range(1)]
    PJ = [P.sb([128, ST + 3], F32) for _ in range(2)]
    HALO = P.sb([128, 16, 3], F32)
    CO = [P.sb([128, ST], F32) for _ in range(16)]
    P.op("pool", lambda e: e.memset(HALO[:], 0.0), writes=[HALO])
    sqt = [P.sb([128, ST], F32) for _ in range(1)]; rst = [P.sb([128, ST], F32) for _ in range(1)]
    ZS = [P.sb([128, 1024], F32) for _ in range(1)]
    BA = [P.sb([128, 16], F32) for _ in range(2)]; BETA = [P.sb([128, 8], F32) for _ in range(2)]; GG = [P.sb([128, 8], F32) for _ in range(2)]
    GC = P.sb([128, 8], F32); GL = P.sb([128, 8], F32); gsel = P.sb([128, 8, 2], F32); EGL = P.sb([128, 16], F32)
    EGC = P.sb([128, 8], F32); BG = P.sb([128, 8], F32); KD = P.sb([128, 8], F32)
    KTM = P.sb([128, 4, 128], F32); VTM = P.sb([128, 8, 128], F32)
    KK = [P.sb([128, 128], F32) for _ in range(4)]; QK = [P.sb([128, 128], F32) for _ in range(4)]
    def mk2(n=2): return [P.sb([128, 128], F32) for _ in range(n)]
    DGB = [P.sb([128, 256], F32) for _ in range(2)]
    DTt = mk2(); DT = mk2(); EGR = mk2(); BR = mk2(); T1 = mk2(); T2 = T1
    XA = mk2(4); XAT = mk2(4); XB = mk2(4); XBT = mk2(4); RR = mk2(4)
    VB = mk2(4); KBG = mk2(4)
    QKm = mk2(8); Usb = mk2(8); WT = mk2(8); QG = mk2(8); KG = mk2(8); VN = mk2(8)
    Sst = mk2(8)
    for h in range(8):
        P.op("pool", lambda e, h=h: e.memset(Sst[h][:], 0.0), writes=[Sst[h]])
    Osb = P.sb([128, 8, 128], F32);  MS = P.sb([128, 8], F32); O2 = P.sb([128, 8, 128], F32); SQo = O2
    OB = [P.sb([128, 1024], BF16) for _ in range(1)]
    OT = P.sb([128, 8, 128], BF16); identb = P.sb([128, 128], BF16)
    P.op("dve", lambda e: e.tensor_copy(out=identb[:], in_=ident), reads=[MK], writes=[identb])
    bankT = [P.ps([128, 512]) for _ in range(8)]
    pjps = [TV(bankT[0], bankT[0][:, 0:256], "pjA"), TV(bankT[1], bankT[1][:, 0:256], "pjB")]
    zps = bankT[2]
    smalls = [TV(bankT[3], bankT[3][:, i * 128:(i + 1) * 128], f"sm{i}") for i in range(4)]
    rowsps = [TV(bankT[4], bankT[4][:, 0:256], "rowsA"), TV(bankT[5], bankT[5][:, 0:256], "rowsB")]
    rotl = [TV(bankT[b_], bankT[b_][:, q_ * 128:(q_ + 1) * 128], f"rot{q_}_{b_}") for q_ in range(4) for b_ in (6, 7, 0, 1, 2)]
    rc = [0]; sc = [0]
    def rot():
        t = rotl[rc[0] % 20]; rc[0] += 1; return t
    def sm():
        t = smalls[sc[0] % 4]; sc[0] += 1; return t

    def mm(o, oap, lhsT, rhs, reads, start=True, stop=True):
        P.op("pe", lambda e: e.matmul(oap, lhsT=lhsT, rhs=rhs, start=start, stop=stop), reads=reads, writes=[o])
    def tp(o, oap, in_, reads):
        P.op("pe", lambda e: e.transpose(out=oap, in_=in_, identity=ident), reads=reads + [MK], writes=[o])
    def cp(eng, o, oap, iap, reads):
        if eng == "act":
            P.op("act", lambda e: e.activation(out=oap, in_=iap, func=AF.Copy), reads=reads, writes=[o])
        else:
            P.op(eng, lambda e: e.tensor_copy(out=oap, in_=iap), reads=reads, writes=[o])
    def act(o, oap, iap, func, reads, bias=None, scale=None):
        kw = {}
        if bias is not None: kw["bias"] = bias
        if scale is not None: kw["scale"] = scale
        P.op("act", lambda e: e.activation(out=oap, in_=iap, func=func, **kw), reads=reads, writes=[o])
    def tt(eng, o, oap, a, b, op, reads):
        P.op(eng, lambda e: e.tensor_tensor(out=oap, in0=a, in1=b, op=op), reads=reads, writes=[o])
    def ts(eng, o, oap, a, s1, s2, op0, op1, reads):
        if op1 is None:
            P.op(eng, lambda e: e.tensor_scalar(out=oap, in0=a, scalar1=s1, scalar2=None, op0=op0), reads=reads, writes=[o])
        else:
            P.op(eng, lambda e: e.tensor_scalar(out=oap, in0=a, scalar1=s1, scalar2=s2, op0=op0, op1=op1), reads=reads, writes=[o])
    def stt(eng, o, oap, a, s, b, op0, op1, reads):
        P.op(eng, lambda e: e.scalar_tensor_tensor(out=oap, in0=a, scalar=s, in1=b, op0=op0, op1=op1), reads=reads, writes=[o])

    for st in range(nst):
        H = HN[0]
        for tl_ in range(2):
            tile_ = st * 2 + tl_; j_ = tile_ // 4; r_ = tile_ % 4
            for k_ in range(8):
                P.dma("sp", H[:, 2 * k_:2 * k_ + 2, tl_ * 128:(tl_ + 1) * 128], hnT[k_ * 1024 + r_ * 256:k_ * 1024 + (r_ + 1) * 256, j_ * 128:(j_ + 1) * 128].rearrange("(two p) t -> p two t", p=128), writes=[H])
        for ch in range(16):
            pj = pjps[ch % 2]; pjt = PJ[ch % 2]; co = CO[ch]
            for c in range(KC):
                mm(pj, pj[:], W[:, c, ch * 128:(ch + 1) * 128], H[:, c, :], [W, H], start=(c == 0), stop=(c == KC - 1))
            cp("pool", pjt, pjt[:, 0:3], HALO[:, ch, :], [HALO])
            cp("act", pjt, pjt[:, 3:ST + 3], pj[:], [pj])
            ts("dve", co, co[:], pjt[:, 3:ST + 3], CW[:, ch, 3:4], None, ALU.mult, None, [pjt, CW])
            for j in (2, 1, 0):
                stt("dve", co, co[:], pjt[:, j:j + ST], CW[:, ch, j:j + 1], co[:], ALU.mult, ALU.add, [pjt, CW, co])
            cp("pool", HALO, HALO[:, ch, :], pjt[:, ST:ST + 3], [pjt])
            act(co, co[:], co[:], AF.Silu, [co])
            if ch < 8:
                sq = sqt[0]; rs = rst[0]; ss = rowsps[ch % 2]
                act(sq, sq[:], co[:], AF.Square, [co])
                mm(ss, ss[:], ones_f[:], sq[:], [ones_f, sq])
                act(rs, rs[:], ss[:], AF.Sqrt, [ss, epsb], bias=epsb[:, 0:1], scale=1.0)
                P.op("dve", lambda e, rs=rs: e.reciprocal(out=rs[:], in_=rs[:]), reads=[rs], writes=[rs])
                if ch < 4:
                    stt("dve", co, co[:], co[:], 128 ** -0.5, rs[:], ALU.mult, ALU.mult, [co, rs])
                else:
                    tt("dve", co, co[:], co[:], rs[:], ALU.mult, [co, rs])
        if maxphase < 2: continue
        for tl in range(2):
            cs = slice(tl * 128, (tl + 1) * 128)
            q = sm()
            for c in range(KC):
                mm(q, q[:, 0:16], H[:, c, cs], W[:, c, 3072:3088], [W, H], start=(c == 0), stop=(c == KC - 1))
            cp("act", BA[tl], BA[tl][:], q[:, 0:16], [q])
            act(BETA[tl], BETA[tl][:], BA[tl][:, 0:8], AF.Sigmoid, [BA[tl]])
            tt("dve", GG[tl], GG[tl][:], BA[tl][:, 8:16], HC[:, 1, :], ALU.add, [BA[tl], HC])
            act(GG[tl], GG[tl][:], GG[tl][:], AF.Exp, [GG[tl]])
            act(GG[tl], GG[tl][:], GG[tl][:], AF.Ln, [GG[tl], one1], bias=one1[:, 0:1], scale=1.0)
            tt("dve", GG[tl], GG[tl][:], GG[tl][:], nega[:], ALU.mult, [GG[tl], nega])
        if maxphase < 3: continue
        for tl in range(2):
            cs = slice(tl * 128, (tl + 1) * 128)
            g = GG[tl]; beta = BETA[tl]
            q = sm(); mm(q, q[:, 0:8], Lm, g[:], [MK, g]); cp("act", GC, GC[:], q[:, 0:8], [q])
            q = sm(); mm(q, q[:, 0:8], Bo, g[:], [MK, g]); cp("act", GL, GL[:], q[:, 0:8], [q])
            tt("dve", gsel, gsel[:], g[:].unsqueeze(2).to_broadcast([128, 8, 2]), cmask.unsqueeze(1).to_broadcast([128, 8, 2]), ALU.mult, [g, MK])
            q = sm(); mm(q, q[:, 0:16], ones_f[:], gsel[:].rearrange("p a b -> p (a b)"), [ones_f, gsel]); act(EGL, EGL[:], q[:, 0:16], AF.Exp, [q])
            act(EGC, EGC[:], GC[:], AF.Exp, [GC])
            tt("dve", BG, BG[:], beta[:], EGC[:], ALU.mult, [beta, EGC])
            tt("dve", KD, KD[:], GL[:], GC[:], ALU.subtract, [GL, GC])
            act(KD, KD[:], KD[:], AF.Exp, [KD])
            if maxphase < 4: continue
            for hq in range(4):
                r = rot(); tp(r, r[:], CO[4 + hq][:, cs], [CO[4 + hq]]); cp("act", KTM, KTM[:, hq, :], r[:], [r])
            for hv_ in range(8):
                r = rot(); tp(r, r[:], CO[8 + hv_][:, cs], [CO[8 + hv_]]); cp("dve" if hv_ % 2 else "act", VTM, VTM[:, hv_, :], r[:], [r])
            for hq in range(4):
                r = rot(); mm(r, r[:], CO[4 + hq][:, cs], CO[4 + hq][:, cs], [CO[4 + hq]]); cp("act", KK[hq], KK[hq][:], r[:], [r])
                r = rot(); mm(r, r[:], CO[4 + hq][:, cs], CO[hq][:, cs], [CO[4 + hq], CO[hq]]); cp("dve", QK[hq], QK[hq][:], r[:], [r])
            if maxphase < 5: continue
            for hb in range(2):
                heads = list(range(4 * hb, 4 * hb + 4))
                for h in heads:
                    hq = h // 2; par = h % 2; b4 = h % 4
                    dgb = DGB[par]; rows = rowsps[par]
                    ts("dve", dgb, dgb[:, 0:128], ident, GC[:, h:h + 1], None, ALU.mult, None, [MK, GC])
                    ts("dve", dgb, dgb[:, 128:256], ident, beta[:, h:h + 1], None, ALU.mult, None, [MK, beta])
                    mm(rows, rows[:], ones_f[:], dgb[:], [ones_f, dgb])
                    ts("dve", DTt[par], DTt[par][:], rows[:, 0:128], GC[:, h:h + 1], 0.0, ALU.subtract, ALU.min, [rows, GC])
                    act(DTt[par], DTt[par][:], DTt[par][:], AF.Exp, [DTt[par]])
                    tt("pool", DT[par], DT[par][:], DTt[par][:], Um, ALU.mult, [DTt[par], MK])
                    act(EGR[par], EGR[par][:], rows[:, 0:128], AF.Exp, [rows])
                    cp("act", BR[par], BR[par][:], rows[:, 128:256], [rows])
                    tt("pool", T1[par], T1[par][:], KK[hq][:], BR[par][:], ALU.mult, [KK[hq], BR[par]])
                    tt("dve", T2[par], T2[par][:], T1[par][:], DT[par][:], ALU.mult, [T1[par], DT[par]])
                    x0 = XA[b4]; x0t = XAT[b4]; R = RR[b4]
                    tt("pool", x0, x0[:], T2[par][:], NUs, ALU.mult, [T2[par], MK])
                    tt("dve", QKm[h], QKm[h][:], QK[hq][:], DT[par][:], ALU.mult, [QK[hq], DT[par]])
                    r = rot(); tp(r, r[:], x0[:], [x0]); cp("act", x0t, x0t[:], r[:], [r])
                    tt("pool", R, R[:], x0[:], ident, ALU.add, [x0, MK])
                    ts("dve", VB[b4], VB[b4][:], VTM[:, h, :], beta[:, h:h + 1], None, ALU.mult, None, [VTM, beta])
                    ts("dve", KBG[b4], KBG[b4][:], KTM[:, hq, :], BG[:, h:h + 1], None, ALU.mult, None, [KTM, BG])
                    tt("pool", QG[h], QG[h][:], CO[hq][:, cs], EGR[par][:], ALU.mult, [CO[hq], EGR[par]])
                    ts("dve", KG[h], KG[h][:], KTM[:, hq, :], KD[:, h:h + 1], None, ALU.mult, None, [KTM, KD])
                for n in range(5):
                    last = n == 4
                    for h in heads:
                        b4 = h % 4
                        xc, xct = ((XA, XAT) if n % 2 == 0 else (XB, XBT))
                        xn, xnt = ((XB, XBT) if n % 2 == 0 else (XA, XAT))
                        xc = xc[b4]; xct = xct[b4]; xn = xn[b4]; xnt = xnt[b4]; R = RR[b4]
                        if not last:
                            r = rot(); mm(r, r[:], xct[:], xc[:], [xct, xc]); cp("act", xn, xn[:], r[:], [r])
                        r = rot(); mm(r, r[:], xc[:], xct[:], [xct, xc]); cp("dve", xnt, xnt[:], r[:], [r])
                    for h in heads:
                        b4 = h % 4
                        xnt = ((XBT) if n % 2 == 0 else (XAT))[b4]; R = RR[b4]
                        r = rot(); mm(r, r[:], xnt[:], R[:], [xnt, R]); tt("dve", R, R[:], R[:], r[:], ALU.add, [R, r])
                for h in heads:
                    b4 = h % 4; R = RR[b4]
                    r = rot(); mm(r, r[:], R[:], VB[b4][:], [R, VB[b4]]); cp("act", Usb[h], Usb[h][:], r[:], [r])
                    r = rot(); mm(r, r[:], KBG[b4][:], R[:], [R, KBG[b4]]); cp("act", WT[h], WT[h][:], r[:], [r])
            if maxphase < 6: continue
            for i in range(2):
                rr = slice(64 * i, 64 * i + 64)
                p1s = []
                for h in range(8):
                    r = rot(); mm(r, r[:], WT[h][:], Sst[h][:], [WT[h], Sst[h]]); p1s.append(r)
                for h in range(8):
                    tt("dve", VN[h], VN[h][rr, :], Usb[h][rr, :], p1s[h][rr, :], ALU.subtract, [Usb[h], p1s[h]])
                p2s = []
                for h in range(8):
                    r = rot()
                    mm(r, r[:], QG[h][:], Sst[h][:], [QG[h], Sst[h]], start=True, stop=False)
                    mm(r, r[:], QKm[h][rr, :], VN[h][rr, :], [QKm[h], VN[h]], start=False, stop=True)
                    p2s.append(r)
                for h in range(8):
                    cp("act", Osb, Osb[rr, h, :], p2s[h][rr, :], [p2s[h]])
                p3s = []
                for h in range(8):
                    r = rot(); mm(r, r[:], KG[h][rr, :], VN[h][rr, :], [KG[h], VN[h]]); p3s.append(r)
                for h in range(8):
                    stt("dve", Sst[h], Sst[h][:], Sst[h][:], EGL[:, 2 * h + i:2 * h + i + 1], p3s[h][:], ALU.mult, ALU.add, [Sst[h], EGL, p3s[h]])
            if maxphase < 7: continue
            for zc in range(2):
                for c in range(KC):
                    mm(zps, zps[:], H[:, c, cs], W[:, c, 2048 + zc * 512:2048 + (zc + 1) * 512], [W, H], start=(c == 0), stop=(c == KC - 1))
                act(ZS[0], ZS[0][:, zc * 512:(zc + 1) * 512], zps[:], AF.Silu, [zps])
            tt("pool", SQo, SQo[:], Osb[:], Osb[:], ALU.mult, [Osb])
            P.op("dve", lambda e: e.tensor_reduce(out=MS[:], in_=SQo[:], axis=AX.X, op=ALU.add), reads=[SQo], writes=[MS])
            act(MS, MS[:], MS[:], AF.Sqrt, [MS, epsb], bias=epsb[:, 0:1], scale=1.0 / 128)
            P.op("dve", lambda e: e.reciprocal(out=MS[:], in_=MS[:]), reads=[MS], writes=[MS])
            tt("dve", O2, O2[:], Osb[:], MS[:].unsqueeze(2).to_broadcast([128, 8, 128]), ALU.mult, [Osb, MS])
            tt("pool", O2, O2[:], O2[:], GN[:].unsqueeze(1).to_broadcast([128, 8, 128]), ALU.mult, [O2, GN])
            ob = OB[0]
            tt("dve", ob, ob[:], O2[:].rearrange("p a b -> p (a b)"), ZS[0][:], ALU.mult, [O2, ZS[0]])
            t0 = st * ST + tl * 128
            for h in range(8):
                r = rot(); rb = r[:].bitcast(BF16)
                P.op("pe", lambda e, rb=rb, h=h: e.transpose(out=rb[:, 0:128], in_=ob[:, h * 128:(h + 1) * 128], identity=identb[:]), reads=[ob, identb], writes=[r])
                cp("act" if h % 2 else "dve", OT, OT[:, h, :], rb[:, 0:128], [r])
            P.dma("sp", og[:, t0:t0 + 128].rearrange("(h f) t -> f h t", f=128), OT[:], reads=[OT])
    return


def host_D(hn_full, z):
    w_in = z['dn_w_in'][0]; conv = z['dn_conv'][0]
    s = np.arange(128)[:, None]; c = np.arange(128)[None, :]
    same = (s // 64) == (c // 64)
    masks = np.zeros((128, 6, 128), np.float32)
    masks[:, 0] = np.eye(128); masks[:, 1] = (same & (s <= c)); masks[:, 2] = same; masks[:, 3] = (same & (c >= s)); masks[:, 4] = -(same & (c > s)).astype(np.float32)
    masks[:, 5, 0] = (np.arange(128) < 64); masks[:, 5, 1] = (np.arange(128) >= 64)
    maps = []
    for core in range(8):
        b = core // 4; hg = core % 4
        qc = np.arange(hg * 512, hg * 512 + 512); kc = 2048 + qc; vc = 4096 + np.arange(hg * 1024, hg * 1024 + 1024); zc = 8192 + np.arange(hg * 1024, hg * 1024 + 1024)
        bc = 12288 + np.arange(hg * 8, hg * 8 + 8); ac = 12320 + np.arange(hg * 8, hg * 8 + 8)
        cols = np.concatenate([qc, kc, vc, zc, bc, ac])
        wcc = np.ascontiguousarray(w_in[:, cols])
        cch = np.concatenate([qc, kc, vc])
        convw = np.ascontiguousarray(conv[:, cch].T.reshape(16, 128, 4).transpose(1, 0, 2))
        hcc = np.zeros((128, 2, 8), np.float32)
        hcc[:, 0, :] = z['dn_a_log'][0][hg * 8:hg * 8 + 8][None]; hcc[:, 1, :] = z['dn_dt_bias'][0][hg * 8:hg * 8 + 8][None]
        gnrow = np.ascontiguousarray(np.broadcast_to(z['dn_norm'][0][None, :], (128, 128))).astype(np.float32)
        maps.append({"hnT": hn_full[b], "wc": wcc, "convw": convw, "hc": hcc, "gnrow": gnrow, "masks": masks})
    return maps


def emit_O1p(P, io):
    ogT = io("ogT", [1024, 8192], BF16, "ExternalInput")
    w = io("w", [1024, 2048], F32, "ExternalInput")
    pb = io("pb", [8192, 2048], F32, "ExternalOutput")
    wb = P.sb([128, 8, 2048], BF16)
    wv = w.rearrange("(c p) n -> p c n", p=128)
    for n0 in range(0, 2048, 512):
        P.dma("pool", wb[:, :, n0:n0 + 512], wv[:, :, n0:n0 + 512], writes=[wb])
    a = [P.sb([128, 8, 512], BF16) for _ in range(2)]
    ob = [P.sb([128, 512], F32) for _ in range(3)]
    ps = [P.ps([128, 512]) for _ in range(3)]
    av = ogT.rearrange("(c p) t -> p c t", p=128)
    pbv = pb.rearrange("(k r p) c -> k p r c", k=16, r=4, p=128)
    n_ = 0
    for s in range(16):
        A_ = a[s % 2]
        P.dma("sp", A_[:], av[:, :, s * 512:(s + 1) * 512], writes=[A_])
        for n in range(16):
            p_ = ps[n_ % 3]; O_ = ob[n_ % 3]; n_ += 1
            for c in range(8):
                P.op("pe", lambda e, c=c, p_=p_, n=n, A_=A_: e.matmul(p_[:], lhsT=wb[:, c, n * 128:(n + 1) * 128], rhs=A_[:, c, :], start=(c == 0), stop=(c == 7)), reads=[wb, A_], writes=[p_])
            if n % 2:
                P.op("act", lambda e, p_=p_, O_=O_: e.activation(out=O_[:], in_=p_[:], func=AF.Copy), reads=[p_], writes=[O_])
            else:
                P.op("dve", lambda e, p_=p_, O_=O_: e.tensor_copy(out=O_[:], in_=p_[:]), reads=[p_], writes=[O_])
            P.dma("act" if n % 2 else "sp", pbv[n][:, :, s * 128:(s + 1) * 128], O_[:].rearrange("p (r t) -> p r t", r=4), reads=[O_])
    return

_G4 = [[0, 1, 2, 3], [4, 5, 6, 7]]


_INFO = {}


def build_fused(upto=None):
    P = Prog()
    AG = "AllGather"
    s_qT = P.scratch("s_qT", [2048, 2048], BF16); s_kT = P.scratch("s_kT", [512, 2048], BF16); s_v = P.scratch("s_v", [2048, 512], BF16)
    s_iqT = P.scratch("s_iqT", [1024, 2048], F32); s_ikT = P.scratch("s_ikT", [64, 2048], F32); s_iw = P.scratch("s_iw", [2048, 16], F32)
    g_kT = P.scratch("g_kT", [2048, 2048], BF16); g_v = P.scratch("g_v", [8192, 512], BF16); g_ik = P.scratch("g_ik", [256, 2048], F32)
    s_oT = P.scratch("s_oT", [2048, 2048], BF16); s_h1 = P.scratch("s_h1", [2048, 2048], F32); s_h3 = P.scratch("s_h3", [2048, 2048], F32)
    s_hn = P.scratch("s_hn", [2048, 2048], BF16); g_hn = P.scratch("g_hn", [8192, 2048], BF16)
    s_og = P.scratch("s_og", [1024, 8192], BF16); s_pb = P.scratch("s_pb", [8192, 2048], F32); s_mix = P.scratch("s_mix", [2048, 2048], F32)
    P.begin_phase(); emit_A(P, IO(P, "A_", {"qT": s_qT, "kT": s_kT, "v": s_v, "iqT": s_iqT, "ikT": s_ikT, "iw": s_iw})); P.end_phase()
    for k in range(2):
        P.coll(AG, ALU.bypass, _G4, s_kT[k * 256:(k + 1) * 256, :], g_kT[k * 1024:(k + 1) * 1024, :])
    for k in range(2):
        P.coll(AG, ALU.bypass, _G4, s_v[k * 1024:(k + 1) * 1024, :], g_v[k * 4096:(k + 1) * 4096, :])
    P.coll(AG, ALU.bypass, _G4, s_ikT, g_ik)
    bB = {"qTc": s_qT, "kT": g_kT, "v": g_v, "ikT": g_ik, "iqTc": s_iqT, "iwc": s_iw, "oT": s_oT}
    if upto == "B":
        del bB["oT"]
    P.begin_phase(); emit_B(P, IO(P, "B_", bB)); P.end_phase()
    if upto == "B":
        _INFO["in"] = list(P.ext_in); _INFO["out"] = list(P.ext_out)
        return P.finish()
    bO = {"aT": s_oT, "hT": s_h1}
    if upto == "O":
        del bO["hT"]
    P.begin_phase(); emit_O(P, IO(P, "O0_", bO), 2048); P.end_phase()
    if upto == "O":
        _INFO["in"] = list(P.ext_in); _INFO["out"] = list(P.ext_out)
        return P.finish()
    P.begin_phase(); emit_P(P, IO(P, "P0_", {"hT": s_h1, "h3T": s_h3, "hnT": s_hn}), True); P.end_phase()
    for k in range(8):
        P.coll(AG, ALU.bypass, _G4, s_hn[k * 256:(k + 1) * 256, :], g_hn[k * 1024:(k + 1) * 1024, :])
    P.begin_phase(); emit_D(P, IO(P, "D_", {"hnT": g_hn, "og": s_og})); P.end_phase()
    P.begin_phase(); emit_O1p(P, IO(P, "Q_", {"ogT": s_og, "pb": s_pb})); P.end_phase()
    for k in range(16):
        P.coll("ReduceScatter", ALU.add, _G4, s_pb[k * 512:(k + 1) * 512, :], s_mix[k * 128:(k + 1) * 128, :])
    P.begin_phase(); emit_P(P, IO(P, "P1_", {"hT": s_h3, "mixT": s_mix}), False, add=True); P.end_phase()
    print("fused stats", P.stats(), flush=True)
    _INFO["in"] = list(P.ext_in); _INFO["out"] = list(P.ext_out)
    return P.finish()


def own_pos(c):
    return np.concatenate([np.arange((4 * j + c) * 128, (4 * j + c + 1) * 128) for j in range(16)])


def host_fused(z):
    x = z['x']
    ident = np.eye(128, dtype=np.float32).astype(BF)
    uT = [np.ascontiguousarray(z['peer_u'][l].T) for l in range(2)]
    keysT = [np.ascontiguousarray(z['peer_keys'][l].reshape(16, 128, 128).transpose(2, 0, 1)) for l in range(2)]
    gns = []
    for l in range(2):
        gnext = z['norm_mix'][l + 1] if l == 0 else np.ones(D, np.float32)
        gns.append(np.ascontiguousarray(np.stack([z['norm_ffn'][l].reshape(KC, 128).T, z['norm_ple'][l].reshape(KC, 128).T, gnext.reshape(KC, 128).T], axis=1)))
    dmaps = host_D([None, None], z)
    maps = []
    for core in range(8):
        b = core // 4; c = core % 4
        pos = own_pos(c)
        csq, csi, perm = rope_tables(pos)
        xT = np.ascontiguousarray(x[b, pos, :].T)
        r = np.arange(128)[:, None]; t = np.arange(512)[None, :]
        m = {"A_xT": xT, "A_gmix": np.ascontiguousarray(z['norm_mix'][0].reshape(KC, 128).T), "A_w_in": z['attn_w_in'][0],
             "A_qkg": np.ascontiguousarray(np.stack([z['attn_q_norm'][0], z['attn_k_norm'][0]], axis=1)), "A_csq": csq, "A_csi": csi, "A_perm": perm,
             "B_cbias": np.where(t <= c * 128 + r, 0.0, -1e5).astype(np.float32), "B_ident": ident,
             "O0_resT": xT, "O0_w": z['attn_w_out'][0]}
        for l, pre in ((0, "P0_"), (1, "P1_")):
            m.update({pre + "gn": gns[l], pre + "w_q": z['peer_w_q'][l], pre + "keysT": keysT[l], pre + "uT": uT[l], pre + "vt": z['peer_v'][l],
                      pre + "wg": z['ple_w_gate'][l], pre + "wpi": z['ple_w_in'][l], pre + "pT": np.ascontiguousarray(z['p'][l, b, pos, :].T), pre + "ident": ident})
        for k in ("wc", "convw", "hc", "gnrow", "masks"):
            m["D_" + k] = dmaps[core][k]
        m["Q_w"] = np.ascontiguousarray(z['dn_w_out'][0][c * 1024:(c + 1) * 1024, :])
        maps.append(m)
    return maps


def kernel(**inputs):
    z = {k: np.ascontiguousarray(np.asarray(v)) for k, v in inputs.items()}
    nc = build_fused()
    maps = [{k: m[k] for k in _INFO["in"]} for m in host_fused(z)]
    res = run_bass_kernel_spmd(nc, maps, core_ids=list(range(8))).results
    out = np.zeros((2, 8192, 2048), np.float32)
    for core in range(8):
        b = core // 4; c = core % 4
        out[b, own_pos(c), :] = res[core]["P1_h3T"].T
    return out
```
